# Optimizing a Trainium2 kernel written in Bass

```python
import math
import jax
import jax.numpy as jnp
from jax import lax
import numpy as np

D_MODEL = 2048
BATCH = 4
SEQ = 2048
DEPTH = 1

D_MIX = D_MODEL
NORM_EPS = 1e-6
ATT_HEADS = 8
ATT_QK_DIM = 64
ATT_V_DIM = 2 * ATT_QK_DIM
ATT_WIDTH = ATT_HEADS * ATT_V_DIM
ATT_QK_COLS = ATT_HEADS * 2 * ATT_QK_DIM
ATT_COLS = 2 * ATT_QK_COLS + ATT_WIDTH
ROPE_THETA = 500000.0
ROPE_DIM = ATT_QK_DIM // 4
Q_BLOCK = 128
SUBLN_EPS = 1e-5
RWKV_WIDTH = D_MIX - ATT_WIDTH
RWKV_HEAD = 64
RWKV_HEADS = RWKV_WIDTH // RWKV_HEAD
DECAY_LORA = 64
ICLR_LORA = 64
GATE_LORA = 160
GN_EPS = 64e-5
SHIFT_WIDTH = 3 * RWKV_WIDTH + DECAY_LORA + ICLR_LORA + GATE_LORA
SHIFT_SPLITS = [RWKV_WIDTH, 2 * RWKV_WIDTH, 3 * RWKV_WIDTH,
                3 * RWKV_WIDTH + DECAY_LORA, 3 * RWKV_WIDTH + DECAY_LORA + ICLR_LORA]
IN_COLS = ATT_COLS + SHIFT_WIDTH
N_DIRS = 2
N_EXPERTS = 16
CAPACITY_FACTOR = 2
EXPERT_FF = 2048

kernel_name = 'hybrid_diffattn_rwkv7_ecmoe'


def rms_norm(x, w, eps=NORM_EPS):
    xf = x.astype(jnp.float32)
    y = xf * lax.rsqrt(jnp.mean(xf * xf, axis=-1, keepdims=True) + eps)
    return (y * w.astype(jnp.float32)).astype(x.dtype)


def partial_rope(x, cos, sin):
    xf = x.astype(jnp.float32)
    half = ROPE_DIM // 2
    x1 = xf[..., :half]
    x2 = xf[..., half:ROPE_DIM]
    out = jnp.concatenate([x1 * cos - x2 * sin, x2 * cos + x1 * sin, xf[..., ROPE_DIM:]], axis=-1)
    return out.astype(x.dtype)


def diff_attention(q, k, v, positions, lq1, lk1, lq2, lk2, subln_w, lambda_init):
    bsz, seq = q.shape[0], q.shape[1]
    inv_freq = ROPE_THETA ** (-jnp.arange(0, ROPE_DIM, 2, dtype=jnp.float32) / ROPE_DIM)
    ang = positions.astype(jnp.float32)[..., None] * inv_freq
    cos = jnp.cos(ang)[:, :, None, None, :]
    sin = jnp.sin(ang)[:, :, None, None, :]
    q = partial_rope(q, cos, sin) * (ATT_QK_DIM ** -0.5)
    k = partial_rope(k, cos, sin)
    f32 = jnp.float32
    lam = (jnp.exp(jnp.sum(lq1.astype(f32) * lk1.astype(f32)))
           - jnp.exp(jnp.sum(lq2.astype(f32) * lk2.astype(f32))) + lambda_init)
    n_blocks = seq // Q_BLOCK
    q_blocks = jnp.moveaxis(q.reshape(bsz, n_blocks, Q_BLOCK, ATT_HEADS, 2, ATT_QK_DIM), 1, 0)

    def block(qb):
        s = jnp.einsum('bqhcd,bkhcd->bhcqk', qb, k).astype(f32)
        p = jax.nn.softmax(s, axis=-1)
        wgt = p[:, :, 0] - lam * p[:, :, 1]
        return jnp.einsum('bhqk,bkhe->bqhe', wgt.astype(v.dtype), v)

    o = lax.map(block, q_blocks)
    o = jnp.moveaxis(o, 0, 1).reshape(bsz, seq, ATT_HEADS, ATT_V_DIM)
    o = rms_norm(o, subln_w, eps=SUBLN_EPS) * (1.0 - lambda_init)
    return o.reshape(bsz, seq, ATT_WIDTH)


def _tm_shared(t):
    s = jnp.stack([t, jnp.flip(t, axis=1)], axis=0)
    return jnp.moveaxis(s, 2, 0).astype(jnp.float32)


def _tm_dir(t):
    s = jnp.stack([t[0], jnp.flip(t[1], axis=1)], axis=0)
    return jnp.moveaxis(s, 2, 0).astype(jnp.float32)


def _rwkv_step(state, inp):
    r_t, w_t, k_t, v_t, a_t, b_t = inp
    sa = jnp.einsum('dbhvk,dbhk->dbhv', state, a_t)
    state = state * w_t[..., None, :] + sa[..., None] * b_t[..., None, :] + v_t[..., None] * k_t[..., None, :]
    y = jnp.einsum('dbhvk,dbhk->dbhv', state, r_t)
    return state, y


def rwkv7_bidir(z, w0, decay_up, a0, iclr_up, gate_up, k_k, k_a, r_k, ln_x_w, ln_x_b):
    f32 = jnp.float32
    bsz, seq = z.shape[0], z.shape[1]
    r, k, v, wd, ad, gd = jnp.split(z, SHIFT_SPLITS, axis=-1)
    w_log = -jax.nn.softplus(-(w0.astype(f32)[:, None, None, :]
                               + jnp.einsum('btr,drc->dbtc', jnp.tanh(wd), decay_up).astype(f32))) - 0.5
    decay = jnp.exp(-jnp.exp(w_log))
    a = jax.nn.sigmoid(a0.astype(f32)[:, None, None, :]
                       + jnp.einsum('btr,drc->dbtc', ad, iclr_up).astype(f32))
    g = (jax.nn.sigmoid(gd) @ gate_up).astype(f32)

    def heads(t):
        return t.reshape(t.shape[:-1] + (RWKV_HEADS, RWKV_HEAD))

    kk = heads((k * k_k).astype(f32))
    kk = kk / jnp.maximum(jnp.sqrt(jnp.sum(kk * kk, axis=-1, keepdims=True)), 1e-12)
    k_dir = heads(k.astype(f32)[None] * (1.0 + (a - 1.0) * k_a.astype(f32)))
    r_h = heads(r.astype(f32))
    v_h = heads(v.astype(f32))
    a_h = heads(a)
    xs = (_tm_shared(r_h), _tm_dir(heads(decay)), _tm_dir(k_dir), _tm_shared(v_h),
          _tm_shared(-kk), _tm_dir(kk[None] * a_h))
    state0 = jnp.zeros((N_DIRS, bsz, RWKV_HEADS, RWKV_HEAD, RWKV_HEAD), f32)
    _, ys = lax.scan(_rwkv_step, state0, xs)
    ys = jnp.moveaxis(ys, 0, 2)
    y = ys[0] + jnp.flip(ys[1], axis=1)
    mu = jnp.mean(y, axis=-1, keepdims=True)
    var = jnp.mean(jnp.square(y - mu), axis=-1, keepdims=True)
    y = ((y - mu) * lax.rsqrt(var + GN_EPS)).reshape(bsz, seq, RWKV_WIDTH)
    y = y * ln_x_w.astype(f32) + ln_x_b.astype(f32)
    bonus = jnp.sum(jnp.sum(r_h[None] * k_dir * r_k.astype(f32), axis=-1, keepdims=True), axis=0) * v_h
    out = (y + bonus.reshape(bsz, seq, RWKV_WIDTH)) * g
    return out.astype(z.dtype)


def expert_choice_moe(h, w_router, e_gate, e_up, e_down):
    bsz, seq, d = h.shape
    cap = CAPACITY_FACTOR * seq // N_EXPERTS
    aff = jax.nn.softmax((h @ w_router).astype(jnp.float32), axis=-1)
    gates, idx = lax.top_k(jnp.swapaxes(aff, 1, 2), cap)
    flat_idx = idx.reshape(bsz, N_EXPERTS * cap)
    xe = jnp.take_along_axis(h, flat_idx[..., None], axis=1).reshape(bsz, N_EXPERTS, cap, d)
    hid = jax.nn.silu(jnp.einsum('becd,edf->becf', xe, e_gate)) * jnp.einsum('becd,edf->becf', xe, e_up)
    ye = jnp.einsum('becf,efd->becd', hid, e_down) * gates[..., None].astype(h.dtype)
    seg = (jnp.arange(bsz, dtype=jnp.int32)[:, None] * seq + flat_idx).reshape(-1)
    out = jax.ops.segment_sum(ye.reshape(-1, d), seg, num_segments=bsz * seq)
    return out.reshape(bsz, seq, d)


def setup_inputs(seed: int = 0) -> dict:
    key = jax.random.key(seed)
    ks = jax.random.split(key, 32)
    f32 = jnp.float32
    L = DEPTH

    def nrm(k, shape, scale):
        return jax.random.normal(k, shape, f32) * scale

    def gain(k, shape):
        return 1.0 + 0.02 * jax.random.normal(k, shape, f32)

    return {
        'x': jax.random.normal(ks[0], (BATCH, SEQ, D_MODEL), f32),
        'positions': jnp.broadcast_to(jnp.arange(SEQ, dtype=jnp.int32), (BATCH, SEQ)),
        'norm1_w': gain(ks[1], (L, D_MODEL)),
        'w_in': nrm(ks[2], (L, D_MODEL, IN_COLS), D_MODEL ** -0.5),
        'mu_prev': jax.random.uniform(ks[3], (L, SHIFT_WIDTH), f32, 0.0, 0.5),
        'mu_next': jax.random.uniform(ks[4], (L, SHIFT_WIDTH), f32, 0.0, 0.5),
        'lambda_q1': nrm(ks[5], (L, ATT_QK_DIM), 0.1),
        'lambda_k1': nrm(ks[6], (L, ATT_QK_DIM), 0.1),
        'lambda_q2': nrm(ks[7], (L, ATT_QK_DIM), 0.1),
        'lambda_k2': nrm(ks[8], (L, ATT_QK_DIM), 0.1),
        'subln_w': gain(ks[9], (L, ATT_V_DIM)),
        'w0': jax.random.uniform(ks[10], (L, N_DIRS, RWKV_WIDTH), f32, -6.0, -1.0),
        'decay_up': nrm(ks[11], (L, N_DIRS, DECAY_LORA, RWKV_WIDTH), 0.1),
        'a0': nrm(ks[12], (L, N_DIRS, RWKV_WIDTH), 0.5),
        'iclr_up': nrm(ks[13], (L, N_DIRS, ICLR_LORA, RWKV_WIDTH), ICLR_LORA ** -0.5),
        'gate_up': nrm(ks[14], (L, GATE_LORA, RWKV_WIDTH), GATE_LORA ** -0.5),
        'k_k': 0.85 + 0.05 * jax.random.normal(ks[15], (L, RWKV_WIDTH), f32),
        'k_a': 1.0 + 0.05 * jax.random.normal(ks[16], (L, RWKV_WIDTH), f32),
        'r_k': nrm(ks[17], (L, RWKV_HEADS, RWKV_HEAD), 0.1),
        'ln_x_w': gain(ks[18], (L, RWKV_WIDTH)),
        'ln_x_b': nrm(ks[19], (L, RWKV_WIDTH), 0.02),
        'w_out': nrm(ks[20], (L, D_MIX, D_MODEL), D_MIX ** -0.5),
        'norm2_w': gain(ks[21], (L, D_MODEL)),
        'w_router': nrm(ks[22], (L, D_MODEL, N_EXPERTS), D_MODEL ** -0.5),
        'e_gate': nrm(ks[23], (L, N_EXPERTS, D_MODEL, EXPERT_FF), D_MODEL ** -0.5),
        'e_up': nrm(ks[24], (L, N_EXPERTS, D_MODEL, EXPERT_FF), D_MODEL ** -0.5),
        'e_down': nrm(ks[25], (L, N_EXPERTS, EXPERT_FF, D_MODEL), EXPERT_FF ** -0.5),
        'norm_f_w': gain(ks[26], (D_MODEL,)),
    }


def reference(x, positions, norm1_w, w_in, mu_prev, mu_next, lambda_q1, lambda_k1, lambda_q2,
              lambda_k2, subln_w, w0, decay_up, a0, iclr_up, gate_up, k_k, k_a, r_k, ln_x_w,
              ln_x_b, w_out, norm2_w, w_router, e_gate, e_up, e_down, norm_f_w):
    bsz, seq, _ = x.shape
    for l in range(DEPTH):
        lambda_init = 0.8 - 0.6 * math.exp(-0.3 * l)
        h = rms_norm(x, norm1_w[l])
        proj = h @ w_in[l]
        q = proj[..., :ATT_QK_COLS].reshape(bsz, seq, ATT_HEADS, 2, ATT_QK_DIM)
        k = proj[..., ATT_QK_COLS:2 * ATT_QK_COLS].reshape(bsz, seq, ATT_HEADS, 2, ATT_QK_DIM)
        v = proj[..., 2 * ATT_QK_COLS:ATT_COLS].reshape(bsz, seq, ATT_HEADS, ATT_V_DIM)
        att = diff_attention(q, k, v, positions, lambda_q1[l], lambda_k1[l], lambda_q2[l],
                             lambda_k2[l], subln_w[l], lambda_init)
        z = proj[..., ATT_COLS:]
        z_prev = jnp.pad(z[:, :-1], ((0, 0), (1, 0), (0, 0)))
        z_next = jnp.pad(z[:, 1:], ((0, 0), (0, 1), (0, 0)))
        z = z + mu_prev[l] * (z_prev - z) + mu_next[l] * (z_next - z)
        rw = rwkv7_bidir(z, w0[l], decay_up[l], a0[l], iclr_up[l], gate_up[l], k_k[l], k_a[l],
                         r_k[l], ln_x_w[l], ln_x_b[l])
        x = x + jnp.concatenate([att, rw], axis=-1) @ w_out[l]
        x = x + expert_choice_moe(rms_norm(x, norm2_w[l]), w_router[l], e_gate[l], e_up[l], e_down[l])
    return rms_norm(x, norm_f_w)
```

```python
import numpy as np
import ml_dtypes
from contextlib import ExitStack
import concourse.bass as bass
import concourse.mybir as mybir
from concourse.bass_utils import run_bass_kernel_spmd

F32 = mybir.dt.float32
BF16 = mybir.dt.bfloat16
I32 = mybir.dt.int32
ALU = mybir.AluOpType
AF = mybir.ActivationFunctionType
AX = mybir.AxisListType
ENG = ['pe', 'act', 'dve', 'pool', 'sp']
NDS = 24


class V:
    def __init__(s, tile, ap):
        s.tile = tile
        s.ap = ap

    def __getitem__(s, k):
        return V(s.tile, s.ap[k])

    def re(s, pat, **kw):
        return V(s.tile, s.ap.rearrange(pat, **kw))

    def bc(s, shape):
        return V(s.tile, s.ap.to_broadcast(shape))

    def bitcast(s, dt):
        return V(s.tile, s.ap.bitcast(dt))


class Tile:
    def __init__(s, h, name):
        s.h = h
        s.name = name
        s.w = None
        s.r = {}

    def __getitem__(s, k):
        return V(s, s.h[k])

    def v(s):
        return V(s, s.h[:])


class Prog:
    def __init__(s, nc, es):
        s.nc = nc
        s.es = es
        s.q = {e: [] for e in ENG}
        s.cnt = {e: 0 for e in ENG}
        s.sems = {e: es.enter_context(nc.semaphore("s_" + e)) for e in ENG}
        s.dsems = [es.enter_context(nc.semaphore("d%d" % i)) for i in range(NDS)]
        s.dcnt = [0] * NDS
        s.dnext = 0
        s.seen = {e: {} for e in ENG}
        s.n = 0

    def semof(s, k):
        return s.dsems[k[1]] if isinstance(k, tuple) else s.sems[k]

    def sb(s, name, shape, dt):
        return Tile(s.es.enter_context(s.nc.sbuf_tensor(name, list(shape), dt)), name)

    def ps(s, name, shape, dt):
        return Tile(s.es.enter_context(s.nc.psum_tensor(name, list(shape), dt)), name)

    def dram(s, name, shape, dt, kind="Internal"):
        return Tile(s.nc.dram_tensor(name, list(shape), dt, kind=kind), name)

    def op(s, eng, fn, reads, writes, dma=False, acc=False):
        waits = {}

        def need(k):
            if k is None:
                return
            waits[k[0]] = max(waits.get(k[0], 0), k[1])

        for v in reads:
            need(v.tile.w)
        for v in writes:
            t = v.tile
            if not (acc and t.w is not None and t.w[0] == 'pe'):
                need(t.w)
            for k, val in t.r.items():
                need((k, val))
        if dma:
            i = s.dnext
            s.dnext = (i + 1) % NDS
            key = ('d', i)
            need((key, s.dcnt[i]))
            s.dcnt[i] += 16
            val = s.dcnt[i]
            inc = 16
        else:
            key = eng
            s.cnt[eng] += 1
            val = s.cnt[eng]
            inc = 1
        wl = []
        for k, v in waits.items():
            if v <= 0 or s.seen[eng].get(k, 0) >= v:
                continue
            s.seen[eng][k] = v
            wl.append((k, v))
        s.q[eng].append((wl, fn, key, inc))
        s.n += 1
        for v in reads:
            v.tile.r[key] = max(v.tile.r.get(key, 0), val)
        for v in writes:
            v.tile.w = (key, val)
            v.tile.r = {}

    def dma(s, out, in_, eng='sp', **kw):
        s.op(eng, lambda e: e.dma_start(out=out.ap, in_=in_.ap, **kw), [in_], [out], dma=True)

    def mm(s, out, lhsT, rhs, start=True, stop=True):
        s.op('pe', lambda e: e.matmul(out.ap, lhsT.ap, rhs.ap, start=start, stop=stop),
             [lhsT, rhs], [out], acc=not start)

    def tr(s, out, in_, ident):
        s.op('pe', lambda e: e.transpose(out.ap, in_.ap, ident.ap), [in_, ident], [out])

    def act(s, out, in_, func, bias=None, scale=None, accum=None):
        kw = {}
        rd = [in_]
        wr = [out]
        if bias is not None:
            if isinstance(bias, V):
                kw['bias'] = bias.ap
                rd.append(bias)
            else:
                kw['bias'] = bias
        if scale is not None:
            if isinstance(scale, V):
                kw['scale'] = scale.ap
                rd.append(scale)
            else:
                kw['scale'] = scale
        if accum is not None:
            kw['accum_out'] = accum.ap
            wr.append(accum)
        s.op('act', lambda e: e.activation(out.ap, in_.ap, func, **kw), rd, wr)

    def tt(s, out, a, b, op, eng='dve'):
        s.op(eng, lambda e: e.tensor_tensor(out.ap, a.ap, b.ap, op), [a, b], [out])

    def ts(s, out, a, s1, op0, s2=None, op1=None, eng='dve', accum=None):
        rd = [a]
        wr = [out]
        a1 = s1
        a2 = s2
        if isinstance(s1, V):
            rd.append(s1)
            a1 = s1.ap
        if isinstance(s2, V):
            rd.append(s2)
            a2 = s2.ap
        kw = {}
        if op1 is not None:
            kw['op1'] = op1
        if accum is not None:
            kw['accum_out'] = accum.ap
            wr.append(accum)
        s.op(eng, lambda e: e.tensor_scalar(out.ap, a.ap, a1, a2, op0, **kw), rd, wr)

    def stt(s, out, a, sc, b, op0, op1, eng='dve'):
        rd = [a, b]
        a1 = sc
        if isinstance(sc, V):
            rd.append(sc)
            a1 = sc.ap
        s.op(eng, lambda e: e.scalar_tensor_tensor(out.ap, a.ap, a1, b.ap, op0, op1), rd, [out])

    def copy(s, out, in_, eng='dve'):
        if eng == 'act':
            s.op('act', lambda e: e.copy(out.ap, in_.ap), [in_], [out])
        else:
            s.op(eng, lambda e: e.tensor_copy(out.ap, in_.ap), [in_], [out])

    def memset(s, out, val, eng='dve'):
        s.op(eng, lambda e: e.memset(out.ap, val), [], [out])

    def red(s, out, in_, op, axis=None, eng='dve'):
        ax = AX.X if axis is None else axis
        s.op(eng, lambda e: e.tensor_reduce(out.ap, in_.ap, ax, op), [in_], [out])

    def recip(s, out, in_):
        s.op('dve', lambda e: e.reciprocal(out.ap, in_.ap), [in_], [out])

    def rsqrt(s, out, in_, mul, add):
        s.ts(out, in_, mul, ALU.mult, add, ALU.add)
        s.act(out, out, AF.Sqrt)
        s.recip(out, out)

    def push(s):
        s._outer = s.es
        s._inner = ExitStack()
        s.es = s._inner

    def pop(s):
        targets = [(e, s.cnt[e]) for e in ENG] + [(('d', i), s.dcnt[i]) for i in range(NDS)]
        for e in ENG:
            wl = []
            for k, v in targets:
                if v <= 0 or s.seen[e].get(k, 0) >= v:
                    continue
                s.seen[e][k] = v
                wl.append((k, v))
            s.q[e].append((wl, None, None, 0))
        s.emit()
        s._inner.close()
        s.es = s._outer

    def coll(s, kind, op, ins, outs, groups):
        s.op('pool', lambda e: e.collective_compute(kind, op, replica_groups=groups,
                                                    ins=[i.ap for i in ins], outs=[o.ap for o in outs]),
             ins, outs, dma=True)

    def finish(s, outs, scratch_dst, scratch_src):
        s.op('sp', lambda e: e.dma_start(out=scratch_dst.ap, in_=scratch_src.ap), list(outs) + [scratch_src],
             [scratch_dst], dma=True)
        k, val = scratch_dst.tile.w
        s.q['sp'].append(([(k, val)], None, None, 0))

    def emit(s):
        nc = s.nc
        qs = s.q
        s.q = {e: [] for e in ENG}
        with nc.Block() as block:
            s._emit_block(block, qs)

    def _emit_block(s, block, qs):
        waited = set()
        for e in ENG:
            for wl, fn, key, inc in qs[e]:
                for k, v in wl:
                    waited.add((k, v))
        if not hasattr(s, 'sig'):
            s.sig = {e: 0 for e in ENG}
            s.pos = {e: 0 for e in ENG}
            s.remap = {e: {0: 0} for e in ENG}
        plan = {}
        for e in ENG:
            pos = s.pos[e]
            sig = s.sig[e]
            flags = []
            for wl, fn, key, inc in qs[e]:
                if fn is None or isinstance(key, tuple):
                    flags.append(False)
                    continue
                pos += 1
                if (e, pos) in waited:
                    sig += 1
                    s.remap[e][pos] = sig
                    flags.append(True)
                else:
                    flags.append(False)
            s.pos[e] = pos
            s.sig[e] = sig
            plan[e] = flags

        def mk(e):
            def body(eng):
                for (wl, fn, key, inc), flag in zip(qs[e], plan[e]):
                    for k, v in wl:
                        if isinstance(k, tuple):
                            eng.wait_ge(s.semof(k), v)
                        else:
                            eng.wait_ge(s.semof(k), s.remap[k][v])
                    if fn is None:
                        continue
                    ins = fn(eng)
                    if isinstance(key, tuple):
                        ins.then_inc(s.semof(key), inc)
                    elif flag:
                        ins.then_inc(s.semof(key), 1)
            return body

        block.tensor(mk('pe'))
        block.scalar(mk('act'))
        block.vector(mk('dve'))
        block.gpsimd(mk('pool'))
        block.sync(mk('sp'))
D = 2048
T = 2048
NT = 16
NCOL = 6432
NCH = 51
EPS = 1e-6


def consts_np():
    c = {}
    c['ident_f'] = np.eye(128, dtype=np.float32)
    rho = np.arange(128)
    hh = rho // 64
    ii = rho % 64
    same = (hh[:, None] == hh[None, :])
    SL = (same & (ii[None, :] < ii[:, None])).astype(np.float32)
    IL = (same & (ii[None, :] <= ii[:, None])).astype(np.float32)
    masks = np.stack([SL, SL.T, IL, IL.T], 0)
    c['masks'] = np.ascontiguousarray(np.tile(masks[:, :, None, :], (1, 1, 4, 1)).reshape(4, 128, 512))
    rm = np.ones((128, T), np.float32)
    rm[:, ::64] = 0.0
    c['resetmask'] = rm
    c['iota1'] = np.tile(np.arange(1, 257, dtype=np.float32)[None, :], (128, 1))
    invf = np.zeros((128, 1), np.float32)
    PT = np.zeros((128, 128), np.float32)
    for cc in range(2):
        for j in range(8):
            f = 500000.0 ** (-(2.0 * j) / 16.0)
            p1 = cc * 64 + j
            p2 = cc * 64 + 8 + j
            invf[p1, 0] = f
            invf[p2, 0] = f
            PT[p2, p1] = -1.0
            PT[p1, p2] = 1.0
    c['invf'] = invf
    c['PT'] = PT
    c['blockones'] = same.astype(np.float32)
    sel = np.zeros((128, 64), np.float32)
    sel[rho, ii] = 1.0
    c['sel'] = sel
    c['iota_p'] = np.arange(128, dtype=np.float32).reshape(128, 1)
    return c


def build(stage=99):
    nc = bass.Bass("TRN2", target_bir_lowering=False)
    es = ExitStack()
    dbg = {}
    with es:
        p = Prog(nc, es)
        IN = lambda n, s, dt=F32: p.dram(n, s, dt, kind="ExternalInput")
        x = IN("x", [T, D])
        pos = IN("pos", [1, T], I32)
        n1w = IN("n1w", [128, 16])
        w_in = IN("w_in", [D, NCOL])
        out = p.dram("out", [T, D], F32, kind="ExternalOutput")
        projT = p.dram("projT", [NCH * 128, T], F32, kind=("ExternalOutput" if stage == 1 else "Internal"))
        scr = p.dram("scr", [1, 16], F32)
        scr2 = p.dram("scr2", [1, 16], F32)
        cn = consts_np()
        C = {k: IN("c_" + k, list(v.shape)) for k, v in cn.items()}

        ident_f = p.sb("ident_f", [128, 128], F32)
        ident_b = p.sb("ident_b", [128, 128], BF16)
        p.dma(ident_f.v(), C['ident_f'].v())
        p.copy(ident_b.v(), ident_f.v())
        n1w_s = p.sb("n1w_s", [128, 16], F32)
        p.dma(n1w_s.v(), n1w.v())

        PB = [p.ps("pb%d" % i, [128, 512], F32) for i in range(6)]
        PH = [p.ps("ph%d" % i, [128, 1024], BF16) for i in range(2)]
        st = {'pb': 0, 'ph': 0}

        def bank():
            st['pb'] = (st['pb'] + 1) % 6
            return PB[st['pb']]

        def hbank():
            st['ph'] = (st['ph'] + 1) % 2
            return PH[st['ph']]

        p.push()
        hT = p.sb("hT", [128, 16, T], BF16)
        xt = [p.sb("xt%d" % i, [128, D], F32) for i in range(2)]
        xn = [p.sb("xn%d" % i, [128, D], BF16) for i in range(2)]
        junk = p.sb("junk", [128, D], F32)
        ssq = p.sb("ssq", [128, NT], F32)
        rstd = p.sb("rstd", [128, NT], F32)
        for i in range(NT):
            a = xt[i % 2]
            b = xn[i % 2]
            p.dma(a.v(), x[i * 128:(i + 1) * 128, :], eng='sp' if i % 2 == 0 else 'pool')
            p.act(junk.v(), a.v(), AF.Square, accum=ssq[:, i:i + 1])
            p.rsqrt(rstd[:, i:i + 1], ssq[:, i:i + 1], 1.0 / D, EPS)
            p.ts(b.v(), a.v(), rstd[:, i:i + 1], ALU.mult)
            for g in range(2):
                hb = hbank()
                for j in range(8):
                    kc = g * 8 + j
                    p.tr(hb[:, j * 128:(j + 1) * 128], b[:, kc * 128:(kc + 1) * 128], ident_b.v())
                src = hb.v().re("p (j t) -> p j t", j=8)
                dst = hT[:, g * 8:(g + 1) * 8, i * 128:(i + 1) * 128]
                if g == 0:
                    p.copy(dst, src, eng='act')
                else:
                    p.copy(dst, src, eng='dve')

        wf = [p.sb("wf%d" % i, [128, 16, 128], F32) for i in range(2)]
        wb = [p.sb("wb%d" % i, [128, 16, 128], BF16) for i in range(2)]
        prow = [p.sb("prow%d" % i, [128, T], F32) for i in range(2)]
        n1w_bc = n1w_s.v().re("p (k o) -> p k o", o=1)
        for ch in range(NCH):
            c0 = ch * 128
            m = min(128, NCOL - c0)
            f = wf[ch % 2]
            b = wb[ch % 2]
            pr = prow[ch % 2]
            p.dma(f[:, :, 0:m], w_in[:, c0:c0 + m].re("(k p) c -> p k c", p=128), eng='sp')
            p.tt(b[:, :, 0:m], f[:, :, 0:m], n1w_bc.bc([128, 16, m]), ALU.mult, eng='pool')
            for tb in range(4):
                bk = bank()
                for kc in range(16):
                    p.mm(bk[0:m, :], b[:, kc, 0:m], hT[:, kc, tb * 512:(tb + 1) * 512],
                         start=(kc == 0), stop=(kc == 15))
                p.copy(pr[0:m, tb * 512:(tb + 1) * 512], bk[0:m, :], eng='act' if tb % 2 == 0 else 'dve')
            p.dma(projT[c0:c0 + m, :], pr[0:m, :], eng='pool')
        if stage == 1:
            p.dma(scr2.v(), projT[0:1, 0:16])
            p.finish([projT.v()], scr.v(), scr2.v())
            p.pop()
            return nc, cn
        p.pop()

        mix_d = p.dram("mix_d", [128, 16, T], BF16)
        lam4 = IN("lam4", [1, 256])
        sublnw = IN("sublnw", [1, 128])
        p.push()
        PTf = p.sb("PTf", [128, 128], F32)
        PTb = p.sb("PTb", [128, 128], BF16)
        p.dma(PTf.v(), C['PT'].v())
        p.copy(PTb.v(), PTf.v())
        invf = p.sb("invf", [128, 1], F32)
        p.dma(invf.v(), C['invf'].v())
        posi = p.sb("posi", [128, T], I32)
        p.dma(posi.v(), pos[0:1, :].bc([128, T]))
        ang = p.sb("ang", [128, T], F32)
        cosF = p.sb("cosF", [128, T], F32)
        sinF = p.sb("sinF", [128, T], F32)
        p.copy(ang.v(), posi.v())
        p.ts(ang.v(), ang.v(), invf[:, 0:1], ALU.mult)
        angi = posi
        def rangered(dst, src):
            p.ts(t1x.v(), src, 1.0 / (2 * np.pi), ALU.mult)
            p.copy(angi.v(), t1x.v())
            p.copy(t1x.v(), angi.v())
            p.stt(dst, t1x.v(), -2 * np.pi, src, ALU.mult, ALU.add)
            p.ts(t1x.v(), dst, np.pi, ALU.is_gt, 2 * np.pi, ALU.mult)
            p.tt(dst, dst, t1x.v(), ALU.subtract)
            p.ts(t1x.v(), dst, -np.pi, ALU.is_lt, 2 * np.pi, ALU.mult)
            p.tt(dst, dst, t1x.v(), ALU.add)
        t1x = p.sb("t1x", [128, T], F32)
        rangered(sinF.v(), ang.v())
        p.act(sinF.v(), sinF.v(), AF.Sin)
        p.ts(ang.v(), ang.v(), np.pi / 2, ALU.add)
        rangered(cosF.v(), ang.v())
        p.act(cosF.v(), cosF.v(), AF.Sin)
        l4 = p.sb("l4", [128, 256], F32)
        p.dma(l4.v(), lam4[0:1, :].bc([128, 256]))
        lprod = p.sb("lprod", [128, 2, 64], F32)
        p.tt(lprod.v(), l4.v().re("p (a b d) -> p a b d", a=2, b=2)[:, :, 0, :],
             l4.v().re("p (a b d) -> p a b d", a=2, b=2)[:, :, 1, :], ALU.mult)
        lsum = p.sb("lsum", [128, 2], F32)
        p.red(lsum.v(), lprod.v(), ALU.add)
        p.act(lsum.v(), lsum.v(), AF.Exp)
        lam = p.sb("lam", [128, 1], F32)
        p.tt(lam.v(), lsum[:, 0:1], lsum[:, 1:2], ALU.subtract)
        p.ts(lam.v(), lam.v(), 0.2, ALU.add)
        slw = p.sb("slw", [128, 128], F32)
        p.dma(slw.v(), sublnw[0:1, :].bc([128, 128]))
        p.ts(slw.v(), slw.v(), 0.8, ALU.mult)

        qf = p.sb("qf", [128, T], F32)
        kf = p.sb("kf", [128, T], F32)
        vf = p.sb("vf", [128, T], F32)
        xq16 = p.sb("xq16", [128, T], BF16)
        xk16 = p.sb("xk16", [128, T], BF16)
        xv16 = p.sb("xv16", [128, T], BF16)
        t1 = p.sb("t1", [128, T], F32)
        QR = [p.sb("qr%d" % i, [128, T], BF16) for i in range(2)]
        KR = [p.sb("kr%d" % i, [128, T], BF16) for i in range(2)]
        VA = [p.sb("vaug%d" % i, [128, 16, 129], BF16) for i in range(2)]
        for i in range(2):
            p.memset(VA[i].v(), 1.0)
        pT = [p.sb("pT%d" % i, [128, 512], BF16) for i in range(3)]
        oacc = [p.sb("oacc%d" % i, [128, 4, 129], F32) for i in range(2)]
        att = p.sb("att", [128, 4, 128], F32)
        att1 = p.sb("att1", [128, 4, 128], F32)
        attb = p.sb("attb", [128, 4, 128], BF16)
        rr = p.sb("rr", [128, 2, 4], F32)
        ssa = p.sb("ssa", [128, 4], F32)
        mst = [p.sb("mst%d" % i, [128, 512], BF16) for i in range(2)]
        cnt = 0

        def prep_load(h):
            p.dma(qf.v(), projT[h * 128:(h + 1) * 128, :], eng='sp')
            p.dma(kf.v(), projT[1024 + h * 128:1024 + (h + 1) * 128, :], eng='sp')
            p.dma(vf.v(), projT[2048 + h * 128:2048 + (h + 1) * 128, :], eng='sp')
            p.copy(xq16.v(), qf.v(), eng='pool')
            p.copy(xk16.v(), kf.v(), eng='pool')
            p.copy(xv16.v(), vf.v(), eng='pool')
            p.tt(qf.v(), qf.v(), cosF.v(), ALU.mult, eng='pool')
            p.tt(kf.v(), kf.v(), cosF.v(), ALU.mult, eng='pool')

        def prep_pe(h):
            qr, kr, vaug = QR[h % 2], KR[h % 2], VA[h % 2]
            for (x16, src, dst) in ((xq16, qf, qr), (xk16, kf, kr)):
                for tb in range(4):
                    bk = PB[4 + tb % 2]
                    sl = slice(tb * 512, (tb + 1) * 512)
                    p.mm(bk.v(), PTb.v(), x16[:, sl])
                    p.tt(t1[:, sl], bk.v(), sinF[:, sl], ALU.mult)
                p.tt(dst.v(), src.v(), t1.v(), ALU.add)
            for g in range(2):
                hb = hbank()
                for j in range(8):
                    kt = g * 8 + j
                    p.tr(hb[:, j * 128:(j + 1) * 128], xv16[:, kt * 128:(kt + 1) * 128], ident_b.v())
                p.copy(vaug[:, g * 8:(g + 1) * 8, 0:128], hb.v().re("p (j e) -> p j e", j=8), eng='dve')

        prep_load(0)
        prep_pe(0)
        for h in range(8):
            qr, kr, vaug = QR[h % 2], KR[h % 2], VA[h % 2]
            for qb in range(4):
                if h + 1 < 8 and qb == 0:
                    prep_load(h + 1)
                if h + 1 < 8 and qb == 2:
                    prep_pe(h + 1)
                qsl = slice(qb * 512, (qb + 1) * 512)
                its = [(c, kt) for c in range(2) for kt in range(16)]

                def qk(i):
                    c, kt = its[i]
                    ps_ = slice(64 * c, 64 * c + 64)
                    p.mm(PB[4 + i % 2].v(), kr[ps_, kt * 128:(kt + 1) * 128], qr[ps_, qsl])
                qk(0)
                for i, (c, kt) in enumerate(its):
                    if i + 1 < len(its):
                        qk(i + 1)
                    sbk = PB[4 + i % 2]
                    pt_ = pT[i % 3]
                    p.act(pt_.v(), sbk.v(), AF.Exp, scale=0.125)
                    for qs in range(4):
                        p.mm(PB[qs][:, 0:129], pt_[:, qs * 128:(qs + 1) * 128], vaug[:, kt, :],
                             start=(kt == 0), stop=(kt == 15))
                    if kt == 15:
                        for qs in range(4):
                            p.copy(oacc[c][:, qs, :], PB[qs][:, 0:129], eng='dve')
                p.recip(rr[:, 0, :], oacc[0][:, :, 128])
                p.recip(rr[:, 1, :], oacc[1][:, :, 128])
                p.ts(rr[:, 1, :], rr[:, 1, :], lam[:, 0:1], ALU.mult)
                p.tt(att.v(), oacc[0][:, :, 0:128], rr[:, 0, :].re("p (q o) -> p q o", o=1).bc([128, 4, 128]), ALU.mult)
                p.tt(att1.v(), oacc[1][:, :, 0:128], rr[:, 1, :].re("p (q o) -> p q o", o=1).bc([128, 4, 128]), ALU.mult, eng='pool')
                p.tt(att.v(), att.v(), att1.v(), ALU.subtract)
                p.tt(att1.v(), att.v(), att.v(), ALU.mult, eng='pool')
                p.red(ssa.v(), att1.v(), ALU.add)
                p.rsqrt(ssa.v(), ssa.v(), 1.0 / 128, 1e-5)
                p.tt(att.v(), att.v(), ssa.v().re("p (q o) -> p q o", o=1).bc([128, 4, 128]), ALU.mult)
                p.tt(attb.v(), att.v(), slw.v().re("p (o e) -> p o e", o=1).bc([128, 4, 128]), ALU.mult)
                hb = hbank()
                for qs in range(4):
                    p.tr(hb[:, qs * 128:(qs + 1) * 128], attb[:, qs, :], ident_b.v())
                ms_ = mst[(h * 4 + qb) % 2]
                p.copy(ms_.v(), hb[:, 0:512], eng='act')
                p.dma(mix_d[:, h, qsl], ms_.v(), eng='pool')
        p.pop()

        NHP = 8 if stage != 3 else 1
        rwkv_stage(p, locals())
        tail_stage(p, locals())
    return nc, cn


def moe_hook(p, L, i):
    pass


def rwkv_inputs(inp):
    m = {}

    def cols28(mu):
        o = np.zeros((128, 28), np.float32)
        o[:, 0:24] = mu[0:3072].reshape(24, 128).T
        o[0:64, 24] = mu[3072:3136]
        o[64:128, 25] = mu[3136:3200]
        o[:, 26] = mu[3200:3328]
        o[0:32, 27] = mu[3328:3360]
        return o
    m["muP"] = cols28(inp['mu_prev'][0])
    m["muN"] = cols28(inp['mu_next'][0])
    m["w0r"] = np.ascontiguousarray(inp['w0'][0].reshape(2, 8, 128).transpose(2, 0, 1).reshape(128, 16))
    m["a0r"] = np.ascontiguousarray(inp['a0'][0].reshape(2, 8, 128).transpose(2, 0, 1).reshape(128, 16))
    m["kkr"] = np.ascontiguousarray(inp['k_k'][0].reshape(8, 128).T)
    m["kar"] = np.ascontiguousarray(inp['k_a'][0].reshape(8, 128).T)
    m["rkr"] = np.ascontiguousarray(inp['r_k'][0].reshape(8, 128).T)
    m["lnw_tok"] = np.ascontiguousarray(np.repeat(inp['ln_x_w'][0].reshape(8, 2, 1, 64), 64, axis=2).reshape(8, 128, 64))
    m["lnb_tok"] = np.ascontiguousarray(np.repeat(inp['ln_x_b'][0].reshape(8, 2, 1, 64), 64, axis=2).reshape(8, 128, 64))
    m["decay_up"] = np.ascontiguousarray(inp['decay_up'][0])
    m["iclr_up"] = np.ascontiguousarray(inp['iclr_up'][0])
    m["gate_up"] = np.ascontiguousarray(inp['gate_up'][0])
    return {k: v.astype(np.float32) for k, v in m.items()}


def kernel(**inputs):
    inp = {k: np.asarray(v) for k, v in inputs.items()}
    nc, cn = build(99)

    def core_inputs(b):
        m = {"x": np.ascontiguousarray(inp['x'][b]),
             "pos": np.ascontiguousarray(inp['positions'][b][None, :].astype(np.int32)),
             "n1w": np.ascontiguousarray(inp['norm1_w'][0].reshape(16, 128).T),
             "w_in": np.ascontiguousarray(inp['w_in'][0]),
             "lam4": np.concatenate([inp['lambda_q1'][0], inp['lambda_k1'][0], inp['lambda_q2'][0],
                                     inp['lambda_k2'][0]])[None, :].astype(np.float32),
             "sublnw": inp['subln_w'].astype(np.float32).reshape(1, 128),
             "w_out": np.ascontiguousarray(inp['w_out'][0]),
             "nfw": inp['norm_f_w'].astype(np.float32).reshape(1, D),
             "n2w": inp['norm2_w'].astype(np.float32).reshape(1, D),
             "w_router": w_router, "e_gate": e_gate, "e_up": e_up, "e_down": e_down}
        m.update(rw)
        for k, v in cn.items():
            m["c_" + k] = v
        return m

    rw = rwkv_inputs(inp)
    w_router = np.ascontiguousarray(inp['w_router'][0])
    e_gate = np.ascontiguousarray(inp['e_gate'][0])
    e_up = np.ascontiguousarray(inp['e_up'][0])
    e_down = np.ascontiguousarray(inp['e_down'][0])
    maps = [core_inputs(c // 2) for c in range(8)]
    res = run_bass_kernel_spmd(nc, maps, core_ids=list(range(8)))
    return np.stack([res.results[2 * b]["out"] for b in range(4)], 0).astype(np.float32)


def rwkv_stage(p, L):
    IN = L['IN']
    C = L['C']
    PB = L['PB']
    bank = L['bank']
    hbank = L['hbank']
    ident_b = L['ident_b']
    ident_f = L['ident_f']
    projT = L['projT']
    mix_d = L['mix_d']
    muP_d = IN("muP", [128, 28])
    muN_d = IN("muN", [128, 28])
    w0r_d = IN("w0r", [128, 16])
    a0r_d = IN("a0r", [128, 16])
    kkr_d = IN("kkr", [128, 8])
    kar_d = IN("kar", [128, 8])
    rkr_d = IN("rkr", [128, 8])
    lnw_d = IN("lnw_tok", [8, 128, 64])
    lnb_d = IN("lnb_tok", [8, 128, 64])
    dup_d = IN("decay_up", [2, 64, 1024])
    iup_d = IN("iclr_up", [2, 64, 1024])
    gup_d = IN("gate_up", [160, 1024])
    p.push()
    SEG = 512
    muP = p.sb("muP_s", [128, 28], F32)
    muN = p.sb("muN_s", [128, 28], F32)
    mu0 = p.sb("mu0_s", [128, 28], F32)
    p.dma(muP.v(), muP_d.v())
    p.dma(muN.v(), muN_d.v())
    p.tt(mu0.v(), muP.v(), muN.v(), ALU.add)
    p.ts(mu0.v(), mu0.v(), -1.0, ALU.mult, 1.0, ALU.add)
    w0r = p.sb("w0r_s", [128, 16], F32)
    a0r = p.sb("a0r_s", [128, 16], F32)
    kkr = p.sb("kkr_s", [128, 8], F32)
    kar = p.sb("kar_s", [128, 8], F32)
    rkr = p.sb("rkr_s", [128, 8], F32)
    for t_, d_ in ((w0r, w0r_d), (a0r, a0r_d), (kkr, kkr_d), (kar, kar_d), (rkr, rkr_d)):
        p.dma(t_.v(), d_.v())
    masks = p.sb("masks_s", [128, 4, 128], F32)
    p.dma(masks.v(), C['masks'].v().re("m p (n r) -> p m n r", n=4)[:, :, 0, :])
    rmask = p.sb("rmask", [128, SEG], F32)
    p.dma(rmask.v(), C['resetmask'][:, 0:SEG])
    bones_f = p.sb("bones_f", [128, 128], F32)
    bones = p.sb("bones", [128, 128], BF16)
    p.dma(bones_f.v(), C['blockones'].v())
    p.copy(bones.v(), bones_f.v())
    sel_f = p.sb("sel_f", [128, 64], F32)
    sel_b = p.sb("sel_b", [128, 64], BF16)
    p.dma(sel_f.v(), C['sel'].v())
    p.copy(sel_b.v(), sel_f.v())
    ones_b = p.sb("ones_b", [128, 1], BF16)
    p.memset(ones_b.v(), 1.0)
    stg = p.sb("lstg", [128, 512], F32)
    lwb = p.sb("lwb", [128, 2, 1024], BF16)
    gup0 = p.sb("gup0", [128, 1024], BF16)
    gup1 = p.sb("gup1", [32, 1024], BF16)
    for d_ in range(2):
        for hf in range(2):
            cs_ = slice(hf * 512, (hf + 1) * 512)
            p.dma(stg[0:64, :], dup_d[d_][:, cs_])
            p.dma(stg[64:128, :], iup_d[d_][:, cs_])
            p.copy(lwb[:, d_, cs_], stg.v())
    for hf in range(2):
        cs_ = slice(hf * 512, (hf + 1) * 512)
        p.dma(stg.v(), gup_d[0:128, cs_])
        p.copy(gup0[:, cs_], stg.v())
        p.dma(stg[0:32, :], gup_d[128:160, cs_])
        p.copy(gup1[:, cs_], stg[0:32, :])

    zs = [p.sb("zs%d" % i, [128, SEG + 2], F32) for i in range(2)]
    zcnt = [0]

    def shift_load(r0, m, mucol, seg, dst, p0=0):
        z = zs[zcnt[0] % 2]
        zcnt[0] += 1
        t0 = seg * SEG - 1
        lo = max(t0, 0)
        hi = min(seg * SEG + SEG + 1, T)
        ps_ = slice(p0, p0 + m)
        if seg == 0:
            p.memset(z[ps_, 0:1], 0.0, eng='pool')
        if hi - t0 < SEG + 2:
            p.memset(z[ps_, SEG + 1:SEG + 2], 0.0, eng='pool')
        p.dma(z[ps_, lo - t0:hi - t0], projT[r0:r0 + m, lo:hi], eng='sp')
        p.ts(dst, z[ps_, 1:SEG + 1], mu0[ps_, mucol:mucol + 1], ALU.mult)
        p.stt(dst, z[ps_, 0:SEG], muP[ps_, mucol:mucol + 1], dst, ALU.mult, ALU.add)
        p.stt(dst, z[ps_, 2:SEG + 2], muN[ps_, mucol:mucol + 1], dst, ALU.mult, ALU.add)

    lact = p.sb("lact", [128, T], BF16)
    sgd0 = p.sb("sgd0", [128, T], BF16)
    sgd1 = p.sb("sgd1", [32, T], BF16)
    TA_ = [p.sb("rw_ta%d" % d_, [128, SEG], F32) for d_ in range(2)]
    ltmp = TA_[0]
    for seg in range(4):
        sl = slice(seg * SEG, (seg + 1) * SEG)
        shift_load(6144, 64, 24, seg, ltmp[0:64, :])
        p.act(lact[0:64, sl], ltmp[0:64, :], AF.Tanh)
        shift_load(6208, 64, 25, seg, ltmp[64:128, :], p0=64)
        p.copy(lact[64:128, sl], ltmp[64:128, :], eng='dve')
        shift_load(6272, 128, 26, seg, ltmp[0:128, :])
        p.act(sgd0[:, sl], ltmp[0:128, :], AF.Sigmoid)
        shift_load(6400, 32, 27, seg, ltmp[0:32, :])
        p.act(sgd1[:, sl], ltmp[0:32, :], AF.Sigmoid)

    def f32t(name):
        return p.sb(name, [128, SEG], F32)
    F32S = [{n: f32t("rw_%s%d" % (n, d_)) for n in "zr zk zv kk logw aic kdir bq cf ci tb".split()} for d_ in range(2)]
    SQB = [p.sb("rw_sqb%d" % d_, [128, SEG], BF16) for d_ in range(2)]
    gam = [p.sb("rw_gam%d" % d_, [128, 8], F32) for d_ in range(2)]

    def etile(name):
        t_ = p.sb(name, [128, 8, 128], BF16)
        p.memset(t_.v(), 0.0, eng='pool')
        return t_
    ES = [{n: etile("rw_%s%d" % (n, d_)) for n in "kE bE khE bhE vE zE".split()} for d_ in range(2)]
    for d_ in range(2):
        t_ = p.sb("rw_arE%d" % d_, [128, 8, 256], BF16)
        p.memset(t_.v(), 0.0, eng='pool')
        ES[d_]['arE'] = t_
    CM = p.sb("rw_cmask", [128, 2, 256], F32)
    p.copy(CM[:, 0, 0:128], masks[:, 1, :], eng='pool')
    p.copy(CM[:, 0, 128:256], masks[:, 3, :], eng='pool')
    p.copy(CM[:, 1, 0:128], masks[:, 0, :], eng='pool')
    p.copy(CM[:, 1, 128:256], masks[:, 2, :], eng='pool')
    AW = [p.sb("rw_aW%d" % d_, [128, 8, 128], BF16) for d_ in range(2)]
    VW = [p.sb("rw_vW%d" % d_, [128, 8, 128], BF16) for d_ in range(2)]
    bW = [p.sb("rw_bW%d" % d_, [128, 8, 128], BF16) for d_ in range(2)]
    kW = [p.sb("rw_kW%d" % d_, [128, 8, 128], BF16) for d_ in range(2)]
    vst = [p.sb("rw_vst%d" % d_, [128, 8, 64], BF16) for d_ in range(2)]
    yacc = [p.sb("rw_yacc%d" % i, [128, 8, 64], F32) for i in range(4)]
    vst_all = [p.sb("rw_vstall%d" % i, [128, 8, 64], BF16) for i in range(4)]
    bsc = [p.sb("rw_bsc%d" % i, [128, 8], F32) for i in range(4)]
    Sm = [[p.sb("rw_Sm%d%d" % (d_, i), [128, 64], F32) for i in range(2)] for d_ in range(2)]
    Sb = [[p.sb("rw_Sb%d%d" % (d_, i), [128, 64], BF16) for i in range(2)] for d_ in range(2)]
    scnt = [0, 0]

    GS, NG = 2, 4

    def gt(name, n2=NG, w=128):
        return [[p.sb("rw_%s%d%d" % (name, d_, i), [128, GS, w], BF16) for i in range(n2)] for d_ in range(2)]
    SLb, YLb = [gt(n) for n in "SL YL".split()]
    LMb = gt("LM", w=256)
    KMb = gt("KM", w=256)
    TNb = gt("TN", w=64)
    Lb = [p.sb("rw_L%d" % i, [128, GS, 128], BF16) for i in range(NG)]
    TAb = [p.sb("rw_TA%d" % i, [128, GS, 128], BF16) for i in range(NG)]
    Pb = [[p.sb("rw_P%d%d" % (g_, i), [128, GS, 128], BF16) for i in range(2)] for g_ in range(NG)]
    PTb = [[p.sb("rw_PT%d%d" % (g_, i), [128, GS, 128], BF16) for i in range(2)] for g_ in range(NG)]
    TTb = [[p.sb("rw_TT%d%d" % (g_, i), [128, GS, 128], BF16) for i in range(2)] for g_ in range(NG)]
    Nb = [p.sb("rw_N%d" % i, [128, GS, 64], BF16) for i in range(NG)]
    owide = ES[0]['zE']
    zr, zk, zv = F32S[0]['zr'], F32S[0]['zk'], F32S[0]['zv']
    mst2 = [p.sb("rw_mst%d" % i, [128, SEG], BF16) for i in range(2)]
    fcnt = [0]
    fin_s = p.sb("rw_fs", [128, 8], F32)
    fin_r = p.sb("rw_fr", [128, 8], F32)
    lnw = p.sb("rw_lnw", [128, 64], F32)
    lnb = p.sb("rw_lnb", [128, 64], F32)
    gT = zv
    ev = [0]

    def evac_eng():
        ev[0] += 1
        return 'act' if ev[0] % 2 == 0 else 'dve'

    def c3(v):
        return v.re("p (c i) -> p c i", i=64)

    def prepA(hp, d, seg, first):
        zr, zk, zv, kk, logw, aic, kdir, bq, cf, ci, tb_ = [F32S[d][n] for n in "zr zk zv kk logw aic kdir bq cf ci tb".split()]
        ta = TA_[d]
        sqb = SQB[d]
        kE, bE, khE, bhE, vE, zE = [ES[d][n] for n in "kE bE khE bhE vE zE".split()]
        aE = ES[d]['arE'][:, :, 0:128]
        rE = ES[d]['arE'][:, :, 128:256]
        aW, vW = AW[d], VW[d]
        sl = slice(seg * SEG, (seg + 1) * SEG)
        cols = slice(hp * 128, (hp + 1) * 128)
        shift_load(3072 + hp * 128, 128, hp, seg, zr.v())
        yield
        shift_load(4096 + hp * 128, 128, 8 + hp, seg, zk.v())
        yield
        shift_load(5120 + hp * 128, 128, 16 + hp, seg, zv.v())
        yield
        bk = bank()
        p.mm(bk.v(), lwb[0:64, d, cols], lact[0:64, sl])
        p.act(logw.v(), bk.v(), AF.Sigmoid, bias=w0r[:, d * 8 + hp:d * 8 + hp + 1])
        p.ts(logw.v(), logw.v(), -0.6065306597126334, ALU.mult)
        yield
        bk = bank()
        p.mm(bk.v(), lwb[64:128, d, cols], lact[64:128, sl])
        p.act(aic.v(), bk.v(), AF.Sigmoid, bias=a0r[:, d * 8 + hp:d * 8 + hp + 1])
        yield
        p.ts(kk.v(), zk.v(), kkr[:, hp:hp + 1], ALU.mult)
        p.tt(sqb.v(), kk.v(), kk.v(), ALU.mult, eng='pool')
        yield
        bk = bank()
        p.mm(bk.v(), bones.v(), sqb.v())
        p.ts(ta.v(), bk.v(), 1e-24, ALU.max)
        p.act(ta.v(), ta.v(), AF.Sqrt)
        yield
        p.recip(ta.v(), ta.v())
        p.tt(kk.v(), kk.v(), ta.v(), ALU.mult, eng='pool')
        yield
        p.ts(ta.v(), aic.v(), -1.0, ALU.add, kar[:, hp:hp + 1], ALU.mult)
        p.stt(kdir.v(), ta.v(), 1.0, zk.v(), ALU.add, ALU.mult)
        yield
        p.tt(bq.v(), kk.v(), aic.v(), ALU.mult, eng='pool')
        p.tt(ta.v(), zr.v(), kdir.v(), ALU.mult, eng='pool')
        yield
        for h in range(2):
            ps_ = slice(64 * h, 64 * h + 64)
            p.ts(zE[ps_, :, 64 * h:64 * h + 64], c3(ta[ps_, :]), rkr[ps_, hp:hp + 1], ALU.mult)
        bk = bank()
        for c in range(8):
            p.mm(bk[:, c:c + 1], zE[:, c, :], ones_b.v())
        if first:
            p.copy(bsc[seg].v(), bk[:, 0:8], eng='act')
        else:
            p.tt(bsc[seg].v(), bsc[seg].v(), bk[:, 0:8], ALU.add)
        yield
        p.op('dve', lambda e: e.tensor_tensor_scan(cf.h[:], rmask.h[:], logw.h[:], 0.0, ALU.mult, ALU.add),
             [rmask.v(), logw.v()], [cf.v()])
        tot = c3(cf.v())[:, :, 63:64]
        if d == 0:
            cisrc = cf
        else:
            p.tt(ci.v(), logw.v(), cf.v(), ALU.subtract, eng='pool')
            p.tt(c3(ci.v()), c3(ci.v()), tot.bc([128, 8, 64]), ALU.add, eng='pool')
            cisrc = ci

        def wE(dst, a_, b_, neg=False):
            for h in range(2):
                ps_ = slice(64 * h, 64 * h + 64)
                o = dst[ps_, :, 64 * h:64 * h + 64]
                if neg:
                    p.stt(o, c3(a_[ps_, :]), -1.0, c3(b_[ps_, :]), ALU.mult, ALU.mult)
                else:
                    p.tt(o, c3(a_[ps_, :]), c3(b_[ps_, :]), ALU.mult, eng='pool' if h else 'dve')
        p.act(ta.v(), cisrc.v(), AF.Exp)
        wE(rE, zr, ta)
        yield
        p.act(ta.v(), cisrc.v(), AF.Exp, scale=-1.0)
        wE(kE, kdir, ta)
        yield
        wE(bE, bq, ta)
        yield
        p.tt(tb_.v(), cisrc.v(), logw.v(), ALU.subtract, eng='pool')
        p.act(tb_.v(), tb_.v(), AF.Exp)
        wE(aE, kk, tb_, neg=True)
        yield
        p.tt(c3(tb_.v()), tot.bc([128, 8, 64]), c3(cisrc.v()), ALU.subtract, eng='pool')
        p.act(tb_.v(), tb_.v(), AF.Exp)
        wE(khE, kdir, tb_)
        yield
        wE(bhE, bq, tb_)
        yield
        for h in range(2):
            ps_ = slice(64 * h, 64 * h + 64)
            p.copy(vE[ps_, :, 64 * h:64 * h + 64], c3(zv[ps_, :]), eng='pool')
        yield

    def prepB(d, seg):
        cf = F32S[d]['cf']
        khE, bhE, vE = [ES[d][n] for n in "khE bhE vE".split()]
        aE = ES[d]['arE'][:, :, 0:128]
        aW, vW = AW[d], VW[d]
        tot = c3(cf.v())[:, :, 63:64]
        p.act(gam[d].v().re("p (c o) -> p c o", o=1), tot, AF.Exp)
        for srcE, dstW in ((aE, aW), (bhE, bW[d]), (khE, kW[d]), (vE, vW)):
            hb = hbank()
            for c in range(8):
                p.tr(hb[:, c * 128:(c + 1) * 128], srcE[:, c, :], ident_b.v())
            p.copy(dstW.v(), hb.v().re("p (c r) -> p c r", c=8), eng=evac_eng())
        p.tt(vst[d].v(), vW[:, :, 0:64], vW[:, :, 64:128], ALU.add, eng='pool')
        if d == 0:
            p.copy(vst_all[seg].v(), vst[d].v(), eng='pool')

    def par_group(d, g, gi):
        kE, bE = ES[d]['kE'], ES[d]['bE']
        arE = ES[d]['arE']
        aE = arE[:, :, 0:128]
        rE = arE[:, :, 128:256]
        aW = AW[d]
        LM, KM = LMb[d][gi], KMb[d][gi]
        LT, MrbT = LM[:, :, 0:128], LM[:, :, 128:256]
        LakT, MrkT = KM[:, :, 0:128], KM[:, :, 128:256]

        def mm2(dst, lhsE):
            bk_ = bank()
            for n in range(GS):
                p.mm(bk_[:, n * 256:(n + 1) * 256], lhsE[:, g * GS + n, :], arE[:, g * GS + n, :])
            p.tt(dst.v(), bk_[:, 0:GS * 256].re("p (n r) -> p n r", n=GS),
                 CM[:, d:d + 1, :].bc([128, GS, 256]), ALU.mult)
        MS, MST, MIT = (0, 1, 3) if d == 0 else (1, 0, 2)

        def mmg(lf, rf, n_out=128):
            bk_ = bank()
            for n in range(GS):
                p.mm(bk_[:, n * n_out:(n + 1) * n_out], lf(n), rf(n))
            return bk_

        def Ec(tl):
            return lambda n, tl=tl: tl[:, g * GS + n, :]

        def Gc(tl):
            return lambda n, tl=tl: tl[:, n, :]

        def b4(bk_):
            return bk_[:, 0:GS * 128].re("p (n r) -> p n r", n=GS)

        def masked(dst, bk_, mi):
            p.tt(dst.v(), b4(bk_), masks[:, mi:mi + 1, :].bc([128, GS, 128]), ALU.mult)

        bk_ = mmg(Ec(aE), Ec(bE))
        masked(Lb[g], bk_, MS)
        mm2(LM, bE)
        tt_ = TTb[g][0]
        p.tt(tt_.v(), LT, ident_f.v().re("p (o r) -> p o r", o=1).bc([128, GS, 128]), ALU.add, eng='pool')
        yield
        P_, PT_ = Lb[g], LT
        for it in range(5):
            bk_ = mmg(Gc(PT_), Gc(P_))
            P2 = Pb[g][it % 2]
            p.copy(P2.v(), b4(bk_), eng='act')
            if it < 4:
                bk_ = mmg(Gc(P_), Gc(PT_))
                PT2 = PTb[g][it % 2]
                p.copy(PT2.v(), b4(bk_), eng='act')
            yield
            bk_ = mmg(Gc(P2), Gc(tt_))
            ttn = TTb[g][(it + 1) % 2]
            p.tt(ttn.v(), b4(bk_), tt_.v(), ALU.add)
            tt_ = ttn
            P_ = P2
            if it < 4:
                PT_ = PT2
            yield
        mm2(KM, kE)
        bk_ = mmg(Gc(tt_), Ec(aW))
        p.copy(TAb[g].v(), b4(bk_), eng='act')
        yield
        bk_ = mmg(Gc(LakT), lambda n: vst[d][:, g * GS + n, :], n_out=64)
        p.copy(Nb[g].v(), bk_[:, 0:GS * 64].re("p (n r) -> p n r", n=GS), eng='act')
        bk_ = mmg(Gc(TAb[g]), Ec(bW[d]))
        p.copy(SLb[d][gi].v(), b4(bk_), eng='act')
        bk_ = mmg(Gc(TAb[g]), Gc(MrbT))
        p.tt(YLb[d][gi].v(), b4(bk_), rE[:, g * GS:(g + 1) * GS, :], ALU.add)
        yield
        bk_ = mmg(Gc(tt_), Gc(Nb[g]), n_out=64)
        p.copy(TNb[d][gi].v(), bk_[:, 0:GS * 64].re("p (n r) -> p n r", n=GS), eng='act')
        yield

    def unit_parallel(d, gbase, extra=()):
        gens = [par_group(d, g, g) for g in range(NG)] + list(extra)
        alive = list(gens)
        while alive:
            for gen in list(alive):
                try:
                    next(gen)
                except StopIteration:
                    alive.remove(gen)
        return {g: g for g in range(NG)}

    def seq_steps(d, seg, G, first):
        order = range(8) if d == 0 else range(7, -1, -1)
        steps = []
        for c in order:
            def step(c=c):
                g = c // GS
                n = c % GS
                gi = G[g]
                k_ = scnt[d]
                scnt[d] += 1
                s_old, s_new = Sm[d][k_ % 2], Sm[d][(k_ + 1) % 2]
                b_old, b_new = Sb[d][k_ % 2], Sb[d][(k_ + 1) % 2]
                bs = bank()
                p.mm(bs[:, 0:64], bW[d][:, c, :], TNb[d][gi][:, n, :], start=True, stop=False)
                p.mm(bs[:, 0:64], kW[d][:, c, :], vst[d][:, c, :], start=False, stop=False)
                p.mm(bs[:, 0:64], SLb[d][gi][:, n, :], b_old.v(), start=False, stop=True)
                by = bank()
                p.mm(by[:, 0:64], LMb[d][gi][:, n, 128:256], TNb[d][gi][:, n, :], start=True, stop=False)
                p.mm(by[:, 0:64], KMb[d][gi][:, n, 128:256], vst[d][:, c, :], start=False, stop=False)
                p.mm(by[:, 0:64], YLb[d][gi][:, n, :], b_old.v(), start=False, stop=True)
                p.stt(b_new.v(), s_old.v(), gam[d][:, c:c + 1], bs[:, 0:64], ALU.mult, ALU.add)
                p.stt(s_new.v(), s_old.v(), gam[d][:, c:c + 1], bs[:, 0:64], ALU.mult, ALU.add)
                if first:
                    p.copy(yacc[seg][:, c, :], by[:, 0:64], eng='act')
                else:
                    p.tt(yacc[seg][:, c, :], yacc[seg][:, c, :], by[:, 0:64], ALU.add, eng='dve')
            steps.append(step)
        return steps

    def finalize(hp):
        FA = c3(zr.v())
        FB = c3(zk.v())
        p.dma(lnw.v(), lnw_d[hp])
        p.dma(lnb.v(), lnb_d[hp])
        cols = slice(hp * 128, (hp + 1) * 128)
        for seg in range(4):
            sl = slice(seg * SEG, (seg + 1) * SEG)
            cs = slice(seg * 8, (seg + 1) * 8)
            y = yacc[seg].v()
            p.red(fin_s.v(), y, ALU.add)
            p.ts(fin_s.v(), fin_s.v(), 1.0 / 64, ALU.mult)
            p.tt(FA, y, fin_s.v().re("p (c o) -> p c o", o=1).bc([128, 8, 64]), ALU.subtract)
            p.tt(FB, FA, FA, ALU.mult, eng='pool')
            p.red(fin_r.v(), FB, ALU.add)
            p.rsqrt(fin_r.v(), fin_r.v(), 1.0 / 64, 64e-5)
            p.tt(FA, FA, fin_r.v().re("p (c o) -> p c o", o=1).bc([128, 8, 64]), ALU.mult)
            p.tt(FA, FA, lnw.v().re("p (o v) -> p o v", o=1).bc([128, 8, 64]), ALU.mult)
            p.tt(FA, FA, lnb.v().re("p (o v) -> p o v", o=1).bc([128, 8, 64]), ALU.add)
            p.tt(FB, vst_all[seg].v(), bsc[seg].v().re("p (c o) -> p c o", o=1).bc([128, 8, 64]), ALU.mult, eng='pool')
            p.tt(FA, FA, FB, ALU.add)
            for h in range(2):
                ps_ = slice(64 * h, 64 * h + 64)
                p.copy(owide[ps_, :, 64 * h:64 * h + 64], c3(zr[ps_, :]), eng='pool' if h else 'dve')
            bk = bank()
            for c in range(8):
                p.mm(bk[:, c * 64:(c + 1) * 64], owide[:, c, :], sel_b.v())
            bg = bank()
            p.mm(bg.v(), gup0[:, cols], sgd0[:, sl], start=True, stop=False)
            p.mm(bg.v(), gup1[:, cols], sgd1[:, sl], start=False, stop=True)
            p.copy(gT.v(), bg.v(), eng='act')
            ms_ = mst2[fcnt[0] % 2]
            fcnt[0] += 1
            p.tt(ms_.v(), bk.v(), gT.v(), ALU.mult)
            p.dma(mix_d[:, 8 + hp, sl], ms_.v(), eng='pool')

    NHP = L.get('NHP', 8)
    gb = 0

    def drain(gen):
        for _ in gen:
            pass
    for hp in range(NHP):
        for d in range(2):
            p.memset(Sm[d][scnt[d] % 2].v(), 0.0)
            p.memset(Sb[d][scnt[d] % 2].v(), 0.0)
        U = []
        for s_ in range(4):
            U.append((0, s_, s_ < 2))
            U.append((1, 3 - s_, s_ < 2))
        drain(prepA(hp, U[0][0], U[0][1], U[0][2]))
        prepB(U[0][0], U[0][1])
        Gs = {}
        for k, (d, seg, first) in enumerate(U):
            nxt = U[k + 1] if k + 1 < len(U) else None
            extra = [prepA(hp, nxt[0], nxt[1], nxt[2])] if nxt else []
            Gs[d] = unit_parallel(d, gb, extra)
            if k % 2 == 1:
                gb += 1
                st0 = seq_steps(0, U[k - 1][1], Gs[0], first)
                st1 = seq_steps(1, seg, Gs[1], first)
                for a_, b_ in zip(st0, st1):
                    a_()
                    b_()
            if nxt:
                prepB(nxt[0], nxt[1])
        finalize(hp)
    p.pop()
def tail_stage(p, L):
    IN = L['IN']
    C = L['C']
    PB = L['PB']
    bank = L['bank']
    hbank = L['hbank']
    ident_b = L['ident_b']
    ident_f = L['ident_f']
    mix_d = L['mix_d']
    mixT = p.sb("mixT", [128, 16, T], BF16)
    x = L['x']
    out = L['out']
    scr = L['scr']
    scr2 = L['scr2']
    projT = L['projT']
    NE = L.get('NE', 16)
    w_out = IN("w_out", [D, D])
    nfw = IN("nfw", [1, D])
    n2w = IN("n2w", [1, D])
    wr_d = IN("w_router", [D, 16])
    eg_d = IN("e_gate", [16, D, D])
    eu_d = IN("e_up", [16, D, D])
    ed_d = IN("e_down", [16, D, D])
    xmid_d = p.dram("xmid_d", [T, D], F32)
    h2_d = p.dram("h2_d", [T, D], BF16)
    ye_d = p.dram("ye_d", [16, 2, 128, D], BF16)
    aff = p.sb("aff", [128, 16, 16], F32)
    valT = p.sb("valT", [128, 16, 16], F32)
    iota1 = p.sb("iota1", [128, 256], F32)
    p.dma(iota1.v(), C['iota1'].v())
    idxs = p.sb("idxs", [128, 32], F32)
    iop_f = p.sb("iop_f", [128, 1], F32)
    p.dma(iop_f.v(), C['iota_p'].v())

    p.push()
    for kc in range(16):
        p.dma(mixT[:, kc, :], mix_d[:, kc, :], eng='sp' if kc % 2 else 'pool')
    wo = p.sb("wo", [128, 16, D], BF16)
    wst = [p.sb("wst0", [128, D], F32)] * 2
    for kc in range(16):
        p.dma(wst[kc % 2].v(), w_out[kc * 128:(kc + 1) * 128, :], eng='sp')
        p.copy(wo[:, kc, :], wst[kc % 2].v(), eng='pool' if kc % 2 == 0 else 'dve')
    n2w_s = p.sb("n2w_s", [128, D], F32)
    p.dma(n2w_s.v(), n2w[0:1, :].bc([128, D]))
    wr = p.sb("wr", [128, 16, 16], F32)
    p.dma(wr.v(), wr_d.v().re("(k p) e -> p k e", p=128))
    xt2 = [p.sb("xt2_%d" % i, [128, D], F32) for i in range(2)]
    xm = [p.sb("xm%d" % i, [128, D], F32) for i in range(2)]
    h2f = p.sb("h2f", [128, D], F32)
    h2b = [p.sb("h2b0", [128, D], BF16)] * 2
    h2T = p.sb("h2T", [128, 16, 128], F32)
    ss2 = p.sb("ss2", [128, NT], F32)
    mx = p.sb("mx", [128, NT], F32)
    sm = p.sb("sm", [128, NT], F32)
    def stA(i):
        xa = xt2[i % 2]
        xo = xm[i % 2]
        p.dma(xa.v(), x[i * 128:(i + 1) * 128, :], eng='sp')
        for db in range(4):
            bk = bank()
            for kc in range(16):
                p.mm(bk.v(), mixT[:, kc, i * 128:(i + 1) * 128], wo[:, kc, db * 512:(db + 1) * 512],
                     start=(kc == 0), stop=(kc == 15))
            p.tt(xo[:, db * 512:(db + 1) * 512], bk.v(), xa[:, db * 512:(db + 1) * 512], ALU.add)
        p.dma(xmid_d[i * 128:(i + 1) * 128, :], xo.v(), eng='pool')

    def stB(i):
        xo = xm[i % 2]
        hb_ = h2b[i % 2]
        p.act(h2f.v(), xo.v(), AF.Square, accum=ss2[:, i:i + 1])
        p.rsqrt(ss2[:, i:i + 1], ss2[:, i:i + 1], 1.0 / D, EPS)
        p.ts(h2f.v(), xo.v(), ss2[:, i:i + 1], ALU.mult)
        p.tt(h2f.v(), h2f.v(), n2w_s.v(), ALU.mult)
        p.copy(hb_.v(), h2f.v(), eng='act')
        p.dma(h2_d[i * 128:(i + 1) * 128, :], hb_.v(), eng='pool')

    def stC(i):
        for q in range(4):
            bk = bank()
            for j in range(4):
                kc = q * 4 + j
                p.tr(bk[:, j * 128:(j + 1) * 128], h2f[:, kc * 128:(kc + 1) * 128], ident_f.v())
            p.copy(h2T[:, q * 4:(q + 1) * 4, :], bk.v().re("p (j t) -> p j t", j=4), eng='act' if q % 2 else 'dve')
        bk = bank()
        for kc in range(16):
            p.mm(bk[:, 0:16], h2T[:, kc, :], wr[:, kc, :], start=(kc == 0), stop=(kc == 15))
        p.red(mx[:, i:i + 1], bk[:, 0:16], ALU.max)
        p.ts(mx[:, i:i + 1], mx[:, i:i + 1], -1.0, ALU.mult)
        p.act(aff[:, i, :], bk[:, 0:16], AF.Exp, bias=mx[:, i:i + 1], accum=sm[:, i:i + 1])
        p.recip(sm[:, i:i + 1], sm[:, i:i + 1])
        p.ts(aff[:, i, :], aff[:, i, :], sm[:, i:i + 1], ALU.mult)

    stA(0)
    for i in range(NT):
        if i + 1 < NT:
            stA(i + 1)
        stB(i)
        stC(i)
    p.pop()

    p.push()
    for i in range(NT):
        p.dma(mixT[:, i, :], h2_d[i * 128:(i + 1) * 128, :], eng='sp' if i % 2 else 'pool')
    affT = p.sb("affT", [16, T], F32)
    work = p.sb("work", [16, T], F32)
    maskT = p.sb("maskT", [16, T], F32)
    onesT = p.sb("onesT", [16, T], F32)
    m8 = p.sb("m8", [16, 8], F32)
    p.memset(onesT.v(), 1.0, eng='pool')
    for q in range(4):
        bk = bank()
        for j in range(4):
            i = q * 4 + j
            p.tr(bk[0:16, j * 128:(j + 1) * 128], aff[:, i, :], ident_f.v())
        p.copy(affT[:, q * 512:(q + 1) * 512], bk[0:16, :], eng='dve')
    p.copy(work.v(), affT.v(), eng='dve')
    for r_ in range(32):
        p.op('dve', lambda e: e.max(out=m8.h[:], in_=work.h[:]), [work.v()], [m8.v()])
        if r_ < 31:
            p.op('dve', lambda e: e.match_replace(out=work.h[:], in_to_replace=m8.h[:], in_values=work.h[:],
                                                  imm_value=-1.0), [work.v(), m8.v()], [work.v()])
    p.ts(maskT.v(), affT.v(), m8[:, 7:8], ALU.is_ge)
    p.op('dve', lambda e: e.tensor_tensor_scan(work.h[:], onesT.h[:], maskT.h[:], 0.0, ALU.mult, ALU.add),
         [onesT.v(), maskT.v()], [work.v()])
    p.tt(work.v(), work.v(), maskT.v(), ALU.mult)
    bk = bank()
    for i in range(16):
        p.tr(bk[:, i * 16:(i + 1) * 16], work[:, i * 128:(i + 1) * 128], ident_f[0:16, 0:16])
    p.copy(valT.v(), bk[:, 0:256].re("p (i e) -> p i e", i=16), eng='dve')
    p.pop()

    p.push()
    h2s = mixT
    oh = p.sb("oh", [128, 16, 256], BF16)
    gm = p.sb("gm", [128, 16, 5], BF16)
    for i in range(16):
        p.memset(gm[:, i, 3:4], float(i))
        p.copy(gm[:, i, 4:5], iop_f.v())
    g1 = p.sb("g1", [128, 16], F32)
    g2 = p.sb("g2", [128, 16], F32)
    gb = p.sb("gb", [128, 16], BF16)
    gate = p.sb("gate", [128, 2], F32)
    g3 = p.sb("g3", [128, 2, 5], F32)
    xeT = p.sb("xeT", [128, 16, 256], BF16)
    hidT = oh
    ye = p.sb("ye", [128, 2, D], BF16)
    wf = [p.sb("ewf%d" % i, [128, 16, 512], F32) for i in range(2)]
    wb = [p.sb("ewb%d" % i, [128, 16, 512], BF16) for i in range(3)]
    sg = [p.sb("sg%d" % i, [128, 256], F32) for i in range(2)]
    wc = [0]

    def wload(src, e, cb):
        k = wc[0]
        wc[0] += 1
        f = wf[k % 2]
        b = wb[k % 3]
        p.dma(f.v(), src[e][:, cb * 512:(cb + 1) * 512].re("(k p) c -> p k c", p=128), eng='sp' if k % 2 else 'pool')
        p.copy(b.v(), f.v(), eng=('act', 'dve')[k % 2])
        return b

    iota_bc = iota1.v().re("p (o s) -> p o s", o=1).bc([128, 16, 256])
    for e in range(NE):
        p.tt(oh.v(), iota_bc, valT[:, :, e:e + 1].bc([128, 16, 256]), ALU.is_equal)
        p.copy(gb.v(), aff[:, :, e])
        p.copy(gm[:, :, 0], gb.v())
        p.tt(g1.v(), aff[:, :, e], gb.v(), ALU.subtract)
        p.copy(gb.v(), g1.v())
        p.copy(gm[:, :, 1], gb.v())
        p.tt(g2.v(), g1.v(), gb.v(), ALU.subtract)
        p.copy(gm[:, :, 2], g2.v())
        bk = bank()
        for sh in range(2):
            for i in range(16):
                p.mm(bk[:, sh * 8:sh * 8 + 5], oh[:, i, sh * 128:(sh + 1) * 128], gm[:, i, :],
                     start=(i == 0), stop=(i == 15))
        p.copy(g3.v(), bk[:, 0:16].re("p (a c) -> p a c", a=2)[:, :, 0:5])
        p.red(gate.v(), g3[:, :, 0:3], ALU.add)
        p.stt(idxs[:, 2 * e:2 * e + 2], g3[:, :, 3], 128.0, g3[:, :, 4], ALU.mult, ALU.add)
        for dq in range(8):
            bk = bank()
            for j in range(2):
                dc = dq * 2 + j
                for i in range(16):
                    p.mm(bk[:, j * 256:(j + 1) * 256], h2s[:, i, dc * 128:(dc + 1) * 128], oh[:, i, :],
                         start=(i == 0), stop=(i == 15))
            p.copy(xeT[:, dq * 2:dq * 2 + 2, :], bk.v().re("p (j s) -> p j s", j=2), eng='act' if dq % 2 else 'dve')
        for fb in range(4):
            wg = wload(eg_d, e, fb)
            wu = wload(eu_d, e, fb)
            for fc2 in range(4):
                fc = fb * 4 + fc2
                bg = bank()
                for dc in range(16):
                    p.mm(bg[:, 0:256], wg[:, dc, fc2 * 128:(fc2 + 1) * 128], xeT[:, dc, :],
                         start=(dc == 0), stop=(dc == 15))
                bu = bank()
                for dc in range(16):
                    p.mm(bu[:, 0:256], wu[:, dc, fc2 * 128:(fc2 + 1) * 128], xeT[:, dc, :],
                         start=(dc == 0), stop=(dc == 15))
                s_ = sg[fc % 2]
                p.act(s_.v(), bg[:, 0:256], AF.Silu)
                p.tt(hidT[:, fc, :], s_.v(), bu[:, 0:256], ALU.mult)
        for db in range(4):
            wd = wload(ed_d, e, db)
            for sh in range(2):
                bk = bank()
                for fc in range(16):
                    p.mm(bk.v(), hidT[:, fc, sh * 128:(sh + 1) * 128], wd[:, fc, :],
                         start=(fc == 0), stop=(fc == 15))
                p.ts(ye[:, sh, db * 512:(db + 1) * 512], bk.v(), gate[:, sh:sh + 1], ALU.mult)
        p.dma(ye_d[e].re("s p d -> p s d"), ye.v(), eng='sp')
    p.pop()

    p.push()
    yeh = p.sb("yeh", [128, 2 * NE, 1024], BF16)
    idm = p.sb("idm", [128, 32], F32)
    GT = p.sb("GT", [128, 32, 128], BF16)
    xr = [p.sb("xr%d" % i, [128, 1024], F32) for i in range(2)]
    iota_bc2 = iota1.v().re("p (o s) -> p o s", o=1).bc([128, 16, 256])
    for half in range(2):
        hs = slice(half * 1024, (half + 1) * 1024)
        for e in range(NE):
            p.dma(yeh[:, 2 * e:2 * e + 2, :], ye_d[e][:, :, hs].re("s p d -> p s d"), eng='sp' if e % 2 else 'pool')
        for i in range(NT):
            p.ts(idm.v(), idxs.v(), float(1 - 128 * i), ALU.add)
            p.tt(GT.v(), iota1[:, 0:128].re("p (o t) -> p o t", o=1).bc([128, 32, 128]),
                 idm.v().re("p (k o) -> p k o", o=1).bc([128, 32, 128]), ALU.is_equal)
            xa = xr[i % 2]
            p.dma(xa.v(), xmid_d[i * 128:(i + 1) * 128, hs], eng='sp')
            for db in range(2):
                bk = bank()
                for k in range(2 * NE):
                    p.mm(bk.v(), GT[:, k, :], yeh[:, k, db * 512:(db + 1) * 512], start=(k == 0), stop=(k == 2 * NE - 1))
                p.tt(xa[:, db * 512:(db + 1) * 512], xa[:, db * 512:(db + 1) * 512], bk.v(), ALU.add)
            p.dma(xmid_d[i * 128:(i + 1) * 128, hs], xa.v(), eng='pool')
    p.pop()

    p.push()
    nfw_s = p.sb("nfw_s", [128, D], F32)
    p.dma(nfw_s.v(), nfw[0:1, :].bc([128, D]))
    xf = [p.sb("xf%d" % i, [128, D], F32) for i in range(2)]
    junk2 = p.sb("junk2", [128, D], F32)
    ss3 = p.sb("ss3", [128, NT], F32)
    for i in range(NT):
        xo = xf[i % 2]
        p.dma(xo.v(), xmid_d[i * 128:(i + 1) * 128, :], eng='sp')
        p.act(junk2.v(), xo.v(), AF.Square, accum=ss3[:, i:i + 1])
        p.rsqrt(ss3[:, i:i + 1], ss3[:, i:i + 1], 1.0 / D, EPS)
        p.ts(xo.v(), xo.v(), ss3[:, i:i + 1], ALU.mult)
        p.tt(xo.v(), xo.v(), nfw_s.v(), ALU.mult)
        p.dma(out[i * 128:(i + 1) * 128, :], xo.v(), eng='pool')
    p.dma(scr2.v(), projT[0:1, 0:16])
    p.finish([out.v()], scr.v(), scr2.v())
    p.pop()
```

```python
import numpy as np
import ml_dtypes
from contextlib import ExitStack
import concourse.bass as bass
import concourse.mybir as mybir
from concourse.bass_utils import run_bass_kernel_spmd

F32 = mybir.dt.float32
BF16 = mybir.dt.bfloat16
I32 = mybir.dt.int32
ALU = mybir.AluOpType
AF = mybir.ActivationFunctionType
AX = mybir.AxisListType
ENG = ['pe', 'act', 'dve', 'pool', 'sp']
NDS = 24


class V:
    def __init__(s, tile, ap):
        s.tile = tile
        s.ap = ap

    def __getitem__(s, k):
        return V(s.tile, s.ap[k])

    def re(s, pat, **kw):
        return V(s.tile, s.ap.rearrange(pat, **kw))

    def bc(s, shape):
        return V(s.tile, s.ap.to_broadcast(shape))

    def bitcast(s, dt):
        return V(s.tile, s.ap.bitcast(dt))


class Tile:
    def __init__(s, h, name):
        s.h = h
        s.name = name
        s.w = None
        s.r = {}

    def __getitem__(s, k):
        return V(s, s.h[k])

    def v(s):
        return V(s, s.h[:])


class Prog:
    def __init__(s, nc, es):
        s.nc = nc
        s.es = es
        s.q = {e: [] for e in ENG}
        s.cnt = {e: 0 for e in ENG}
        s.sems = {e: es.enter_context(nc.semaphore("s_" + e)) for e in ENG}
        s.dsems = [es.enter_context(nc.semaphore("d%d" % i)) for i in range(NDS)]
        s.dcnt = [0] * NDS
        s.dnext = 0
        s.seen = {e: {} for e in ENG}
        s.n = 0

    def semof(s, k):
        return s.dsems[k[1]] if isinstance(k, tuple) else s.sems[k]

    def sb(s, name, shape, dt):
        return Tile(s.es.enter_context(s.nc.sbuf_tensor(name, list(shape), dt)), name)

    def ps(s, name, shape, dt):
        return Tile(s.es.enter_context(s.nc.psum_tensor(name, list(shape), dt)), name)

    def dram(s, name, shape, dt, kind="Internal"):
        return Tile(s.nc.dram_tensor(name, list(shape), dt, kind=kind), name)

    def op(s, eng, fn, reads, writes, dma=False, acc=False):
        waits = {}

        def need(k):
            if k is None:
                return
            waits[k[0]] = max(waits.get(k[0], 0), k[1])

        for v in reads:
            need(v.tile.w)
        for v in writes:
            t = v.tile
            if not (acc and t.w is not None and t.w[0] == 'pe'):
                need(t.w)
            for k, val in t.r.items():
                need((k, val))
        if dma:
            i = s.dnext
            s.dnext = (i + 1) % NDS
            key = ('d', i)
            need((key, s.dcnt[i]))
            s.dcnt[i] += 16
            val = s.dcnt[i]
            inc = 16
        else:
            key = eng
            s.cnt[eng] += 1
            val = s.cnt[eng]
            inc = 1
        wl = []
        for k, v in waits.items():
            if v <= 0 or s.seen[eng].get(k, 0) >= v:
                continue
            s.seen[eng][k] = v
            wl.append((k, v))
        s.q[eng].append((wl, fn, key, inc))
        s.n += 1
        for v in reads:
            v.tile.r[key] = max(v.tile.r.get(key, 0), val)
        for v in writes:
            v.tile.w = (key, val)
            v.tile.r = {}

    def dma(s, out, in_, eng='sp', **kw):
        s.op(eng, lambda e: e.dma_start(out=out.ap, in_=in_.ap, **kw), [in_], [out], dma=True)

    def mm(s, out, lhsT, rhs, start=True, stop=True):
        s.op('pe', lambda e: e.matmul(out.ap, lhsT.ap, rhs.ap, start=start, stop=stop),
             [lhsT, rhs], [out], acc=not start)

    def tr(s, out, in_, ident):
        s.op('pe', lambda e: e.transpose(out.ap, in_.ap, ident.ap), [in_, ident], [out])

    def act(s, out, in_, func, bias=None, scale=None, accum=None):
        kw = {}
        rd = [in_]
        wr = [out]
        if bias is not None:
            if isinstance(bias, V):
                kw['bias'] = bias.ap
                rd.append(bias)
            else:
                kw['bias'] = bias
        if scale is not None:
            if isinstance(scale, V):
                kw['scale'] = scale.ap
                rd.append(scale)
            else:
                kw['scale'] = scale
        if accum is not None:
            kw['accum_out'] = accum.ap
            wr.append(accum)
        s.op('act', lambda e: e.activation(out.ap, in_.ap, func, **kw), rd, wr)

    def tt(s, out, a, b, op, eng='dve'):
        s.op(eng, lambda e: e.tensor_tensor(out.ap, a.ap, b.ap, op), [a, b], [out])

    def ts(s, out, a, s1, op0, s2=None, op1=None, eng='dve', accum=None):
        rd = [a]
        wr = [out]
        a1 = s1
        a2 = s2
        if isinstance(s1, V):
            rd.append(s1)
            a1 = s1.ap
        if isinstance(s2, V):
            rd.append(s2)
            a2 = s2.ap
        kw = {}
        if op1 is not None:
            kw['op1'] = op1
        if accum is not None:
            kw['accum_out'] = accum.ap
            wr.append(accum)
        s.op(eng, lambda e: e.tensor_scalar(out.ap, a.ap, a1, a2, op0, **kw), rd, wr)

    def stt(s, out, a, sc, b, op0, op1, eng='dve'):
        rd = [a, b]
        a1 = sc
        if isinstance(sc, V):
            rd.append(sc)
            a1 = sc.ap
        s.op(eng, lambda e: e.scalar_tensor_tensor(out.ap, a.ap, a1, b.ap, op0, op1), rd, [out])

    def copy(s, out, in_, eng='dve'):
        if eng == 'act':
            s.op('act', lambda e: e.copy(out.ap, in_.ap), [in_], [out])
        else:
            s.op(eng, lambda e: e.tensor_copy(out.ap, in_.ap), [in_], [out])

    def memset(s, out, val, eng='dve'):
        s.op(eng, lambda e: e.memset(out.ap, val), [], [out])

    def red(s, out, in_, op, axis=None, eng='dve'):
        ax = AX.X if axis is None else axis
        s.op(eng, lambda e: e.tensor_reduce(out.ap, in_.ap, ax, op), [in_], [out])

    def recip(s, out, in_):
        s.op('dve', lambda e: e.reciprocal(out.ap, in_.ap), [in_], [out])

    def rsqrt(s, out, in_, mul, add):
        s.ts(out, in_, mul, ALU.mult, add, ALU.add)
        s.act(out, out, AF.Sqrt)
        s.recip(out, out)

    def push(s):
        s._outer = s.es
        s._inner = ExitStack()
        s.es = s._inner

    def pop(s):
        targets = [(e, s.cnt[e]) for e in ENG] + [(('d', i), s.dcnt[i]) for i in range(NDS)]
        for e in ENG:
            wl = []
            for k, v in targets:
                if v <= 0 or s.seen[e].get(k, 0) >= v:
                    continue
                s.seen[e][k] = v
                wl.append((k, v))
            s.q[e].append((wl, None, None, 0))
        s.emit()
        s._inner.close()
        s.es = s._outer

    def coll(s, kind, op, ins, outs, groups):
        s.op('pool', lambda e: e.collective_compute(kind, op, replica_groups=groups,
                                                    ins=[i.ap for i in ins], outs=[o.ap for o in outs]),
             ins, outs, dma=True)

    def finish(s, outs, scratch_dst, scratch_src):
        s.op('sp', lambda e: e.dma_start(out=scratch_dst.ap, in_=scratch_src.ap), list(outs) + [scratch_src],
             [scratch_dst], dma=True)
        k, val = scratch_dst.tile.w
        s.q['sp'].append(([(k, val)], None, None, 0))

    def emit(s):
        nc = s.nc
        qs = s.q
        s.q = {e: [] for e in ENG}
        with nc.Block() as block:
            s._emit_block(block, qs)

    def _emit_block(s, block, qs):
        waited = set()
        for e in ENG:
            for wl, fn, key, inc in qs[e]:
                for k, v in wl:
                    waited.add((k, v))
        if not hasattr(s, 'sig'):
            s.sig = {e: 0 for e in ENG}
            s.pos = {e: 0 for e in ENG}
            s.remap = {e: {0: 0} for e in ENG}
        plan = {}
        for e in ENG:
            pos = s.pos[e]
            sig = s.sig[e]
            flags = []
            for wl, fn, key, inc in qs[e]:
                if fn is None or isinstance(key, tuple):
                    flags.append(False)
                    continue
                pos += 1
                if (e, pos) in waited:
                    sig += 1
                    s.remap[e][pos] = sig
                    flags.append(True)
                else:
                    flags.append(False)
            s.pos[e] = pos
            s.sig[e] = sig
            plan[e] = flags

        def mk(e):
            def body(eng):
                for (wl, fn, key, inc), flag in zip(qs[e], plan[e]):
                    for k, v in wl:
                        if isinstance(k, tuple):
                            eng.wait_ge(s.semof(k), v)
                        else:
                            eng.wait_ge(s.semof(k), s.remap[k][v])
                    if fn is None:
                        continue
                    ins = fn(eng)
                    if isinstance(key, tuple):
                        ins.then_inc(s.semof(key), inc)
                    elif flag:
                        ins.then_inc(s.semof(key), 1)
            return body

        block.tensor(mk('pe'))
        block.scalar(mk('act'))
        block.vector(mk('dve'))
        block.gpsimd(mk('pool'))
        block.sync(mk('sp'))
D = 2048
T = 2048
NT = 16
NCOL = 6432
NCH = 51
EPS = 1e-6


def consts_np():
    c = {}
    c['ident_f'] = np.eye(128, dtype=np.float32)
    rho = np.arange(128)
    hh = rho // 64
    ii = rho % 64
    same = (hh[:, None] == hh[None, :])
    SL = (same & (ii[None, :] < ii[:, None])).astype(np.float32)
    IL = (same & (ii[None, :] <= ii[:, None])).astype(np.float32)
    masks = np.stack([SL, SL.T, IL, IL.T], 0)
    c['masks'] = np.ascontiguousarray(np.tile(masks[:, :, None, :], (1, 1, 4, 1)).reshape(4, 128, 512))
    rm = np.ones((128, T), np.float32)
    rm[:, ::64] = 0.0
    c['resetmask'] = rm
    c['iota1'] = np.tile(np.arange(1, 257, dtype=np.float32)[None, :], (128, 1))
    invf = np.zeros((128, 1), np.float32)
    PT = np.zeros((128, 128), np.float32)
    for cc in range(2):
        for j in range(8):
            f = 500000.0 ** (-(2.0 * j) / 16.0)
            p1 = cc * 64 + j
            p2 = cc * 64 + 8 + j
            invf[p1, 0] = f
            invf[p2, 0] = f
            PT[p2, p1] = -1.0
            PT[p1, p2] = 1.0
    c['invf'] = invf
    c['PT'] = PT
    c['blockones'] = same.astype(np.float32)
    sel = np.zeros((128, 64), np.float32)
    sel[rho, ii] = 1.0
    c['sel'] = sel
    c['iota_p'] = np.arange(128, dtype=np.float32).reshape(128, 1)
    return c


def build(stage=99):
    nc = bass.Bass("TRN2", target_bir_lowering=False)
    es = ExitStack()
    dbg = {}
    with es:
        p = Prog(nc, es)
        IN = lambda n, s, dt=F32: p.dram(n, s, dt, kind="ExternalInput")
        x = IN("x", [T, D])
        pos = IN("pos", [1, T], I32)
        n1w = IN("n1w", [128, 16])
        w_in = IN("w_in", [D, NCOL])
        out = p.dram("out", [T, D], F32, kind="ExternalOutput")
        projT = p.dram("projT", [NCH * 128, T], F32, kind=("ExternalOutput" if stage == 1 else "Internal"))
        scr = p.dram("scr", [1, 16], F32)
        scr2 = p.dram("scr2", [1, 16], F32)
        cn = consts_np()
        C = {k: IN("c_" + k, list(v.shape)) for k, v in cn.items()}

        ident_f = p.sb("ident_f", [128, 128], F32)
        ident_b = p.sb("ident_b", [128, 128], BF16)
        p.dma(ident_f.v(), C['ident_f'].v())
        p.copy(ident_b.v(), ident_f.v())
        n1w_s = p.sb("n1w_s", [128, 16], F32)
        p.dma(n1w_s.v(), n1w.v())

        PB = [p.ps("pb%d" % i, [128, 512], F32) for i in range(6)]
        PH = [p.ps("ph%d" % i, [128, 1024], BF16) for i in range(2)]
        st = {'pb': 0, 'ph': 0}

        def bank():
            st['pb'] = (st['pb'] + 1) % 6
            return PB[st['pb']]

        def hbank():
            st['ph'] = (st['ph'] + 1) % 2
            return PH[st['ph']]

        p.push()
        hT = p.sb("hT", [128, 16, T], BF16)
        xt = [p.sb("xt%d" % i, [128, D], F32) for i in range(2)]
        xn = [p.sb("xn%d" % i, [128, D], BF16) for i in range(2)]
        junk = p.sb("junk", [128, D], F32)
        ssq = p.sb("ssq", [128, NT], F32)
        rstd = p.sb("rstd", [128, NT], F32)
        for i in range(NT):
            a = xt[i % 2]
            b = xn[i % 2]
            p.dma(a.v(), x[i * 128:(i + 1) * 128, :], eng='sp' if i % 2 == 0 else 'pool')
            p.act(junk.v(), a.v(), AF.Square, accum=ssq[:, i:i + 1])
            p.rsqrt(rstd[:, i:i + 1], ssq[:, i:i + 1], 1.0 / D, EPS)
            p.ts(b.v(), a.v(), rstd[:, i:i + 1], ALU.mult)
            for g in range(2):
                hb = hbank()
                for j in range(8):
                    kc = g * 8 + j
                    p.tr(hb[:, j * 128:(j + 1) * 128], b[:, kc * 128:(kc + 1) * 128], ident_b.v())
                src = hb.v().re("p (j t) -> p j t", j=8)
                dst = hT[:, g * 8:(g + 1) * 8, i * 128:(i + 1) * 128]
                if g == 0:
                    p.copy(dst, src, eng='act')
                else:
                    p.copy(dst, src, eng='dve')

        wf = [p.sb("wf%d" % i, [128, 16, 128], F32) for i in range(2)]
        wb = [p.sb("wb%d" % i, [128, 16, 128], BF16) for i in range(2)]
        prow = [p.sb("prow%d" % i, [128, T], F32) for i in range(2)]
        n1w_bc = n1w_s.v().re("p (k o) -> p k o", o=1)
        for ch in range(NCH):
            c0 = ch * 128
            m = min(128, NCOL - c0)
            f = wf[ch % 2]
            b = wb[ch % 2]
            pr = prow[ch % 2]
            p.dma(f[:, :, 0:m], w_in[:, c0:c0 + m].re("(k p) c -> p k c", p=128), eng='sp')
            p.tt(b[:, :, 0:m], f[:, :, 0:m], n1w_bc.bc([128, 16, m]), ALU.mult, eng='pool')
            for tb in range(4):
                bk = bank()
                for kc in range(16):
                    p.mm(bk[0:m, :], b[:, kc, 0:m], hT[:, kc, tb * 512:(tb + 1) * 512],
                         start=(kc == 0), stop=(kc == 15))
                p.copy(pr[0:m, tb * 512:(tb + 1) * 512], bk[0:m, :], eng='act' if tb % 2 == 0 else 'dve')
            p.dma(projT[c0:c0 + m, :], pr[0:m, :], eng='pool')
        if stage == 1:
            p.dma(scr2.v(), projT[0:1, 0:16])
            p.finish([projT.v()], scr.v(), scr2.v())
            p.pop()
            return nc, cn
        p.pop()

        mix_d = p.dram("mix_d", [128, 16, T], BF16)
        lam4 = IN("lam4", [1, 256])
        sublnw = IN("sublnw", [1, 128])
        p.push()
        PTf = p.sb("PTf", [128, 128], F32)
        PTb = p.sb("PTb", [128, 128], BF16)
        p.dma(PTf.v(), C['PT'].v())
        p.copy(PTb.v(), PTf.v())
        invf = p.sb("invf", [128, 1], F32)
        p.dma(invf.v(), C['invf'].v())
        posi = p.sb("posi", [128, T], I32)
        p.dma(posi.v(), pos[0:1, :].bc([128, T]))
        ang = p.sb("ang", [128, T], F32)
        cosF = p.sb("cosF", [128, T], F32)
        sinF = p.sb("sinF", [128, T], F32)
        p.copy(ang.v(), posi.v())
        p.ts(ang.v(), ang.v(), invf[:, 0:1], ALU.mult)
        angi = posi
        def rangered(dst, src):
            p.ts(t1x.v(), src, 1.0 / (2 * np.pi), ALU.mult)
            p.copy(angi.v(), t1x.v())
            p.copy(t1x.v(), angi.v())
            p.stt(dst, t1x.v(), -2 * np.pi, src, ALU.mult, ALU.add)
            p.ts(t1x.v(), dst, np.pi, ALU.is_gt, 2 * np.pi, ALU.mult)
            p.tt(dst, dst, t1x.v(), ALU.subtract)
            p.ts(t1x.v(), dst, -np.pi, ALU.is_lt, 2 * np.pi, ALU.mult)
            p.tt(dst, dst, t1x.v(), ALU.add)
        t1x = p.sb("t1x", [128, T], F32)
        rangered(sinF.v(), ang.v())
        p.act(sinF.v(), sinF.v(), AF.Sin)
        p.ts(ang.v(), ang.v(), np.pi / 2, ALU.add)
        rangered(cosF.v(), ang.v())
        p.act(cosF.v(), cosF.v(), AF.Sin)
        l4 = p.sb("l4", [128, 256], F32)
        p.dma(l4.v(), lam4[0:1, :].bc([128, 256]))
        lprod = p.sb("lprod", [128, 2, 64], F32)
        p.tt(lprod.v(), l4.v().re("p (a b d) -> p a b d", a=2, b=2)[:, :, 0, :],
             l4.v().re("p (a b d) -> p a b d", a=2, b=2)[:, :, 1, :], ALU.mult)
        lsum = p.sb("lsum", [128, 2], F32)
        p.red(lsum.v(), lprod.v(), ALU.add)
        p.act(lsum.v(), lsum.v(), AF.Exp)
        lam = p.sb("lam", [128, 1], F32)
        p.tt(lam.v(), lsum[:, 0:1], lsum[:, 1:2], ALU.subtract)
        p.ts(lam.v(), lam.v(), 0.2, ALU.add)
        slw = p.sb("slw", [128, 128], F32)
        p.dma(slw.v(), sublnw[0:1, :].bc([128, 128]))
        p.ts(slw.v(), slw.v(), 0.8, ALU.mult)

        qf = p.sb("qf", [128, T], F32)
        kf = p.sb("kf", [128, T], F32)
        vf = p.sb("vf", [128, T], F32)
        xq16 = p.sb("xq16", [128, T], BF16)
        xk16 = p.sb("xk16", [128, T], BF16)
        xv16 = p.sb("xv16", [128, T], BF16)
        t1 = p.sb("t1", [128, T], F32)
        QR = [p.sb("qr%d" % i, [128, T], BF16) for i in range(2)]
        KR = [p.sb("kr%d" % i, [128, T], BF16) for i in range(2)]
        VA = [p.sb("vaug%d" % i, [128, 16, 129], BF16) for i in range(2)]
        for i in range(2):
            p.memset(VA[i].v(), 1.0)
        pT = [p.sb("pT%d" % i, [128, 512], BF16) for i in range(3)]
        oacc = [p.sb("oacc%d" % i, [128, 4, 129], F32) for i in range(2)]
        att = p.sb("att", [128, 4, 128], F32)
        att1 = p.sb("att1", [128, 4, 128], F32)
        attb = p.sb("attb", [128, 4, 128], BF16)
        rr = p.sb("rr", [128, 2, 4], F32)
        ssa = p.sb("ssa", [128, 4], F32)
        mst = [p.sb("mst%d" % i, [128, 512], BF16) for i in range(2)]
        cnt = 0

        def prep_load(h):
            p.dma(qf.v(), projT[h * 128:(h + 1) * 128, :], eng='sp')
            p.dma(kf.v(), projT[1024 + h * 128:1024 + (h + 1) * 128, :], eng='sp')
            p.dma(vf.v(), projT[2048 + h * 128:2048 + (h + 1) * 128, :], eng='sp')
            p.copy(xq16.v(), qf.v(), eng='pool')
            p.copy(xk16.v(), kf.v(), eng='pool')
            p.copy(xv16.v(), vf.v(), eng='pool')
            p.tt(qf.v(), qf.v(), cosF.v(), ALU.mult, eng='pool')
            p.tt(kf.v(), kf.v(), cosF.v(), ALU.mult, eng='pool')

        def prep_pe(h):
            qr, kr, vaug = QR[h % 2], KR[h % 2], VA[h % 2]
            for (x16, src, dst) in ((xq16, qf, qr), (xk16, kf, kr)):
                for tb in range(4):
                    bk = PB[4 + tb % 2]
                    sl = slice(tb * 512, (tb + 1) * 512)
                    p.mm(bk.v(), PTb.v(), x16[:, sl])
                    p.tt(t1[:, sl], bk.v(), sinF[:, sl], ALU.mult)
                p.tt(dst.v(), src.v(), t1.v(), ALU.add)
            for g in range(2):
                hb = hbank()
                for j in range(8):
                    kt = g * 8 + j
                    p.tr(hb[:, j * 128:(j + 1) * 128], xv16[:, kt * 128:(kt + 1) * 128], ident_b.v())
                p.copy(vaug[:, g * 8:(g + 1) * 8, 0:128], hb.v().re("p (j e) -> p j e", j=8), eng='dve')

        prep_load(0)
        prep_pe(0)
        for h in range(8):
            qr, kr, vaug = QR[h % 2], KR[h % 2], VA[h % 2]
            for qb in range(4):
                if h + 1 < 8 and qb == 0:
                    prep_load(h + 1)
                if h + 1 < 8 and qb == 2:
                    prep_pe(h + 1)
                qsl = slice(qb * 512, (qb + 1) * 512)
                its = [(c, kt) for c in range(2) for kt in range(16)]

                def qk(i):
                    c, kt = its[i]
                    ps_ = slice(64 * c, 64 * c + 64)
                    p.mm(PB[4 + i % 2].v(), kr[ps_, kt * 128:(kt + 1) * 128], qr[ps_, qsl])
                qk(0)
                for i, (c, kt) in enumerate(its):
                    if i + 1 < len(its):
                        qk(i + 1)
                    sbk = PB[4 + i % 2]
                    pt_ = pT[i % 3]
                    p.act(pt_.v(), sbk.v(), AF.Exp, scale=0.125)
                    for qs in range(4):
                        p.mm(PB[qs][:, 0:129], pt_[:, qs * 128:(qs + 1) * 128], vaug[:, kt, :],
                             start=(kt == 0), stop=(kt == 15))
                    if kt == 15:
                        for qs in range(4):
                            p.copy(oacc[c][:, qs, :], PB[qs][:, 0:129], eng='dve')
                p.recip(rr[:, 0, :], oacc[0][:, :, 128])
                p.recip(rr[:, 1, :], oacc[1][:, :, 128])
                p.ts(rr[:, 1, :], rr[:, 1, :], lam[:, 0:1], ALU.mult)
                p.tt(att.v(), oacc[0][:, :, 0:128], rr[:, 0, :].re("p (q o) -> p q o", o=1).bc([128, 4, 128]), ALU.mult)
                p.tt(att1.v(), oacc[1][:, :, 0:128], rr[:, 1, :].re("p (q o) -> p q o", o=1).bc([128, 4, 128]), ALU.mult, eng='pool')
                p.tt(att.v(), att.v(), att1.v(), ALU.subtract)
                p.tt(att1.v(), att.v(), att.v(), ALU.mult, eng='pool')
                p.red(ssa.v(), att1.v(), ALU.add)
                p.rsqrt(ssa.v(), ssa.v(), 1.0 / 128, 1e-5)
                p.tt(att.v(), att.v(), ssa.v().re("p (q o) -> p q o", o=1).bc([128, 4, 128]), ALU.mult)
                p.tt(attb.v(), att.v(), slw.v().re("p (o e) -> p o e", o=1).bc([128, 4, 128]), ALU.mult)
                hb = hbank()
                for qs in range(4):
                    p.tr(hb[:, qs * 128:(qs + 1) * 128], attb[:, qs, :], ident_b.v())
                ms_ = mst[(h * 4 + qb) % 2]
                p.copy(ms_.v(), hb[:, 0:512], eng='act')
                p.dma(mix_d[:, h, qsl], ms_.v(), eng='pool')
        p.pop()

        NHP = 8 if stage != 3 else 1
        rwkv_stage(p, locals())
        tail_stage(p, locals())
    return nc, cn


def moe_hook(p, L, i):
    pass


def rwkv_inputs(inp):
    m = {}

    def cols28(mu):
        o = np.zeros((128, 28), np.float32)
        o[:, 0:24] = mu[0:3072].reshape(24, 128).T
        o[0:64, 24] = mu[3072:3136]
        o[64:128, 25] = mu[3136:3200]
        o[:, 26] = mu[3200:3328]
        o[0:32, 27] = mu[3328:3360]
        return o
    m["muP"] = cols28(inp['mu_prev'][0])
    m["muN"] = cols28(inp['mu_next'][0])
    m["w0r"] = np.ascontiguousarray(inp['w0'][0].reshape(2, 8, 128).transpose(2, 0, 1).reshape(128, 16))
    m["a0r"] = np.ascontiguousarray(inp['a0'][0].reshape(2, 8, 128).transpose(2, 0, 1).reshape(128, 16))
    m["kkr"] = np.ascontiguousarray(inp['k_k'][0].reshape(8, 128).T)
    m["kar"] = np.ascontiguousarray(inp['k_a'][0].reshape(8, 128).T)
    m["rkr"] = np.ascontiguousarray(inp['r_k'][0].reshape(8, 128).T)
    m["lnw_tok"] = np.ascontiguousarray(np.repeat(inp['ln_x_w'][0].reshape(8, 2, 1, 64), 64, axis=2).reshape(8, 128, 64))
    m["lnb_tok"] = np.ascontiguousarray(np.repeat(inp['ln_x_b'][0].reshape(8, 2, 1, 64), 64, axis=2).reshape(8, 128, 64))
    m["decay_up"] = np.ascontiguousarray(inp['decay_up'][0])
    m["iclr_up"] = np.ascontiguousarray(inp['iclr_up'][0])
    m["gate_up"] = np.ascontiguousarray(inp['gate_up'][0])
    return {k: v.astype(np.float32) for k, v in m.items()}


def kernel(**inputs):
    inp = {k: np.asarray(v) for k, v in inputs.items()}
    nc, cn = build(99)

    def core_inputs(b):
        m = {"x": np.ascontiguousarray(inp['x'][b]),
             "pos": np.ascontiguousarray(inp['positions'][b][None, :].astype(np.int32)),
             "n1w": np.ascontiguousarray(inp['norm1_w'][0].reshape(16, 128).T),
             "w_in": np.ascontiguousarray(inp['w_in'][0]),
             "lam4": np.concatenate([inp['lambda_q1'][0], inp['lambda_k1'][0], inp['lambda_q2'][0],
                                     inp['lambda_k2'][0]])[None, :].astype(np.float32),
             "sublnw": inp['subln_w'].astype(np.float32).reshape(1, 128),
             "w_out": np.ascontiguousarray(inp['w_out'][0]),
             "nfw": inp['norm_f_w'].astype(np.float32).reshape(1, D),
             "n2w": inp['norm2_w'].astype(np.float32).reshape(1, D),
             "w_router": w_router, "e_gate": e_gate, "e_up": e_up, "e_down": e_down}
        m.update(rw)
        for k, v in cn.items():
            m["c_" + k] = v
        return m

    rw = rwkv_inputs(inp)
    w_router = np.ascontiguousarray(inp['w_router'][0])
    e_gate = np.ascontiguousarray(inp['e_gate'][0])
    e_up = np.ascontiguousarray(inp['e_up'][0])
    e_down = np.ascontiguousarray(inp['e_down'][0])
    maps = [core_inputs(c // 2) for c in range(8)]
    res = run_bass_kernel_spmd(nc, maps, core_ids=list(range(8)))
    return np.stack([res.results[2 * b]["out"] for b in range(4)], 0).astype(np.float32)


def rwkv_stage(p, L):
    IN = L['IN']
    C = L['C']
    PB = L['PB']
    bank = L['bank']
    hbank = L['hbank']
    ident_b = L['ident_b']
    ident_f = L['ident_f']
    projT = L['projT']
    mix_d = L['mix_d']
    muP_d = IN("muP", [128, 28])
    muN_d = IN("muN", [128, 28])
    w0r_d = IN("w0r", [128, 16])
    a0r_d = IN("a0r", [128, 16])
    kkr_d = IN("kkr", [128, 8])
    kar_d = IN("kar", [128, 8])
    rkr_d = IN("rkr", [128, 8])
    lnw_d = IN("lnw_tok", [8, 128, 64])
    lnb_d = IN("lnb_tok", [8, 128, 64])
    dup_d = IN("decay_up", [2, 64, 1024])
    iup_d = IN("iclr_up", [2, 64, 1024])
    gup_d = IN("gate_up", [160, 1024])
    p.push()
    SEG = 512
    muP = p.sb("muP_s", [128, 28], F32)
    muN = p.sb("muN_s", [128, 28], F32)
    mu0 = p.sb("mu0_s", [128, 28], F32)
    p.dma(muP.v(), muP_d.v())
    p.dma(muN.v(), muN_d.v())
    p.tt(mu0.v(), muP.v(), muN.v(), ALU.add)
    p.ts(mu0.v(), mu0.v(), -1.0, ALU.mult, 1.0, ALU.add)
    w0r = p.sb("w0r_s", [128, 16], F32)
    a0r = p.sb("a0r_s", [128, 16], F32)
    kkr = p.sb("kkr_s", [128, 8], F32)
    kar = p.sb("kar_s", [128, 8], F32)
    rkr = p.sb("rkr_s", [128, 8], F32)
    for t_, d_ in ((w0r, w0r_d), (a0r, a0r_d), (kkr, kkr_d), (kar, kar_d), (rkr, rkr_d)):
        p.dma(t_.v(), d_.v())
    masks = p.sb("masks_s", [128, 4, 128], F32)
    p.dma(masks.v(), C['masks'].v().re("m p (n r) -> p m n r", n=4)[:, :, 0, :])
    rmask = p.sb("rmask", [128, SEG], F32)
    p.dma(rmask.v(), C['resetmask'][:, 0:SEG])
    bones_f = p.sb("bones_f", [128, 128], F32)
    bones = p.sb("bones", [128, 128], BF16)
    p.dma(bones_f.v(), C['blockones'].v())
    p.copy(bones.v(), bones_f.v())
    sel_f = p.sb("sel_f", [128, 64], F32)
    sel_b = p.sb("sel_b", [128, 64], BF16)
    p.dma(sel_f.v(), C['sel'].v())
    p.copy(sel_b.v(), sel_f.v())
    ones_b = p.sb("ones_b", [128, 1], BF16)
    p.memset(ones_b.v(), 1.0)
    stg = p.sb("lstg", [128, 512], F32)
    lwb = p.sb("lwb", [128, 2, 1024], BF16)
    gup0 = p.sb("gup0", [128, 1024], BF16)
    gup1 = p.sb("gup1", [32, 1024], BF16)
    for d_ in range(2):
        for hf in range(2):
            cs_ = slice(hf * 512, (hf + 1) * 512)
            p.dma(stg[0:64, :], dup_d[d_][:, cs_])
            p.dma(stg[64:128, :], iup_d[d_][:, cs_])
            p.copy(lwb[:, d_, cs_], stg.v())
    for hf in range(2):
        cs_ = slice(hf * 512, (hf + 1) * 512)
        p.dma(stg.v(), gup_d[0:128, cs_])
        p.copy(gup0[:, cs_], stg.v())
        p.dma(stg[0:32, :], gup_d[128:160, cs_])
        p.copy(gup1[:, cs_], stg[0:32, :])

    zs = [p.sb("zs%d" % i, [128, SEG + 2], F32) for i in range(2)]
    zcnt = [0]

    def shift_load(r0, m, mucol, seg, dst, p0=0):
        z = zs[zcnt[0] % 2]
        zcnt[0] += 1
        t0 = seg * SEG - 1
        lo = max(t0, 0)
        hi = min(seg * SEG + SEG + 1, T)
        ps_ = slice(p0, p0 + m)
        if seg == 0:
            p.memset(z[ps_, 0:1], 0.0, eng='pool')
        if hi - t0 < SEG + 2:
            p.memset(z[ps_, SEG + 1:SEG + 2], 0.0, eng='pool')
        p.dma(z[ps_, lo - t0:hi - t0], projT[r0:r0 + m, lo:hi], eng='sp')
        p.ts(dst, z[ps_, 1:SEG + 1], mu0[ps_, mucol:mucol + 1], ALU.mult)
        p.stt(dst, z[ps_, 0:SEG], muP[ps_, mucol:mucol + 1], dst, ALU.mult, ALU.add)
        p.stt(dst, z[ps_, 2:SEG + 2], muN[ps_, mucol:mucol + 1], dst, ALU.mult, ALU.add)

    lact = p.sb("lact", [128, T], BF16)
    sgd0 = p.sb("sgd0", [128, T], BF16)
    sgd1 = p.sb("sgd1", [32, T], BF16)
    TA_ = [p.sb("rw_ta%d" % d_, [128, SEG], F32) for d_ in range(2)]
    ltmp = TA_[0]
    for seg in range(4):
        sl = slice(seg * SEG, (seg + 1) * SEG)
        shift_load(6144, 64, 24, seg, ltmp[0:64, :])
        p.act(lact[0:64, sl], ltmp[0:64, :], AF.Tanh)
        shift_load(6208, 64, 25, seg, ltmp[64:128, :], p0=64)
        p.copy(lact[64:128, sl], ltmp[64:128, :], eng='dve')
        shift_load(6272, 128, 26, seg, ltmp[0:128, :])
        p.act(sgd0[:, sl], ltmp[0:128, :], AF.Sigmoid)
        shift_load(6400, 32, 27, seg, ltmp[0:32, :])
        p.act(sgd1[:, sl], ltmp[0:32, :], AF.Sigmoid)

    def f32t(name):
        return p.sb(name, [128, SEG], F32)
    F32S = [{n: f32t("rw_%s%d" % (n, d_)) for n in "zr zk zv kk logw aic kdir bq cf ci tb".split()} for d_ in range(2)]
    SQB = [p.sb("rw_sqb%d" % d_, [128, SEG], BF16) for d_ in range(2)]
    gam = [p.sb("rw_gam%d" % d_, [128, 8], F32) for d_ in range(2)]

    def etile(name):
        t_ = p.sb(name, [128, 8, 128], BF16)
        p.memset(t_.v(), 0.0, eng='pool')
        return t_
    ES = [{n: etile("rw_%s%d" % (n, d_)) for n in "kE bE khE bhE vE zE".split()} for d_ in range(2)]
    for d_ in range(2):
        t_ = p.sb("rw_arE%d" % d_, [128, 8, 256], BF16)
        p.memset(t_.v(), 0.0, eng='pool')
        ES[d_]['arE'] = t_
    CM = p.sb("rw_cmask", [128, 2, 256], F32)
    p.copy(CM[:, 0, 0:128], masks[:, 1, :], eng='pool')
    p.copy(CM[:, 0, 128:256], masks[:, 3, :], eng='pool')
    p.copy(CM[:, 1, 0:128], masks[:, 0, :], eng='pool')
    p.copy(CM[:, 1, 128:256], masks[:, 2, :], eng='pool')
    AW = [p.sb("rw_aW%d" % d_, [128, 8, 128], BF16) for d_ in range(2)]
    VW = [p.sb("rw_vW%d" % d_, [128, 8, 128], BF16) for d_ in range(2)]
    bW = [p.sb("rw_bW%d" % d_, [128, 8, 128], BF16) for d_ in range(2)]
    kW = [p.sb("rw_kW%d" % d_, [128, 8, 128], BF16) for d_ in range(2)]
    vst = [p.sb("rw_vst%d" % d_, [128, 8, 64], BF16) for d_ in range(2)]
    yacc = [p.sb("rw_yacc%d" % i, [128, 8, 64], F32) for i in range(4)]
    vst_all = [p.sb("rw_vstall%d" % i, [128, 8, 64], BF16) for i in range(4)]
    bsc = [p.sb("rw_bsc%d" % i, [128, 8], F32) for i in range(4)]
    Sm = [[p.sb("rw_Sm%d%d" % (d_, i), [128, 64], F32) for i in range(2)] for d_ in range(2)]
    Sb = [[p.sb("rw_Sb%d%d" % (d_, i), [128, 64], BF16) for i in range(2)] for d_ in range(2)]
    scnt = [0, 0]

    GS, NG = 2, 4

    def gt(name, n2=NG, w=128):
        return [[p.sb("rw_%s%d%d" % (name, d_, i), [128, GS, w], BF16) for i in range(n2)] for d_ in range(2)]
    SLb, YLb = [gt(n) for n in "SL YL".split()]
    LMb = gt("LM", w=256)
    KMb = gt("KM", w=256)
    TNb = gt("TN", w=64)
    Lb = [p.sb("rw_L%d" % i, [128, GS, 128], BF16) for i in range(NG)]
    TAb = [p.sb("rw_TA%d" % i, [128, GS, 128], BF16) for i in range(NG)]
    Pb = [[p.sb("rw_P%d%d" % (g_, i), [128, GS, 128], BF16) for i in range(2)] for g_ in range(NG)]
    PTb = [[p.sb("rw_PT%d%d" % (g_, i), [128, GS, 128], BF16) for i in range(2)] for g_ in range(NG)]
    TTb = [[p.sb("rw_TT%d%d" % (g_, i), [128, GS, 128], BF16) for i in range(2)] for g_ in range(NG)]
    Nb = [p.sb("rw_N%d" % i, [128, GS, 64], BF16) for i in range(NG)]
    owide = ES[0]['zE']
    zr, zk, zv = F32S[0]['zr'], F32S[0]['zk'], F32S[0]['zv']
    mst2 = [p.sb("rw_mst%d" % i, [128, SEG], BF16) for i in range(2)]
    fcnt = [0]
    fin_s = p.sb("rw_fs", [128, 8], F32)
    fin_r = p.sb("rw_fr", [128, 8], F32)
    lnw = p.sb("rw_lnw", [128, 64], F32)
    lnb = p.sb("rw_lnb", [128, 64], F32)
    gT = zv
    ev = [0]

    def evac_eng():
        ev[0] += 1
        return 'act' if ev[0] % 2 == 0 else 'dve'

    def c3(v):
        return v.re("p (c i) -> p c i", i=64)

    def prepA(hp, d, seg, first):
        zr, zk, zv, kk, logw, aic, kdir, bq, cf, ci, tb_ = [F32S[d][n] for n in "zr zk zv kk logw aic kdir bq cf ci tb".split()]
        ta = TA_[d]
        sqb = SQB[d]
        kE, bE, khE, bhE, vE, zE = [ES[d][n] for n in "kE bE khE bhE vE zE".split()]
        aE = ES[d]['arE'][:, :, 0:128]
        rE = ES[d]['arE'][:, :, 128:256]
        aW, vW = AW[d], VW[d]
        sl = slice(seg * SEG, (seg + 1) * SEG)
        cols = slice(hp * 128, (hp + 1) * 128)
        shift_load(3072 + hp * 128, 128, hp, seg, zr.v())
        yield
        shift_load(4096 + hp * 128, 128, 8 + hp, seg, zk.v())
        yield
        shift_load(5120 + hp * 128, 128, 16 + hp, seg, zv.v())
        yield
        bk = bank()
        p.mm(bk.v(), lwb[0:64, d, cols], lact[0:64, sl])
        p.act(logw.v(), bk.v(), AF.Sigmoid, bias=w0r[:, d * 8 + hp:d * 8 + hp + 1])
        p.ts(logw.v(), logw.v(), -0.6065306597126334, ALU.mult)
        yield
        bk = bank()
        p.mm(bk.v(), lwb[64:128, d, cols], lact[64:128, sl])
        p.act(aic.v(), bk.v(), AF.Sigmoid, bias=a0r[:, d * 8 + hp:d * 8 + hp + 1])
        yield
        p.ts(kk.v(), zk.v(), kkr[:, hp:hp + 1], ALU.mult)
        p.tt(sqb.v(), kk.v(), kk.v(), ALU.mult, eng='pool')
        yield
        bk = bank()
        p.mm(bk.v(), bones.v(), sqb.v())
        p.ts(ta.v(), bk.v(), 1e-24, ALU.max)
        p.act(ta.v(), ta.v(), AF.Sqrt)
        yield
        p.recip(ta.v(), ta.v())
        p.tt(kk.v(), kk.v(), ta.v(), ALU.mult, eng='pool')
        yield
        p.ts(ta.v(), aic.v(), -1.0, ALU.add, kar[:, hp:hp + 1], ALU.mult)
        p.stt(kdir.v(), ta.v(), 1.0, zk.v(), ALU.add, ALU.mult)
        yield
        p.tt(bq.v(), kk.v(), aic.v(), ALU.mult, eng='pool')
        p.tt(ta.v(), zr.v(), kdir.v(), ALU.mult, eng='pool')
        yield
        for h in range(2):
            ps_ = slice(64 * h, 64 * h + 64)
            p.ts(zE[ps_, :, 64 * h:64 * h + 64], c3(ta[ps_, :]), rkr[ps_, hp:hp + 1], ALU.mult)
        bk = bank()
        for c in range(8):
            p.mm(bk[:, c:c + 1], zE[:, c, :], ones_b.v())
        if first:
            p.copy(bsc[seg].v(), bk[:, 0:8], eng='act')
        else:
            p.tt(bsc[seg].v(), bsc[seg].v(), bk[:, 0:8], ALU.add)
        yield
        p.op('dve', lambda e: e.tensor_tensor_scan(cf.h[:], rmask.h[:], logw.h[:], 0.0, ALU.mult, ALU.add),
             [rmask.v(), logw.v()], [cf.v()])
        tot = c3(cf.v())[:, :, 63:64]
        if d == 0:
            cisrc = cf
        else:
            p.tt(ci.v(), logw.v(), cf.v(), ALU.subtract, eng='pool')
            p.tt(c3(ci.v()), c3(ci.v()), tot.bc([128, 8, 64]), ALU.add, eng='pool')
            cisrc = ci

        def wE(dst, a_, b_, neg=False):
            for h in range(2):
                ps_ = slice(64 * h, 64 * h + 64)
                o = dst[ps_, :, 64 * h:64 * h + 64]
                if neg:
                    p.stt(o, c3(a_[ps_, :]), -1.0, c3(b_[ps_, :]), ALU.mult, ALU.mult)
                else:
                    p.tt(o, c3(a_[ps_, :]), c3(b_[ps_, :]), ALU.mult, eng='pool' if h else 'dve')
        p.act(ta.v(), cisrc.v(), AF.Exp)
        wE(rE, zr, ta)
        yield
        p.act(ta.v(), cisrc.v(), AF.Exp, scale=-1.0)
        wE(kE, kdir, ta)
        yield
        wE(bE, bq, ta)
        yield
        p.tt(tb_.v(), cisrc.v(), logw.v(), ALU.subtract, eng='pool')
        p.act(tb_.v(), tb_.v(), AF.Exp)
        wE(aE, kk, tb_, neg=True)
        yield
        p.tt(c3(tb_.v()), tot.bc([128, 8, 64]), c3(cisrc.v()), ALU.subtract, eng='pool')
        p.act(tb_.v(), tb_.v(), AF.Exp)
        wE(khE, kdir, tb_)
        yield
        wE(bhE, bq, tb_)
        yield
        for h in range(2):
            ps_ = slice(64 * h, 64 * h + 64)
            p.copy(vE[ps_, :, 64 * h:64 * h + 64], c3(zv[ps_, :]), eng='pool')
        yield

    def prepB(d, seg):
        cf = F32S[d]['cf']
        khE, bhE, vE = [ES[d][n] for n in "khE bhE vE".split()]
        aE = ES[d]['arE'][:, :, 0:128]
        aW, vW = AW[d], VW[d]
        tot = c3(cf.v())[:, :, 63:64]
        p.act(gam[d].v().re("p (c o) -> p c o", o=1), tot, AF.Exp)
        for srcE, dstW in ((aE, aW), (bhE, bW[d]), (khE, kW[d]), (vE, vW)):
            hb = hbank()
            for c in range(8):
                p.tr(hb[:, c * 128:(c + 1) * 128], srcE[:, c, :], ident_b.v())
            p.copy(dstW.v(), hb.v().re("p (c r) -> p c r", c=8), eng=evac_eng())
        p.tt(vst[d].v(), vW[:, :, 0:64], vW[:, :, 64:128], ALU.add, eng='pool')
        if d == 0:
            p.copy(vst_all[seg].v(), vst[d].v(), eng='pool')

    def par_group(d, g, gi):
        kE, bE = ES[d]['kE'], ES[d]['bE']
        arE = ES[d]['arE']
        aE = arE[:, :, 0:128]
        rE = arE[:, :, 128:256]
        aW = AW[d]
        LM, KM = LMb[d][gi], KMb[d][gi]
        LT, MrbT = LM[:, :, 0:128], LM[:, :, 128:256]
        LakT, MrkT = KM[:, :, 0:128], KM[:, :, 128:256]

        def mm2(dst, lhsE):
            bk_ = bank()
            for n in range(GS):
                p.mm(bk_[:, n * 256:(n + 1) * 256], lhsE[:, g * GS + n, :], arE[:, g * GS + n, :])
            p.tt(dst.v(), bk_[:, 0:GS * 256].re("p (n r) -> p n r", n=GS),
                 CM[:, d:d + 1, :].bc([128, GS, 256]), ALU.mult)
        MS, MST, MIT = (0, 1, 3) if d == 0 else (1, 0, 2)

        def mmg(lf, rf, n_out=128):
            bk_ = bank()
            for n in range(GS):
                p.mm(bk_[:, n * n_out:(n + 1) * n_out], lf(n), rf(n))
            return bk_

        def Ec(tl):
            return lambda n, tl=tl: tl[:, g * GS + n, :]

        def Gc(tl):
            return lambda n, tl=tl: tl[:, n, :]

        def b4(bk_):
            return bk_[:, 0:GS * 128].re("p (n r) -> p n r", n=GS)

        def masked(dst, bk_, mi):
            p.tt(dst.v(), b4(bk_), masks[:, mi:mi + 1, :].bc([128, GS, 128]), ALU.mult)

        bk_ = mmg(Ec(aE), Ec(bE))
        masked(Lb[g], bk_, MS)
        mm2(LM, bE)
        tt_ = TTb[g][0]
        p.tt(tt_.v(), LT, ident_f.v().re("p (o r) -> p o r", o=1).bc([128, GS, 128]), ALU.add, eng='pool')
        yield
        P_, PT_ = Lb[g], LT
        for it in range(5):
            bk_ = mmg(Gc(PT_), Gc(P_))
            P2 = Pb[g][it % 2]
            p.copy(P2.v(), b4(bk_), eng='act')
            if it < 4:
                bk_ = mmg(Gc(P_), Gc(PT_))
                PT2 = PTb[g][it % 2]
                p.copy(PT2.v(), b4(bk_), eng='act')
            yield
            bk_ = mmg(Gc(P2), Gc(tt_))
            ttn = TTb[g][(it + 1) % 2]
            p.tt(ttn.v(), b4(bk_), tt_.v(), ALU.add)
            tt_ = ttn
            P_ = P2
            if it < 4:
                PT_ = PT2
            yield
        mm2(KM, kE)
        bk_ = mmg(Gc(tt_), Ec(aW))
        p.copy(TAb[g].v(), b4(bk_), eng='act')
        yield
        bk_ = mmg(Gc(LakT), lambda n: vst[d][:, g * GS + n, :], n_out=64)
        p.copy(Nb[g].v(), bk_[:, 0:GS * 64].re("p (n r) -> p n r", n=GS), eng='act')
        bk_ = mmg(Gc(TAb[g]), Ec(bW[d]))
        p.copy(SLb[d][gi].v(), b4(bk_), eng='act')
        bk_ = mmg(Gc(TAb[g]), Gc(MrbT))
        p.tt(YLb[d][gi].v(), b4(bk_), rE[:, g * GS:(g + 1) * GS, :], ALU.add)
        yield
        bk_ = mmg(Gc(tt_), Gc(Nb[g]), n_out=64)
        p.copy(TNb[d][gi].v(), bk_[:, 0:GS * 64].re("p (n r) -> p n r", n=GS), eng='act')
        yield

    def unit_parallel(d, gbase, extra=()):
        gens = [par_group(d, g, g) for g in range(NG)] + list(extra)
        alive = list(gens)
        while alive:
            for gen in list(alive):
                try:
                    next(gen)
                except StopIteration:
                    alive.remove(gen)
        return {g: g for g in range(NG)}

    def seq_steps(d, seg, G, first):
        order = range(8) if d == 0 else range(7, -1, -1)
        steps = []
        for c in order:
            def step(c=c):
                g = c // GS
                n = c % GS
                gi = G[g]
                k_ = scnt[d]
                scnt[d] += 1
                s_old, s_new = Sm[d][k_ % 2], Sm[d][(k_ + 1) % 2]
                b_old, b_new = Sb[d][k_ % 2], Sb[d][(k_ + 1) % 2]
                bs = bank()
                p.mm(bs[:, 0:64], bW[d][:, c, :], TNb[d][gi][:, n, :], start=True, stop=False)
                p.mm(bs[:, 0:64], kW[d][:, c, :], vst[d][:, c, :], start=False, stop=False)
                p.mm(bs[:, 0:64], SLb[d][gi][:, n, :], b_old.v(), start=False, stop=True)
                by = bank()
                p.mm(by[:, 0:64], LMb[d][gi][:, n, 128:256], TNb[d][gi][:, n, :], start=True, stop=False)
                p.mm(by[:, 0:64], KMb[d][gi][:, n, 128:256], vst[d][:, c, :], start=False, stop=False)
                p.mm(by[:, 0:64], YLb[d][gi][:, n, :], b_old.v(), start=False, stop=True)
                p.stt(b_new.v(), s_old.v(), gam[d][:, c:c + 1], bs[:, 0:64], ALU.mult, ALU.add)
                p.stt(s_new.v(), s_old.v(), gam[d][:, c:c + 1], bs[:, 0:64], ALU.mult, ALU.add)
                if first:
                    p.copy(yacc[seg][:, c, :], by[:, 0:64], eng='act')
                else:
                    p.tt(yacc[seg][:, c, :], yacc[seg][:, c, :], by[:, 0:64], ALU.add, eng='dve')
            steps.append(step)
        return steps

    def finalize(hp):
        FA = c3(zr.v())
        FB = c3(zk.v())
        p.dma(lnw.v(), lnw_d[hp])
        p.dma(lnb.v(), lnb_d[hp])
        cols = slice(hp * 128, (hp + 1) * 128)
        for seg in range(4):
            sl = slice(seg * SEG, (seg + 1) * SEG)
            cs = slice(seg * 8, (seg + 1) * 8)
            y = yacc[seg].v()
            p.red(fin_s.v(), y, ALU.add)
            p.ts(fin_s.v(), fin_s.v(), 1.0 / 64, ALU.mult)
            p.tt(FA, y, fin_s.v().re("p (c o) -> p c o", o=1).bc([128, 8, 64]), ALU.subtract)
            p.tt(FB, FA, FA, ALU.mult, eng='pool')
            p.red(fin_r.v(), FB, ALU.add)
            p.rsqrt(fin_r.v(), fin_r.v(), 1.0 / 64, 64e-5)
            p.tt(FA, FA, fin_r.v().re("p (c o) -> p c o", o=1).bc([128, 8, 64]), ALU.mult)
            p.tt(FA, FA, lnw.v().re("p (o v) -> p o v", o=1).bc([128, 8, 64]), ALU.mult)
            p.tt(FA, FA, lnb.v().re("p (o v) -> p o v", o=1).bc([128, 8, 64]), ALU.add)
            p.tt(FB, vst_all[seg].v(), bsc[seg].v().re("p (c o) -> p c o", o=1).bc([128, 8, 64]), ALU.mult, eng='pool')
            p.tt(FA, FA, FB, ALU.add)
            for h in range(2):
                ps_ = slice(64 * h, 64 * h + 64)
                p.copy(owide[ps_, :, 64 * h:64 * h + 64], c3(zr[ps_, :]), eng='pool' if h else 'dve')
            bk = bank()
            for c in range(8):
                p.mm(bk[:, c * 64:(c + 1) * 64], owide[:, c, :], sel_b.v())
            bg = bank()
            p.mm(bg.v(), gup0[:, cols], sgd0[:, sl], start=True, stop=False)
            p.mm(bg.v(), gup1[:, cols], sgd1[:, sl], start=False, stop=True)
            p.copy(gT.v(), bg.v(), eng='act')
            ms_ = mst2[fcnt[0] % 2]
            fcnt[0] += 1
            p.tt(ms_.v(), bk.v(), gT.v(), ALU.mult)
            p.dma(mix_d[:, 8 + hp, sl], ms_.v(), eng='pool')

    NHP = L.get('NHP', 8)
    gb = 0

    def drain(gen):
        for _ in gen:
            pass
    for hp in range(NHP):
        for d in range(2):
            p.memset(Sm[d][scnt[d] % 2].v(), 0.0)
            p.memset(Sb[d][scnt[d] % 2].v(), 0.0)
        U = []
        for s_ in range(4):
            U.append((0, s_, s_ < 2))
            U.append((1, 3 - s_, s_ < 2))
        drain(prepA(hp, U[0][0], U[0][1], U[0][2]))
        prepB(U[0][0], U[0][1])
        Gs = {}
        for k, (d, seg, first) in enumerate(U):
            nxt = U[k + 1] if k + 1 < len(U) else None
            extra = [prepA(hp, nxt[0], nxt[1], nxt[2])] if nxt else []
            Gs[d] = unit_parallel(d, gb, extra)
            if k % 2 == 1:
                gb += 1
                st0 = seq_steps(0, U[k - 1][1], Gs[0], first)
                st1 = seq_steps(1, seg, Gs[1], first)
                for a_, b_ in zip(st0, st1):
                    a_()
                    b_()
            if nxt:
                prepB(nxt[0], nxt[1])
        finalize(hp)
    p.pop()
def tail_stage(p, L):
    IN = L['IN']
    C = L['C']
    PB = L['PB']
    bank = L['bank']
    hbank = L['hbank']
    ident_b = L['ident_b']
    ident_f = L['ident_f']
    mix_d = L['mix_d']
    mixT = p.sb("mixT", [128, 16, T], BF16)
    x = L['x']
    out = L['out']
    scr = L['scr']
    scr2 = L['scr2']
    projT = L['projT']
    NE = L.get('NE', 16)
    w_out = IN("w_out", [D, D])
    nfw = IN("nfw", [1, D])
    n2w = IN("n2w", [1, D])
    wr_d = IN("w_router", [D, 16])
    eg_d = IN("e_gate", [16, D, D])
    eu_d = IN("e_up", [16, D, D])
    ed_d = IN("e_down", [16, D, D])
    xmid_d = p.dram("xmid_d", [T, D], F32)
    h2_d = p.dram("h2_d", [T, D], BF16)
    ye_d = p.dram("ye_d", [16, 2, 128, D], BF16)
    aff = p.sb("aff", [128, 16, 16], F32)
    valT = p.sb("valT", [128, 16, 16], F32)
    iota1 = p.sb("iota1", [128, 256], F32)
    p.dma(iota1.v(), C['iota1'].v())
    idxs = p.sb("idxs", [128, 32], F32)
    iop_f = p.sb("iop_f", [128, 1], F32)
    p.dma(iop_f.v(), C['iota_p'].v())

    p.push()
    for kc in range(16):
        p.dma(mixT[:, kc, :], mix_d[:, kc, :], eng='sp' if kc % 2 else 'pool')
    wo = p.sb("wo", [128, 16, D], BF16)
    wst = [p.sb("wst0", [128, D], F32)] * 2
    for kc in range(16):
        p.dma(wst[kc % 2].v(), w_out[kc * 128:(kc + 1) * 128, :], eng='sp')
        p.copy(wo[:, kc, :], wst[kc % 2].v(), eng='pool' if kc % 2 == 0 else 'dve')
    n2w_s = p.sb("n2w_s", [128, D], F32)
    p.dma(n2w_s.v(), n2w[0:1, :].bc([128, D]))
    wr = p.sb("wr", [128, 16, 16], F32)
    p.dma(wr.v(), wr_d.v().re("(k p) e -> p k e", p=128))
    xt2 = [p.sb("xt2_%d" % i, [128, D], F32) for i in range(2)]
    xm = [p.sb("xm%d" % i, [128, D], F32) for i in range(2)]
    h2f = p.sb("h2f", [128, D], F32)
    h2b = [p.sb("h2b0", [128, D], BF16)] * 2
    h2T = p.sb("h2T", [128, 16, 128], F32)
    ss2 = p.sb("ss2", [128, NT], F32)
    mx = p.sb("mx", [128, NT], F32)
    sm = p.sb("sm", [128, NT], F32)
    def stA(i):
        xa = xt2[i % 2]
        xo = xm[i % 2]
        p.dma(xa.v(), x[i * 128:(i + 1) * 128, :], eng='sp')
        for db in range(4):
            bk = bank()
            for kc in range(16):
                p.mm(bk.v(), mixT[:, kc, i * 128:(i + 1) * 128], wo[:, kc, db * 512:(db + 1) * 512],
                     start=(kc == 0), stop=(kc == 15))
            p.tt(xo[:, db * 512:(db + 1) * 512], bk.v(), xa[:, db * 512:(db + 1) * 512], ALU.add)
        p.dma(xmid_d[i * 128:(i + 1) * 128, :], xo.v(), eng='pool')

    def stB(i):
        xo = xm[i % 2]
        hb_ = h2b[i % 2]
        p.act(h2f.v(), xo.v(), AF.Square, accum=ss2[:, i:i + 1])
        p.rsqrt(ss2[:, i:i + 1], ss2[:, i:i + 1], 1.0 / D, EPS)
        p.ts(h2f.v(), xo.v(), ss2[:, i:i + 1], ALU.mult)
        p.tt(h2f.v(), h2f.v(), n2w_s.v(), ALU.mult)
        p.copy(hb_.v(), h2f.v(), eng='act')
        p.dma(h2_d[i * 128:(i + 1) * 128, :], hb_.v(), eng='pool')

    def stC(i):
        for q in range(4):
            bk = bank()
            for j in range(4):
                kc = q * 4 + j
                p.tr(bk[:, j * 128:(j + 1) * 128], h2f[:, kc * 128:(kc + 1) * 128], ident_f.v())
            p.copy(h2T[:, q * 4:(q + 1) * 4, :], bk.v().re("p (j t) -> p j t", j=4), eng='act' if q % 2 else 'dve')
        bk = bank()
        for kc in range(16):
            p.mm(bk[:, 0:16], h2T[:, kc, :], wr[:, kc, :], start=(kc == 0), stop=(kc == 15))
        p.red(mx[:, i:i + 1], bk[:, 0:16], ALU.max)
        p.ts(mx[:, i:i + 1], mx[:, i:i + 1], -1.0, ALU.mult)
        p.act(aff[:, i, :], bk[:, 0:16], AF.Exp, bias=mx[:, i:i + 1], accum=sm[:, i:i + 1])
        p.recip(sm[:, i:i + 1], sm[:, i:i + 1])
        p.ts(aff[:, i, :], aff[:, i, :], sm[:, i:i + 1], ALU.mult)

    stA(0)
    for i in range(NT):
        if i + 1 < NT:
            stA(i + 1)
        stB(i)
        stC(i)
    p.pop()

    p.push()
    for i in range(NT):
        p.dma(mixT[:, i, :], h2_d[i * 128:(i + 1) * 128, :], eng='sp' if i % 2 else 'pool')
    affT = p.sb("affT", [16, T], F32)
    work = p.sb("work", [16, T], F32)
    maskT = p.sb("maskT", [16, T], F32)
    onesT = p.sb("onesT", [16, T], F32)
    m8 = p.sb("m8", [16, 8], F32)
    p.memset(onesT.v(), 1.0, eng='pool')
    for q in range(4):
        bk = bank()
        for j in range(4):
            i = q * 4 + j
            p.tr(bk[0:16, j * 128:(j + 1) * 128], aff[:, i, :], ident_f.v())
        p.copy(affT[:, q * 512:(q + 1) * 512], bk[0:16, :], eng='dve')
    p.copy(work.v(), affT.v(), eng='dve')
    for r_ in range(32):
        p.op('dve', lambda e: e.max(out=m8.h[:], in_=work.h[:]), [work.v()], [m8.v()])
        if r_ < 31:
            p.op('dve', lambda e: e.match_replace(out=work.h[:], in_to_replace=m8.h[:], in_values=work.h[:],
                                                  imm_value=-1.0), [work.v(), m8.v()], [work.v()])
    p.ts(maskT.v(), affT.v(), m8[:, 7:8], ALU.is_ge)
    p.op('dve', lambda e: e.tensor_tensor_scan(work.h[:], onesT.h[:], maskT.h[:], 0.0, ALU.mult, ALU.add),
         [onesT.v(), maskT.v()], [work.v()])
    p.tt(work.v(), work.v(), maskT.v(), ALU.mult)
    bk = bank()
    for i in range(16):
        p.tr(bk[:, i * 16:(i + 1) * 16], work[:, i * 128:(i + 1) * 128], ident_f[0:16, 0:16])
    p.copy(valT.v(), bk[:, 0:256].re("p (i e) -> p i e", i=16), eng='dve')
    p.pop()

    p.push()
    h2s = mixT
    oh = p.sb("oh", [128, 16, 256], BF16)
    gm = p.sb("gm", [128, 16, 5], BF16)
    for i in range(16):
        p.memset(gm[:, i, 3:4], float(i))
        p.copy(gm[:, i, 4:5], iop_f.v())
    g1 = p.sb("g1", [128, 16], F32)
    g2 = p.sb("g2", [128, 16], F32)
    gb = p.sb("gb", [128, 16], BF16)
    gate = p.sb("gate", [128, 2], F32)
    g3 = p.sb("g3", [128, 2, 5], F32)
    xeT = p.sb("xeT", [128, 16, 256], BF16)
    hidT = oh
    ye = p.sb("ye", [128, 2, D], BF16)
    wb = [p.sb("ewb%d" % i, [128, 16, 512], BF16) for i in range(5)]
    sg = [p.sb("sg%d" % i, [128, 256], F32) for i in range(2)]
    wc = [0]

    def wload(src, e, cb):
        k = wc[0]
        wc[0] += 1
        b = wb[k % 5]
        p.dma(b.v(), src[e][:, cb * 512:(cb + 1) * 512].re("(k p) c -> p k c", p=128), eng='pool')
        return b

    iota_bc = iota1.v().re("p (o s) -> p o s", o=1).bc([128, 16, 256])
    for e in range(NE):
        p.tt(oh.v(), iota_bc, valT[:, :, e:e + 1].bc([128, 16, 256]), ALU.is_equal)
        p.copy(gb.v(), aff[:, :, e])
        p.copy(gm[:, :, 0], gb.v())
        p.tt(g1.v(), aff[:, :, e], gb.v(), ALU.subtract)
        p.copy(gb.v(), g1.v())
        p.copy(gm[:, :, 1], gb.v())
        p.tt(g2.v(), g1.v(), gb.v(), ALU.subtract)
        p.copy(gm[:, :, 2], g2.v())
        bk = bank()
        for sh in range(2):
            for i in range(16):
                p.mm(bk[:, sh * 8:sh * 8 + 5], oh[:, i, sh * 128:(sh + 1) * 128], gm[:, i, :],
                     start=(i == 0), stop=(i == 15))
        p.copy(g3.v(), bk[:, 0:16].re("p (a c) -> p a c", a=2)[:, :, 0:5])
        p.red(gate.v(), g3[:, :, 0:3], ALU.add)
        p.stt(idxs[:, 2 * e:2 * e + 2], g3[:, :, 3], 128.0, g3[:, :, 4], ALU.mult, ALU.add)
        for dq in range(8):
            bk = bank()
            for j in range(2):
                dc = dq * 2 + j
                for i in range(16):
                    p.mm(bk[:, j * 256:(j + 1) * 256], h2s[:, i, dc * 128:(dc + 1) * 128], oh[:, i, :],
                         start=(i == 0), stop=(i == 15))
            p.copy(xeT[:, dq * 2:dq * 2 + 2, :], bk.v().re("p (j s) -> p j s", j=2), eng='act' if dq % 2 else 'dve')
        for fb in range(4):
            wg = wload(eg_d, e, fb)
            wu = wload(eu_d, e, fb)
            for fc2 in range(4):
                fc = fb * 4 + fc2
                bg = bank()
                for dc in range(16):
                    p.mm(bg[:, 0:256], wg[:, dc, fc2 * 128:(fc2 + 1) * 128], xeT[:, dc, :],
                         start=(dc == 0), stop=(dc == 15))
                bu = bank()
                for dc in range(16):
                    p.mm(bu[:, 0:256], wu[:, dc, fc2 * 128:(fc2 + 1) * 128], xeT[:, dc, :],
                         start=(dc == 0), stop=(dc == 15))
                s_ = sg[fc % 2]
                p.act(s_.v(), bg[:, 0:256], AF.Silu)
                p.tt(hidT[:, fc, :], s_.v(), bu[:, 0:256], ALU.mult)
        for db in range(4):
            wd = wload(ed_d, e, db)
            for sh in range(2):
                bk = bank()
                for fc in range(16):
                    p.mm(bk.v(), hidT[:, fc, sh * 128:(sh + 1) * 128], wd[:, fc, :],
                         start=(fc == 0), stop=(fc == 15))
                p.ts(ye[:, sh, db * 512:(db + 1) * 512], bk.v(), gate[:, sh:sh + 1], ALU.mult)
        p.dma(ye_d[e].re("s p d -> p s d"), ye.v(), eng='sp')
    p.pop()

    p.push()
    yeh = p.sb("yeh", [128, 2 * NE, 1024], BF16)
    idm = p.sb("idm", [128, 32], F32)
    GT = p.sb("GT", [128, 32, 128], BF16)
    xr = [p.sb("xr%d" % i, [128, 1024], F32) for i in range(2)]
    iota_bc2 = iota1.v().re("p (o s) -> p o s", o=1).bc([128, 16, 256])
    for half in range(2):
        hs = slice(half * 1024, (half + 1) * 1024)
        for e in range(NE):
            p.dma(yeh[:, 2 * e:2 * e + 2, :], ye_d[e][:, :, hs].re("s p d -> p s d"), eng='sp' if e % 2 else 'pool')
        for i in range(NT):
            p.ts(idm.v(), idxs.v(), float(1 - 128 * i), ALU.add)
            p.tt(GT.v(), iota1[:, 0:128].re("p (o t) -> p o t", o=1).bc([128, 32, 128]),
                 idm.v().re("p (k o) -> p k o", o=1).bc([128, 32, 128]), ALU.is_equal)
            xa = xr[i % 2]
            p.dma(xa.v(), xmid_d[i * 128:(i + 1) * 128, hs], eng='sp')
            for db in range(2):
                bk = bank()
                for k in range(2 * NE):
                    p.mm(bk.v(), GT[:, k, :], yeh[:, k, db * 512:(db + 1) * 512], start=(k == 0), stop=(k == 2 * NE - 1))
                p.tt(xa[:, db * 512:(db + 1) * 512], xa[:, db * 512:(db + 1) * 512], bk.v(), ALU.add)
            p.dma(xmid_d[i * 128:(i + 1) * 128, hs], xa.v(), eng='pool')
    p.pop()

    p.push()
    nfw_s = p.sb("nfw_s", [128, D], F32)
    p.dma(nfw_s.v(), nfw[0:1, :].bc([128, D]))
    xf = [p.sb("xf%d" % i, [128, D], F32) for i in range(2)]
    junk2 = p.sb("junk2", [128, D], F32)
    ss3 = p.sb("ss3", [128, NT], F32)
    for i in range(NT):
        xo = xf[i % 2]
        p.dma(xo.v(), xmid_d[i * 128:(i + 1) * 128, :], eng='sp')
        p.act(junk2.v(), xo.v(), AF.Square, accum=ss3[:, i:i + 1])
        p.rsqrt(ss3[:, i:i + 1], ss3[:, i:i + 1], 1.0 / D, EPS)
        p.ts(xo.v(), xo.v(), ss3[:, i:i + 1], ALU.mult)
        p.tt(xo.v(), xo.v(), nfw_s.v(), ALU.mult)
        p.dma(out[i * 128:(i + 1) * 128, :], xo.v(), eng='pool')
    p.dma(scr2.v(), projT[0:1, 0:16])
    p.finish([out.v()], scr.v(), scr2.v())
    p.pop()
```

```python
import numpy as np
import ml_dtypes
from contextlib import ExitStack
import concourse.bass as bass
import concourse.mybir as mybir
from concourse.bass_utils import run_bass_kernel_spmd

F32 = mybir.dt.float32
BF16 = mybir.dt.bfloat16
I32 = mybir.dt.int32
ALU = mybir.AluOpType
AF = mybir.ActivationFunctionType
AX = mybir.AxisListType
ENG = ['pe', 'act', 'dve', 'pool', 'sp']
NDS = 24


class V:
    def __init__(s, tile, ap):
        s.tile = tile
        s.ap = ap

    def __getitem__(s, k):
        return V(s.tile, s.ap[k])

    def re(s, pat, **kw):
        return V(s.tile, s.ap.rearrange(pat, **kw))

    def bc(s, shape):
        return V(s.tile, s.ap.to_broadcast(shape))

    def bitcast(s, dt):
        return V(s.tile, s.ap.bitcast(dt))


class Tile:
    def __init__(s, h, name):
        s.h = h
        s.name = name
        s.w = None
        s.r = {}

    def __getitem__(s, k):
        return V(s, s.h[k])

    def v(s):
        return V(s, s.h[:])


class Prog:
    def __init__(s, nc, es):
        s.nc = nc
        s.es = es
        s.q = {e: [] for e in ENG}
        s.cnt = {e: 0 for e in ENG}
        s.sems = {e: es.enter_context(nc.semaphore("s_" + e)) for e in ENG}
        s.dsems = [es.enter_context(nc.semaphore("d%d" % i)) for i in range(NDS)]
        s.dcnt = [0] * NDS
        s.dnext = 0
        s.seen = {e: {} for e in ENG}
        s.n = 0

    def semof(s, k):
        return s.dsems[k[1]] if isinstance(k, tuple) else s.sems[k]

    def sb(s, name, shape, dt):
        return Tile(s.es.enter_context(s.nc.sbuf_tensor(name, list(shape), dt)), name)

    def ps(s, name, shape, dt):
        return Tile(s.es.enter_context(s.nc.psum_tensor(name, list(shape), dt)), name)

    def dram(s, name, shape, dt, kind="Internal"):
        return Tile(s.nc.dram_tensor(name, list(shape), dt, kind=kind), name)

    def op(s, eng, fn, reads, writes, dma=False, acc=False):
        waits = {}

        def need(k):
            if k is None:
                return
            waits[k[0]] = max(waits.get(k[0], 0), k[1])

        for v in reads:
            need(v.tile.w)
        for v in writes:
            t = v.tile
            if not (acc and t.w is not None and t.w[0] == 'pe'):
                need(t.w)
            for k, val in t.r.items():
                need((k, val))
        if dma:
            i = s.dnext
            s.dnext = (i + 1) % NDS
            key = ('d', i)
            need((key, s.dcnt[i]))
            s.dcnt[i] += 16
            val = s.dcnt[i]
            inc = 16
        else:
            key = eng
            s.cnt[eng] += 1
            val = s.cnt[eng]
            inc = 1
        wl = []
        for k, v in waits.items():
            if v <= 0 or s.seen[eng].get(k, 0) >= v:
                continue
            s.seen[eng][k] = v
            wl.append((k, v))
        s.q[eng].append((wl, fn, key, inc))
        s.n += 1
        for v in reads:
            v.tile.r[key] = max(v.tile.r.get(key, 0), val)
        for v in writes:
            v.tile.w = (key, val)
            v.tile.r = {}

    def dma(s, out, in_, eng='sp', **kw):
        s.op(eng, lambda e: e.dma_start(out=out.ap, in_=in_.ap, **kw), [in_], [out], dma=True)

    def mm(s, out, lhsT, rhs, start=True, stop=True):
        s.op('pe', lambda e: e.matmul(out.ap, lhsT.ap, rhs.ap, start=start, stop=stop),
             [lhsT, rhs], [out], acc=not start)

    def tr(s, out, in_, ident):
        s.op('pe', lambda e: e.transpose(out.ap, in_.ap, ident.ap), [in_, ident], [out])

    def act(s, out, in_, func, bias=None, scale=None, accum=None):
        kw = {}
        rd = [in_]
        wr = [out]
        if bias is not None:
            if isinstance(bias, V):
                kw['bias'] = bias.ap
                rd.append(bias)
            else:
                kw['bias'] = bias
        if scale is not None:
            if isinstance(scale, V):
                kw['scale'] = scale.ap
                rd.append(scale)
            else:
                kw['scale'] = scale
        if accum is not None:
            kw['accum_out'] = accum.ap
            wr.append(accum)
        s.op('act', lambda e: e.activation(out.ap, in_.ap, func, **kw), rd, wr)

    def tt(s, out, a, b, op, eng='dve'):
        s.op(eng, lambda e: e.tensor_tensor(out.ap, a.ap, b.ap, op), [a, b], [out])

    def ts(s, out, a, s1, op0, s2=None, op1=None, eng='dve', accum=None):
        rd = [a]
        wr = [out]
        a1 = s1
        a2 = s2
        if isinstance(s1, V):
            rd.append(s1)
            a1 = s1.ap
        if isinstance(s2, V):
            rd.append(s2)
            a2 = s2.ap
        kw = {}
        if op1 is not None:
            kw['op1'] = op1
        if accum is not None:
            kw['accum_out'] = accum.ap
            wr.append(accum)
        s.op(eng, lambda e: e.tensor_scalar(out.ap, a.ap, a1, a2, op0, **kw), rd, wr)

    def stt(s, out, a, sc, b, op0, op1, eng='dve'):
        rd = [a, b]
        a1 = sc
        if isinstance(sc, V):
            rd.append(sc)
            a1 = sc.ap
        s.op(eng, lambda e: e.scalar_tensor_tensor(out.ap, a.ap, a1, b.ap, op0, op1), rd, [out])

    def copy(s, out, in_, eng='dve'):
        if eng == 'act':
            s.op('act', lambda e: e.copy(out.ap, in_.ap), [in_], [out])
        else:
            s.op(eng, lambda e: e.tensor_copy(out.ap, in_.ap), [in_], [out])

    def memset(s, out, val, eng='dve'):
        s.op(eng, lambda e: e.memset(out.ap, val), [], [out])

    def red(s, out, in_, op, axis=None, eng='dve'):
        ax = AX.X if axis is None else axis
        s.op(eng, lambda e: e.tensor_reduce(out.ap, in_.ap, ax, op), [in_], [out])

    def recip(s, out, in_):
        s.op('dve', lambda e: e.reciprocal(out.ap, in_.ap), [in_], [out])

    def rsqrt(s, out, in_, mul, add):
        s.ts(out, in_, mul, ALU.mult, add, ALU.add)
        s.act(out, out, AF.Sqrt)
        s.recip(out, out)

    def push(s):
        s._outer = s.es
        s._inner = ExitStack()
        s.es = s._inner

    def pop(s):
        targets = [(e, s.cnt[e]) for e in ENG] + [(('d', i), s.dcnt[i]) for i in range(NDS)]
        for e in ENG:
            wl = []
            for k, v in targets:
                if v <= 0 or s.seen[e].get(k, 0) >= v:
                    continue
                s.seen[e][k] = v
                wl.append((k, v))
            s.q[e].append((wl, None, None, 0))
        s.emit()
        s._inner.close()
        s.es = s._outer

    def coll(s, kind, op, ins, outs, groups):
        s.op('pool', lambda e: e.collective_compute(kind, op, replica_groups=groups,
                                                    ins=[i.ap for i in ins], outs=[o.ap for o in outs]),
             ins, outs, dma=True)

    def finish(s, outs, scratch_dst, scratch_src):
        s.op('sp', lambda e: e.dma_start(out=scratch_dst.ap, in_=scratch_src.ap), list(outs) + [scratch_src],
             [scratch_dst], dma=True)
        k, val = scratch_dst.tile.w
        s.q['sp'].append(([(k, val)], None, None, 0))

    def emit(s):
        nc = s.nc
        qs = s.q
        s.q = {e: [] for e in ENG}
        with nc.Block() as block:
            s._emit_block(block, qs)

    def _emit_block(s, block, qs):
        waited = set()
        for e in ENG:
            for wl, fn, key, inc in qs[e]:
                for k, v in wl:
                    waited.add((k, v))
        if not hasattr(s, 'sig'):
            s.sig = {e: 0 for e in ENG}
            s.pos = {e: 0 for e in ENG}
            s.remap = {e: {0: 0} for e in ENG}
        plan = {}
        for e in ENG:
            pos = s.pos[e]
            sig = s.sig[e]
            flags = []
            for wl, fn, key, inc in qs[e]:
                if fn is None or isinstance(key, tuple):
                    flags.append(False)
                    continue
                pos += 1
                if (e, pos) in waited:
                    sig += 1
                    s.remap[e][pos] = sig
                    flags.append(True)
                else:
                    flags.append(False)
            s.pos[e] = pos
            s.sig[e] = sig
            plan[e] = flags

        def mk(e):
            def body(eng):
                for (wl, fn, key, inc), flag in zip(qs[e], plan[e]):
                    for k, v in wl:
                        if isinstance(k, tuple):
                            eng.wait_ge(s.semof(k), v)
                        else:
                            eng.wait_ge(s.semof(k), s.remap[k][v])
                    if fn is None:
                        continue
                    ins = fn(eng)
                    if isinstance(key, tuple):
                        ins.then_inc(s.semof(key), inc)
                    elif flag:
                        ins.then_inc(s.semof(key), 1)
            return body

        block.tensor(mk('pe'))
        block.scalar(mk('act'))
        block.vector(mk('dve'))
        block.gpsimd(mk('pool'))
        block.sync(mk('sp'))
D = 2048
T = 2048
NT = 16
NCOL = 6432
NCH = 51
EPS = 1e-6


def consts_np():
    c = {}
    c['ident_f'] = np.eye(128, dtype=np.float32)
    rho = np.arange(128)
    hh = rho // 64
    ii = rho % 64
    same = (hh[:, None] == hh[None, :])
    SL = (same & (ii[None, :] < ii[:, None])).astype(np.float32)
    IL = (same & (ii[None, :] <= ii[:, None])).astype(np.float32)
    masks = np.stack([SL, SL.T, IL, IL.T], 0)
    c['masks'] = np.ascontiguousarray(np.tile(masks[:, :, None, :], (1, 1, 4, 1)).reshape(4, 128, 512))
    rm = np.ones((128, T), np.float32)
    rm[:, ::64] = 0.0
    c['resetmask'] = rm
    c['iota1'] = np.tile(np.arange(1, 257, dtype=np.float32)[None, :], (128, 1))
    invf = np.zeros((128, 1), np.float32)
    PT = np.zeros((128, 128), np.float32)
    for cc in range(2):
        for j in range(8):
            f = 500000.0 ** (-(2.0 * j) / 16.0)
            p1 = cc * 64 + j
            p2 = cc * 64 + 8 + j
            invf[p1, 0] = f
            invf[p2, 0] = f
            PT[p2, p1] = -1.0
            PT[p1, p2] = 1.0
    c['invf'] = invf
    c['PT'] = PT
    c['blockones'] = same.astype(np.float32)
    sel = np.zeros((128, 64), np.float32)
    sel[rho, ii] = 1.0
    c['sel'] = sel
    c['iota_p'] = np.arange(128, dtype=np.float32).reshape(128, 1)
    return c


def build(stage=99):
    nc = bass.Bass("TRN2", target_bir_lowering=False)
    es = ExitStack()
    dbg = {}
    with es:
        p = Prog(nc, es)
        IN = lambda n, s, dt=F32: p.dram(n, s, dt, kind="ExternalInput")
        x = IN("x", [T, D])
        pos = IN("pos", [1, T], I32)
        n1w = IN("n1w", [128, 16])
        w_in = IN("w_in", [D, NCOL])
        out = p.dram("out", [T, D], F32, kind="ExternalOutput")
        projT = p.dram("projT", [NCH * 128, T], F32, kind=("ExternalOutput" if stage == 1 else "Internal"))
        scr = p.dram("scr", [1, 16], F32)
        scr2 = p.dram("scr2", [1, 16], F32)
        cn = consts_np()
        C = {k: IN("c_" + k, list(v.shape)) for k, v in cn.items()}

        ident_f = p.sb("ident_f", [128, 128], F32)
        ident_b = p.sb("ident_b", [128, 128], BF16)
        p.dma(ident_f.v(), C['ident_f'].v())
        p.copy(ident_b.v(), ident_f.v())
        n1w_s = p.sb("n1w_s", [128, 16], F32)
        p.dma(n1w_s.v(), n1w.v())

        PB = [p.ps("pb%d" % i, [128, 512], F32) for i in range(6)]
        PH = [p.ps("ph%d" % i, [128, 1024], BF16) for i in range(2)]
        st = {'pb': 0, 'ph': 0}

        def bank():
            st['pb'] = (st['pb'] + 1) % 6
            return PB[st['pb']]

        def hbank():
            st['ph'] = (st['ph'] + 1) % 2
            return PH[st['ph']]

        p.push()
        hT = p.sb("hT", [128, 16, T], BF16)
        xt = [p.sb("xt%d" % i, [128, D], F32) for i in range(2)]
        xn = [p.sb("xn%d" % i, [128, D], BF16) for i in range(2)]
        junk = p.sb("junk", [128, D], F32)
        ssq = p.sb("ssq", [128, NT], F32)
        rstd = p.sb("rstd", [128, NT], F32)
        for i in range(NT):
            a = xt[i % 2]
            b = xn[i % 2]
            p.dma(a.v(), x[i * 128:(i + 1) * 128, :], eng='sp' if i % 2 == 0 else 'pool')
            p.act(junk.v(), a.v(), AF.Square, accum=ssq[:, i:i + 1])
            p.rsqrt(rstd[:, i:i + 1], ssq[:, i:i + 1], 1.0 / D, EPS)
            p.ts(b.v(), a.v(), rstd[:, i:i + 1], ALU.mult)
            for g in range(2):
                hb = hbank()
                for j in range(8):
                    kc = g * 8 + j
                    p.tr(hb[:, j * 128:(j + 1) * 128], b[:, kc * 128:(kc + 1) * 128], ident_b.v())
                src = hb.v().re("p (j t) -> p j t", j=8)
                dst = hT[:, g * 8:(g + 1) * 8, i * 128:(i + 1) * 128]
                if g == 0:
                    p.copy(dst, src, eng='act')
                else:
                    p.copy(dst, src, eng='dve')

        wf = [p.sb("wf%d" % i, [128, 16, 128], F32) for i in range(2)]
        wb = [p.sb("wb%d" % i, [128, 16, 128], BF16) for i in range(2)]
        prow = [p.sb("prow%d" % i, [128, T], F32) for i in range(2)]
        n1w_bc = n1w_s.v().re("p (k o) -> p k o", o=1)
        for ch in range(NCH):
            c0 = ch * 128
            m = min(128, NCOL - c0)
            f = wf[ch % 2]
            b = wb[ch % 2]
            pr = prow[ch % 2]
            p.dma(f[:, :, 0:m], w_in[:, c0:c0 + m].re("(k p) c -> p k c", p=128), eng='sp')
            p.tt(b[:, :, 0:m], f[:, :, 0:m], n1w_bc.bc([128, 16, m]), ALU.mult, eng='pool')
            for tb in range(4):
                bk = bank()
                for kc in range(16):
                    p.mm(bk[0:m, :], b[:, kc, 0:m], hT[:, kc, tb * 512:(tb + 1) * 512],
                         start=(kc == 0), stop=(kc == 15))
                p.copy(pr[0:m, tb * 512:(tb + 1) * 512], bk[0:m, :], eng='act' if tb % 2 == 0 else 'dve')
            p.dma(projT[c0:c0 + m, :], pr[0:m, :], eng='pool')
        if stage == 1:
            p.dma(scr2.v(), projT[0:1, 0:16])
            p.finish([projT.v()], scr.v(), scr2.v())
            p.pop()
            return nc, cn
        p.pop()

        mix_d = p.dram("mix_d", [128, 16, T], BF16)
        lam4 = IN("lam4", [1, 256])
        sublnw = IN("sublnw", [1, 128])
        p.push()
        PTf = p.sb("PTf", [128, 128], F32)
        PTb = p.sb("PTb", [128, 128], BF16)
        p.dma(PTf.v(), C['PT'].v())
        p.copy(PTb.v(), PTf.v())
        invf = p.sb("invf", [128, 1], F32)
        p.dma(invf.v(), C['invf'].v())
        posi = p.sb("posi", [128, T], I32)
        p.dma(posi.v(), pos[0:1, :].bc([128, T]))
        ang = p.sb("ang", [128, T], F32)
        cosF = p.sb("cosF", [128, T], F32)
        sinF = p.sb("sinF", [128, T], F32)
        p.copy(ang.v(), posi.v())
        p.ts(ang.v(), ang.v(), invf[:, 0:1], ALU.mult)
        angi = posi
        def rangered(dst, src):
            p.ts(t1x.v(), src, 1.0 / (2 * np.pi), ALU.mult)
            p.copy(angi.v(), t1x.v())
            p.copy(t1x.v(), angi.v())
            p.stt(dst, t1x.v(), -2 * np.pi, src, ALU.mult, ALU.add)
            p.ts(t1x.v(), dst, np.pi, ALU.is_gt, 2 * np.pi, ALU.mult)
            p.tt(dst, dst, t1x.v(), ALU.subtract)
            p.ts(t1x.v(), dst, -np.pi, ALU.is_lt, 2 * np.pi, ALU.mult)
            p.tt(dst, dst, t1x.v(), ALU.add)
        t1x = p.sb("t1x", [128, T], F32)
        rangered(sinF.v(), ang.v())
        p.act(sinF.v(), sinF.v(), AF.Sin)
        p.ts(ang.v(), ang.v(), np.pi / 2, ALU.add)
        rangered(cosF.v(), ang.v())
        p.act(cosF.v(), cosF.v(), AF.Sin)
        l4 = p.sb("l4", [128, 256], F32)
        p.dma(l4.v(), lam4[0:1, :].bc([128, 256]))
        lprod = p.sb("lprod", [128, 2, 64], F32)
        p.tt(lprod.v(), l4.v().re("p (a b d) -> p a b d", a=2, b=2)[:, :, 0, :],
             l4.v().re("p (a b d) -> p a b d", a=2, b=2)[:, :, 1, :], ALU.mult)
        lsum = p.sb("lsum", [128, 2], F32)
        p.red(lsum.v(), lprod.v(), ALU.add)
        p.act(lsum.v(), lsum.v(), AF.Exp)
        lam = p.sb("lam", [128, 1], F32)
        p.tt(lam.v(), lsum[:, 0:1], lsum[:, 1:2], ALU.subtract)
        p.ts(lam.v(), lam.v(), 0.2, ALU.add)
        slw = p.sb("slw", [128, 128], F32)
        p.dma(slw.v(), sublnw[0:1, :].bc([128, 128]))
        p.ts(slw.v(), slw.v(), 0.8, ALU.mult)

        qf = p.sb("qf", [128, T], F32)
        kf = p.sb("kf", [128, T], F32)
        vf = p.sb("vf", [128, T], F32)
        xq16 = p.sb("xq16", [128, T], BF16)
        xk16 = p.sb("xk16", [128, T], BF16)
        xv16 = p.sb("xv16", [128, T], BF16)
        t1 = p.sb("t1", [128, T], F32)
        QR = [p.sb("qr%d" % i, [128, T], BF16) for i in range(2)]
        KR = [p.sb("kr%d" % i, [128, T], BF16) for i in range(2)]
        VA = [p.sb("vaug%d" % i, [128, 16, 129], BF16) for i in range(2)]
        for i in range(2):
            p.memset(VA[i].v(), 1.0)
        pT = [p.sb("pT%d" % i, [128, 512], BF16) for i in range(3)]
        oacc = [p.sb("oacc%d" % i, [128, 4, 129], F32) for i in range(2)]
        att = p.sb("att", [128, 4, 128], F32)
        att1 = p.sb("att1", [128, 4, 128], F32)
        attb = p.sb("attb", [128, 4, 128], BF16)
        rr = p.sb("rr", [128, 2, 4], F32)
        ssa = p.sb("ssa", [128, 4], F32)
        mst = [p.sb("mst%d" % i, [128, 512], BF16) for i in range(2)]
        cnt = 0

        def prep_load(h):
            p.dma(qf.v(), projT[h * 128:(h + 1) * 128, :], eng='sp')
            p.dma(kf.v(), projT[1024 + h * 128:1024 + (h + 1) * 128, :], eng='sp')
            p.dma(vf.v(), projT[2048 + h * 128:2048 + (h + 1) * 128, :], eng='sp')
            p.copy(xq16.v(), qf.v(), eng='pool')
            p.copy(xk16.v(), kf.v(), eng='pool')
            p.copy(xv16.v(), vf.v(), eng='pool')
            p.tt(qf.v(), qf.v(), cosF.v(), ALU.mult, eng='pool')
            p.tt(kf.v(), kf.v(), cosF.v(), ALU.mult, eng='pool')

        def prep_pe(h):
            qr, kr, vaug = QR[h % 2], KR[h % 2], VA[h % 2]
            for (x16, src, dst) in ((xq16, qf, qr), (xk16, kf, kr)):
                for tb in range(4):
                    bk = PB[4 + tb % 2]
                    sl = slice(tb * 512, (tb + 1) * 512)
                    p.mm(bk.v(), PTb.v(), x16[:, sl])
                    p.tt(t1[:, sl], bk.v(), sinF[:, sl], ALU.mult)
                p.tt(dst.v(), src.v(), t1.v(), ALU.add)
            for g in range(2):
                hb = hbank()
                for j in range(8):
                    kt = g * 8 + j
                    p.tr(hb[:, j * 128:(j + 1) * 128], xv16[:, kt * 128:(kt + 1) * 128], ident_b.v())
                p.copy(vaug[:, g * 8:(g + 1) * 8, 0:128], hb.v().re("p (j e) -> p j e", j=8), eng='dve')

        prep_load(0)
        prep_pe(0)
        for h in range(8):
            qr, kr, vaug = QR[h % 2], KR[h % 2], VA[h % 2]
            for qb in range(4):
                if h + 1 < 8 and qb == 0:
                    prep_load(h + 1)
                if h + 1 < 8 and qb == 2:
                    prep_pe(h + 1)
                qsl = slice(qb * 512, (qb + 1) * 512)
                its = [(c, kt) for c in range(2) for kt in range(16)]

                def qk(i):
                    c, kt = its[i]
                    ps_ = slice(64 * c, 64 * c + 64)
                    p.mm(PB[4 + i % 2].v(), kr[ps_, kt * 128:(kt + 1) * 128], qr[ps_, qsl])
                qk(0)
                for i, (c, kt) in enumerate(its):
                    if i + 1 < len(its):
                        qk(i + 1)
                    sbk = PB[4 + i % 2]
                    pt_ = pT[i % 3]
                    p.act(pt_.v(), sbk.v(), AF.Exp, scale=0.125)
                    for qs in range(4):
                        p.mm(PB[qs][:, 0:129], pt_[:, qs * 128:(qs + 1) * 128], vaug[:, kt, :],
                             start=(kt == 0), stop=(kt == 15))
                    if kt == 15:
                        for qs in range(4):
                            p.copy(oacc[c][:, qs, :], PB[qs][:, 0:129], eng='dve')
                p.recip(rr[:, 0, :], oacc[0][:, :, 128])
                p.recip(rr[:, 1, :], oacc[1][:, :, 128])
                p.ts(rr[:, 1, :], rr[:, 1, :], lam[:, 0:1], ALU.mult)
                p.tt(att.v(), oacc[0][:, :, 0:128], rr[:, 0, :].re("p (q o) -> p q o", o=1).bc([128, 4, 128]), ALU.mult)
                p.tt(att1.v(), oacc[1][:, :, 0:128], rr[:, 1, :].re("p (q o) -> p q o", o=1).bc([128, 4, 128]), ALU.mult, eng='pool')
                p.tt(att.v(), att.v(), att1.v(), ALU.subtract)
                p.tt(att1.v(), att.v(), att.v(), ALU.mult, eng='pool')
                p.red(ssa.v(), att1.v(), ALU.add)
                p.rsqrt(ssa.v(), ssa.v(), 1.0 / 128, 1e-5)
                p.tt(att.v(), att.v(), ssa.v().re("p (q o) -> p q o", o=1).bc([128, 4, 128]), ALU.mult)
                p.tt(attb.v(), att.v(), slw.v().re("p (o e) -> p o e", o=1).bc([128, 4, 128]), ALU.mult)
                hb = hbank()
                for qs in range(4):
                    p.tr(hb[:, qs * 128:(qs + 1) * 128], attb[:, qs, :], ident_b.v())
                ms_ = mst[(h * 4 + qb) % 2]
                p.copy(ms_.v(), hb[:, 0:512], eng='act')
                p.dma(mix_d[:, h, qsl], ms_.v(), eng='pool')
        p.pop()

        NHP = 8 if stage != 3 else 1
        rwkv_stage(p, locals())
        tail_stage(p, locals())
    return nc, cn


def moe_hook(p, L, i):
    pass


def rwkv_inputs(inp):
    m = {}

    def cols28(mu):
        o = np.zeros((128, 28), np.float32)
        o[:, 0:24] = mu[0:3072].reshape(24, 128).T
        o[0:64, 24] = mu[3072:3136]
        o[64:128, 25] = mu[3136:3200]
        o[:, 26] = mu[3200:3328]
        o[0:32, 27] = mu[3328:3360]
        return o
    m["muP"] = cols28(inp['mu_prev'][0])
    m["muN"] = cols28(inp['mu_next'][0])
    m["w0r"] = np.ascontiguousarray(inp['w0'][0].reshape(2, 8, 128).transpose(2, 0, 1).reshape(128, 16))
    m["a0r"] = np.ascontiguousarray(inp['a0'][0].reshape(2, 8, 128).transpose(2, 0, 1).reshape(128, 16))
    m["kkr"] = np.ascontiguousarray(inp['k_k'][0].reshape(8, 128).T)
    m["kar"] = np.ascontiguousarray(inp['k_a'][0].reshape(8, 128).T)
    m["rkr"] = np.ascontiguousarray(inp['r_k'][0].reshape(8, 128).T)
    m["lnw_tok"] = np.ascontiguousarray(np.repeat(inp['ln_x_w'][0].reshape(8, 2, 1, 64), 64, axis=2).reshape(8, 128, 64))
    m["lnb_tok"] = np.ascontiguousarray(np.repeat(inp['ln_x_b'][0].reshape(8, 2, 1, 64), 64, axis=2).reshape(8, 128, 64))
    m["decay_up"] = np.ascontiguousarray(inp['decay_up'][0])
    m["iclr_up"] = np.ascontiguousarray(inp['iclr_up'][0])
    m["gate_up"] = np.ascontiguousarray(inp['gate_up'][0])
    return {k: v.astype(np.float32) for k, v in m.items()}


def kernel(**inputs):
    inp = {k: np.asarray(v) for k, v in inputs.items()}
    nc, cn = build(99)

    def core_inputs(b):
        m = {"x": np.ascontiguousarray(inp['x'][b]),
             "pos": np.ascontiguousarray(inp['positions'][b][None, :].astype(np.int32)),
             "n1w": np.ascontiguousarray(inp['norm1_w'][0].reshape(16, 128).T),
             "w_in": np.ascontiguousarray(inp['w_in'][0]),
             "lam4": np.concatenate([inp['lambda_q1'][0], inp['lambda_k1'][0], inp['lambda_q2'][0],
                                     inp['lambda_k2'][0]])[None, :].astype(np.float32),
             "sublnw": inp['subln_w'].astype(np.float32).reshape(1, 128),
             "w_out": np.ascontiguousarray(inp['w_out'][0]),
             "nfw": inp['norm_f_w'].astype(np.float32).reshape(1, D),
             "n2w": inp['norm2_w'].astype(np.float32).reshape(1, D),
             "w_router": w_router, "e_gate": e_gate, "e_up": e_up, "e_down": e_down}
        m.update(rw)
        for k, v in cn.items():
            m["c_" + k] = v
        return m

    rw = rwkv_inputs(inp)
    w_router = np.ascontiguousarray(inp['w_router'][0])
    e_gate = np.ascontiguousarray(inp['e_gate'][0])
    e_up = np.ascontiguousarray(inp['e_up'][0])
    e_down = np.ascontiguousarray(inp['e_down'][0])
    maps = [core_inputs(c // 2) for c in range(8)]
    res = run_bass_kernel_spmd(nc, maps, core_ids=list(range(8)))
    return np.stack([res.results[2 * b]["out"] for b in range(4)], 0).astype(np.float32)


def rwkv_stage(p, L):
    IN = L['IN']
    C = L['C']
    PB = L['PB']
    bank = L['bank']
    hbank = L['hbank']
    ident_b = L['ident_b']
    ident_f = L['ident_f']
    projT = L['projT']
    mix_d = L['mix_d']
    muP_d = IN("muP", [128, 28])
    muN_d = IN("muN", [128, 28])
    w0r_d = IN("w0r", [128, 16])
    a0r_d = IN("a0r", [128, 16])
    kkr_d = IN("kkr", [128, 8])
    kar_d = IN("kar", [128, 8])
    rkr_d = IN("rkr", [128, 8])
    lnw_d = IN("lnw_tok", [8, 128, 64])
    lnb_d = IN("lnb_tok", [8, 128, 64])
    dup_d = IN("decay_up", [2, 64, 1024])
    iup_d = IN("iclr_up", [2, 64, 1024])
    gup_d = IN("gate_up", [160, 1024])
    p.push()
    SEG = 512
    muP = p.sb("muP_s", [128, 28], F32)
    muN = p.sb("muN_s", [128, 28], F32)
    mu0 = p.sb("mu0_s", [128, 28], F32)
    p.dma(muP.v(), muP_d.v())
    p.dma(muN.v(), muN_d.v())
    p.tt(mu0.v(), muP.v(), muN.v(), ALU.add)
    p.ts(mu0.v(), mu0.v(), -1.0, ALU.mult, 1.0, ALU.add)
    w0r = p.sb("w0r_s", [128, 16], F32)
    a0r = p.sb("a0r_s", [128, 16], F32)
    kkr = p.sb("kkr_s", [128, 8], F32)
    kar = p.sb("kar_s", [128, 8], F32)
    rkr = p.sb("rkr_s", [128, 8], F32)
    for t_, d_ in ((w0r, w0r_d), (a0r, a0r_d), (kkr, kkr_d), (kar, kar_d), (rkr, rkr_d)):
        p.dma(t_.v(), d_.v())
    masks = p.sb("masks_s", [128, 4, 128], F32)
    p.dma(masks.v(), C['masks'].v().re("m p (n r) -> p m n r", n=4)[:, :, 0, :])
    rmask = p.sb("rmask", [128, SEG], F32)
    p.dma(rmask.v(), C['resetmask'][:, 0:SEG])
    bones_f = p.sb("bones_f", [128, 128], F32)
    bones = p.sb("bones", [128, 128], BF16)
    p.dma(bones_f.v(), C['blockones'].v())
    p.copy(bones.v(), bones_f.v())
    sel_f = p.sb("sel_f", [128, 64], F32)
    sel_b = p.sb("sel_b", [128, 64], BF16)
    p.dma(sel_f.v(), C['sel'].v())
    p.copy(sel_b.v(), sel_f.v())
    ones_b = p.sb("ones_b", [128, 1], BF16)
    p.memset(ones_b.v(), 1.0)
    stg = p.sb("lstg", [128, 512], F32)
    lwb = p.sb("lwb", [128, 2, 1024], BF16)
    gup0 = p.sb("gup0", [128, 1024], BF16)
    gup1 = p.sb("gup1", [32, 1024], BF16)
    for d_ in range(2):
        for hf in range(2):
            cs_ = slice(hf * 512, (hf + 1) * 512)
            p.dma(stg[0:64, :], dup_d[d_][:, cs_])
            p.dma(stg[64:128, :], iup_d[d_][:, cs_])
            p.copy(lwb[:, d_, cs_], stg.v())
    for hf in range(2):
        cs_ = slice(hf * 512, (hf + 1) * 512)
        p.dma(stg.v(), gup_d[0:128, cs_])
        p.copy(gup0[:, cs_], stg.v())
        p.dma(stg[0:32, :], gup_d[128:160, cs_])
        p.copy(gup1[:, cs_], stg[0:32, :])

    zs = [p.sb("zs%d" % i, [128, SEG + 2], F32) for i in range(2)]
    zcnt = [0]

    def shift_load(r0, m, mucol, seg, dst, p0=0):
        z = zs[zcnt[0] % 2]
        zcnt[0] += 1
        t0 = seg * SEG - 1
        lo = max(t0, 0)
        hi = min(seg * SEG + SEG + 1, T)
        ps_ = slice(p0, p0 + m)
        if seg == 0:
            p.memset(z[ps_, 0:1], 0.0, eng='pool')
        if hi - t0 < SEG + 2:
            p.memset(z[ps_, SEG + 1:SEG + 2], 0.0, eng='pool')
        p.dma(z[ps_, lo - t0:hi - t0], projT[r0:r0 + m, lo:hi], eng='sp')
        p.ts(dst, z[ps_, 1:SEG + 1], mu0[ps_, mucol:mucol + 1], ALU.mult)
        p.stt(dst, z[ps_, 0:SEG], muP[ps_, mucol:mucol + 1], dst, ALU.mult, ALU.add)
        p.stt(dst, z[ps_, 2:SEG + 2], muN[ps_, mucol:mucol + 1], dst, ALU.mult, ALU.add)

    lact = p.sb("lact", [128, T], BF16)
    sgd0 = p.sb("sgd0", [128, T], BF16)
    sgd1 = p.sb("sgd1", [32, T], BF16)
    TA_ = [p.sb("rw_ta%d" % d_, [128, SEG], F32) for d_ in range(2)]
    ltmp = TA_[0]
    for seg in range(4):
        sl = slice(seg * SEG, (seg + 1) * SEG)
        shift_load(6144, 64, 24, seg, ltmp[0:64, :])
        p.act(lact[0:64, sl], ltmp[0:64, :], AF.Tanh)
        shift_load(6208, 64, 25, seg, ltmp[64:128, :], p0=64)
        p.copy(lact[64:128, sl], ltmp[64:128, :], eng='dve')
        shift_load(6272, 128, 26, seg, ltmp[0:128, :])
        p.act(sgd0[:, sl], ltmp[0:128, :], AF.Sigmoid)
        shift_load(6400, 32, 27, seg, ltmp[0:32, :])
        p.act(sgd1[:, sl], ltmp[0:32, :], AF.Sigmoid)

    def f32t(name):
        return p.sb(name, [128, SEG], F32)
    F32S = [{n: f32t("rw_%s%d" % (n, d_)) for n in "zr zk zv kk logw aic kdir bq cf ci tb".split()} for d_ in range(2)]
    SQB = [p.sb("rw_sqb%d" % d_, [128, SEG], BF16) for d_ in range(2)]
    gam = [p.sb("rw_gam%d" % d_, [128, 8], F32) for d_ in range(2)]

    def etile(name):
        t_ = p.sb(name, [128, 8, 128], BF16)
        p.memset(t_.v(), 0.0, eng='pool')
        return t_
    ES = [{n: etile("rw_%s%d" % (n, d_)) for n in "kE bE khE bhE vE zE".split()} for d_ in range(2)]
    for d_ in range(2):
        t_ = p.sb("rw_arE%d" % d_, [128, 8, 256], BF16)
        p.memset(t_.v(), 0.0, eng='pool')
        ES[d_]['arE'] = t_
    CM = p.sb("rw_cmask", [128, 2, 256], F32)
    p.copy(CM[:, 0, 0:128], masks[:, 1, :], eng='pool')
    p.copy(CM[:, 0, 128:256], masks[:, 3, :], eng='pool')
    p.copy(CM[:, 1, 0:128], masks[:, 0, :], eng='pool')
    p.copy(CM[:, 1, 128:256], masks[:, 2, :], eng='pool')
    AW = [p.sb("rw_aW%d" % d_, [128, 8, 128], BF16) for d_ in range(2)]
    VW = [p.sb("rw_vW%d" % d_, [128, 8, 128], BF16) for d_ in range(2)]
    bW = [p.sb("rw_bW%d" % d_, [128, 8, 128], BF16) for d_ in range(2)]
    kW = [p.sb("rw_kW%d" % d_, [128, 8, 128], BF16) for d_ in range(2)]
    vst = [p.sb("rw_vst%d" % d_, [128, 8, 64], BF16) for d_ in range(2)]
    yacc = [p.sb("rw_yacc%d" % i, [128, 8, 64], F32) for i in range(4)]
    vst_all = [p.sb("rw_vstall%d" % i, [128, 8, 64], BF16) for i in range(4)]
    bsc = [p.sb("rw_bsc%d" % i, [128, 8], F32) for i in range(4)]
    Sm = [[p.sb("rw_Sm%d%d" % (d_, i), [128, 64], F32) for i in range(2)] for d_ in range(2)]
    Sb = [[p.sb("rw_Sb%d%d" % (d_, i), [128, 64], BF16) for i in range(2)] for d_ in range(2)]
    scnt = [0, 0]

    GS, NG = 2, 4

    def gt(name, n2=NG, w=128):
        return [[p.sb("rw_%s%d%d" % (name, d_, i), [128, GS, w], BF16) for i in range(n2)] for d_ in range(2)]
    SLb, YLb = [gt(n) for n in "SL YL".split()]
    LMb = gt("LM", w=256)
    KMb = gt("KM", w=256)
    TNb = gt("TN", w=64)
    Lb = [p.sb("rw_L%d" % i, [128, GS, 128], BF16) for i in range(NG)]
    TAb = [p.sb("rw_TA%d" % i, [128, GS, 128], BF16) for i in range(NG)]
    Pb = [[p.sb("rw_P%d%d" % (g_, i), [128, GS, 128], BF16) for i in range(2)] for g_ in range(NG)]
    PTb = [[p.sb("rw_PT%d%d" % (g_, i), [128, GS, 128], BF16) for i in range(2)] for g_ in range(NG)]
    TTb = [[p.sb("rw_TT%d%d" % (g_, i), [128, GS, 128], BF16) for i in range(2)] for g_ in range(NG)]
    Nb = [p.sb("rw_N%d" % i, [128, GS, 64], BF16) for i in range(NG)]
    owide = ES[0]['zE']
    zr, zk, zv = F32S[0]['zr'], F32S[0]['zk'], F32S[0]['zv']
    mst2 = [p.sb("rw_mst%d" % i, [128, SEG], BF16) for i in range(2)]
    fcnt = [0]
    fin_s = p.sb("rw_fs", [128, 8], F32)
    fin_r = p.sb("rw_fr", [128, 8], F32)
    lnw = p.sb("rw_lnw", [128, 64], F32)
    lnb = p.sb("rw_lnb", [128, 64], F32)
    gT = zv
    ev = [0]

    def evac_eng():
        ev[0] += 1
        return 'act' if ev[0] % 2 == 0 else 'dve'

    def c3(v):
        return v.re("p (c i) -> p c i", i=64)

    def prepA(hp, d, seg, first):
        zr, zk, zv, kk, logw, aic, kdir, bq, cf, ci, tb_ = [F32S[d][n] for n in "zr zk zv kk logw aic kdir bq cf ci tb".split()]
        ta = TA_[d]
        sqb = SQB[d]
        kE, bE, khE, bhE, vE, zE = [ES[d][n] for n in "kE bE khE bhE vE zE".split()]
        aE = ES[d]['arE'][:, :, 0:128]
        rE = ES[d]['arE'][:, :, 128:256]
        aW, vW = AW[d], VW[d]
        sl = slice(seg * SEG, (seg + 1) * SEG)
        cols = slice(hp * 128, (hp + 1) * 128)
        shift_load(3072 + hp * 128, 128, hp, seg, zr.v())
        yield
        shift_load(4096 + hp * 128, 128, 8 + hp, seg, zk.v())
        yield
        shift_load(5120 + hp * 128, 128, 16 + hp, seg, zv.v())
        yield
        bk = bank()
        p.mm(bk.v(), lwb[0:64, d, cols], lact[0:64, sl])
        p.act(logw.v(), bk.v(), AF.Sigmoid, bias=w0r[:, d * 8 + hp:d * 8 + hp + 1])
        p.ts(logw.v(), logw.v(), -0.6065306597126334, ALU.mult)
        yield
        bk = bank()
        p.mm(bk.v(), lwb[64:128, d, cols], lact[64:128, sl])
        p.act(aic.v(), bk.v(), AF.Sigmoid, bias=a0r[:, d * 8 + hp:d * 8 + hp + 1])
        yield
        p.ts(kk.v(), zk.v(), kkr[:, hp:hp + 1], ALU.mult)
        p.tt(sqb.v(), kk.v(), kk.v(), ALU.mult, eng='pool')
        yield
        bk = bank()
        p.mm(bk.v(), bones.v(), sqb.v())
        p.ts(ta.v(), bk.v(), 1e-24, ALU.max)
        p.act(ta.v(), ta.v(), AF.Sqrt)
        yield
        p.recip(ta.v(), ta.v())
        p.tt(kk.v(), kk.v(), ta.v(), ALU.mult, eng='pool')
        yield
        p.ts(ta.v(), aic.v(), -1.0, ALU.add, kar[:, hp:hp + 1], ALU.mult)
        p.stt(kdir.v(), ta.v(), 1.0, zk.v(), ALU.add, ALU.mult)
        yield
        p.tt(bq.v(), kk.v(), aic.v(), ALU.mult, eng='pool')
        p.tt(ta.v(), zr.v(), kdir.v(), ALU.mult, eng='pool')
        yield
        for h in range(2):
            ps_ = slice(64 * h, 64 * h + 64)
            p.ts(zE[ps_, :, 64 * h:64 * h + 64], c3(ta[ps_, :]), rkr[ps_, hp:hp + 1], ALU.mult)
        bk = bank()
        for c in range(8):
            p.mm(bk[:, c:c + 1], zE[:, c, :], ones_b.v())
        if first:
            p.copy(bsc[seg].v(), bk[:, 0:8], eng='act')
        else:
            p.tt(bsc[seg].v(), bsc[seg].v(), bk[:, 0:8], ALU.add)
        yield
        p.op('dve', lambda e: e.tensor_tensor_scan(cf.h[:], rmask.h[:], logw.h[:], 0.0, ALU.mult, ALU.add),
             [rmask.v(), logw.v()], [cf.v()])
        tot = c3(cf.v())[:, :, 63:64]
        if d == 0:
            cisrc = cf
        else:
            p.tt(ci.v(), logw.v(), cf.v(), ALU.subtract, eng='pool')
            p.tt(c3(ci.v()), c3(ci.v()), tot.bc([128, 8, 64]), ALU.add, eng='pool')
            cisrc = ci

        def wE(dst, a_, b_, neg=False):
            for h in range(2):
                ps_ = slice(64 * h, 64 * h + 64)
                o = dst[ps_, :, 64 * h:64 * h + 64]
                if neg:
                    p.stt(o, c3(a_[ps_, :]), -1.0, c3(b_[ps_, :]), ALU.mult, ALU.mult)
                else:
                    p.tt(o, c3(a_[ps_, :]), c3(b_[ps_, :]), ALU.mult, eng='pool' if h else 'dve')
        p.act(ta.v(), cisrc.v(), AF.Exp)
        wE(rE, zr, ta)
        yield
        p.act(ta.v(), cisrc.v(), AF.Exp, scale=-1.0)
        wE(kE, kdir, ta)
        yield
        wE(bE, bq, ta)
        yield
        p.tt(tb_.v(), cisrc.v(), logw.v(), ALU.subtract, eng='pool')
        p.act(tb_.v(), tb_.v(), AF.Exp)
        wE(aE, kk, tb_, neg=True)
        yield
        p.tt(c3(tb_.v()), tot.bc([128, 8, 64]), c3(cisrc.v()), ALU.subtract, eng='pool')
        p.act(tb_.v(), tb_.v(), AF.Exp)
        wE(khE, kdir, tb_)
        yield
        wE(bhE, bq, tb_)
        yield
        for h in range(2):
            ps_ = slice(64 * h, 64 * h + 64)
            p.copy(vE[ps_, :, 64 * h:64 * h + 64], c3(zv[ps_, :]), eng='pool')
        yield

    def prepB(d, seg):
        cf = F32S[d]['cf']
        khE, bhE, vE = [ES[d][n] for n in "khE bhE vE".split()]
        aE = ES[d]['arE'][:, :, 0:128]
        aW, vW = AW[d], VW[d]
        tot = c3(cf.v())[:, :, 63:64]
        p.act(gam[d].v().re("p (c o) -> p c o", o=1), tot, AF.Exp)
        for srcE, dstW in ((aE, aW), (bhE, bW[d]), (khE, kW[d]), (vE, vW)):
            hb = hbank()
            for c in range(8):
                p.tr(hb[:, c * 128:(c + 1) * 128], srcE[:, c, :], ident_b.v())
            p.copy(dstW.v(), hb.v().re("p (c r) -> p c r", c=8), eng=evac_eng())
        p.tt(vst[d].v(), vW[:, :, 0:64], vW[:, :, 64:128], ALU.add, eng='pool')
        if d == 0:
            p.copy(vst_all[seg].v(), vst[d].v(), eng='pool')

    def par_group(d, g, gi):
        kE, bE = ES[d]['kE'], ES[d]['bE']
        arE = ES[d]['arE']
        aE = arE[:, :, 0:128]
        rE = arE[:, :, 128:256]
        aW = AW[d]
        LM, KM = LMb[d][gi], KMb[d][gi]
        LT, MrbT = LM[:, :, 0:128], LM[:, :, 128:256]
        LakT, MrkT = KM[:, :, 0:128], KM[:, :, 128:256]

        def mm2(dst, lhsE):
            bk_ = bank()
            for n in range(GS):
                p.mm(bk_[:, n * 256:(n + 1) * 256], lhsE[:, g * GS + n, :], arE[:, g * GS + n, :])
            p.tt(dst.v(), bk_[:, 0:GS * 256].re("p (n r) -> p n r", n=GS),
                 CM[:, d:d + 1, :].bc([128, GS, 256]), ALU.mult)
        MS, MST, MIT = (0, 1, 3) if d == 0 else (1, 0, 2)

        def mmg(lf, rf, n_out=128):
            bk_ = bank()
            for n in range(GS):
                p.mm(bk_[:, n * n_out:(n + 1) * n_out], lf(n), rf(n))
            return bk_

        def Ec(tl):
            return lambda n, tl=tl: tl[:, g * GS + n, :]

        def Gc(tl):
            return lambda n, tl=tl: tl[:, n, :]

        def b4(bk_):
            return bk_[:, 0:GS * 128].re("p (n r) -> p n r", n=GS)

        def masked(dst, bk_, mi):
            p.tt(dst.v(), b4(bk_), masks[:, mi:mi + 1, :].bc([128, GS, 128]), ALU.mult)

        bk_ = mmg(Ec(aE), Ec(bE))
        masked(Lb[g], bk_, MS)
        mm2(LM, bE)
        tt_ = TTb[g][0]
        p.tt(tt_.v(), LT, ident_f.v().re("p (o r) -> p o r", o=1).bc([128, GS, 128]), ALU.add, eng='pool')
        yield
        P_, PT_ = Lb[g], LT
        for it in range(5):
            bk_ = mmg(Gc(PT_), Gc(P_))
            P2 = Pb[g][it % 2]
            p.copy(P2.v(), b4(bk_), eng='act')
            if it < 4:
                bk_ = mmg(Gc(P_), Gc(PT_))
                PT2 = PTb[g][it % 2]
                p.copy(PT2.v(), b4(bk_), eng='act')
            yield
            bk_ = mmg(Gc(P2), Gc(tt_))
            ttn = TTb[g][(it + 1) % 2]
            p.tt(ttn.v(), b4(bk_), tt_.v(), ALU.add)
            tt_ = ttn
            P_ = P2
            if it < 4:
                PT_ = PT2
            yield
        mm2(KM, kE)
        bk_ = mmg(Gc(tt_), Ec(aW))
        p.copy(TAb[g].v(), b4(bk_), eng='act')
        yield
        bk_ = mmg(Gc(LakT), lambda n: vst[d][:, g * GS + n, :], n_out=64)
        p.copy(Nb[g].v(), bk_[:, 0:GS * 64].re("p (n r) -> p n r", n=GS), eng='act')
        bk_ = mmg(Gc(TAb[g]), Ec(bW[d]))
        p.copy(SLb[d][gi].v(), b4(bk_), eng='act')
        bk_ = mmg(Gc(TAb[g]), Gc(MrbT))
        p.tt(YLb[d][gi].v(), b4(bk_), rE[:, g * GS:(g + 1) * GS, :], ALU.add)
        yield
        bk_ = mmg(Gc(tt_), Gc(Nb[g]), n_out=64)
        p.copy(TNb[d][gi].v(), bk_[:, 0:GS * 64].re("p (n r) -> p n r", n=GS), eng='act')
        yield

    def unit_parallel(d, gbase, extra=()):
        gens = [par_group(d, g, g) for g in range(NG)] + list(extra)
        alive = list(gens)
        while alive:
            for gen in list(alive):
                try:
                    next(gen)
                except StopIteration:
                    alive.remove(gen)
        return {g: g for g in range(NG)}

    def seq_steps(d, seg, G, first):
        order = range(8) if d == 0 else range(7, -1, -1)
        steps = []
        for c in order:
            def step(c=c):
                g = c // GS
                n = c % GS
                gi = G[g]
                k_ = scnt[d]
                scnt[d] += 1
                s_old, s_new = Sm[d][k_ % 2], Sm[d][(k_ + 1) % 2]
                b_old, b_new = Sb[d][k_ % 2], Sb[d][(k_ + 1) % 2]
                bs = bank()
                p.mm(bs[:, 0:64], bW[d][:, c, :], TNb[d][gi][:, n, :], start=True, stop=False)
                p.mm(bs[:, 0:64], kW[d][:, c, :], vst[d][:, c, :], start=False, stop=False)
                p.mm(bs[:, 0:64], SLb[d][gi][:, n, :], b_old.v(), start=False, stop=True)
                by = bank()
                p.mm(by[:, 0:64], LMb[d][gi][:, n, 128:256], TNb[d][gi][:, n, :], start=True, stop=False)
                p.mm(by[:, 0:64], KMb[d][gi][:, n, 128:256], vst[d][:, c, :], start=False, stop=False)
                p.mm(by[:, 0:64], YLb[d][gi][:, n, :], b_old.v(), start=False, stop=True)
                p.stt(b_new.v(), s_old.v(), gam[d][:, c:c + 1], bs[:, 0:64], ALU.mult, ALU.add)
                p.stt(s_new.v(), s_old.v(), gam[d][:, c:c + 1], bs[:, 0:64], ALU.mult, ALU.add)
                if first:
                    p.copy(yacc[seg][:, c, :], by[:, 0:64], eng='act')
                else:
                    p.tt(yacc[seg][:, c, :], yacc[seg][:, c, :], by[:, 0:64], ALU.add, eng='dve')
            steps.append(step)
        return steps

    def finalize(hp):
        FA = c3(zr.v())
        FB = c3(zk.v())
        p.dma(lnw.v(), lnw_d[hp])
        p.dma(lnb.v(), lnb_d[hp])
        cols = slice(hp * 128, (hp + 1) * 128)
        for seg in range(4):
            sl = slice(seg * SEG, (seg + 1) * SEG)
            cs = slice(seg * 8, (seg + 1) * 8)
            y = yacc[seg].v()
            p.red(fin_s.v(), y, ALU.add)
            p.ts(fin_s.v(), fin_s.v(), 1.0 / 64, ALU.mult)
            p.tt(FA, y, fin_s.v().re("p (c o) -> p c o", o=1).bc([128, 8, 64]), ALU.subtract)
            p.tt(FB, FA, FA, ALU.mult, eng='pool')
            p.red(fin_r.v(), FB, ALU.add)
            p.rsqrt(fin_r.v(), fin_r.v(), 1.0 / 64, 64e-5)
            p.tt(FA, FA, fin_r.v().re("p (c o) -> p c o", o=1).bc([128, 8, 64]), ALU.mult)
            p.tt(FA, FA, lnw.v().re("p (o v) -> p o v", o=1).bc([128, 8, 64]), ALU.mult)
            p.tt(FA, FA, lnb.v().re("p (o v) -> p o v", o=1).bc([128, 8, 64]), ALU.add)
            p.tt(FB, vst_all[seg].v(), bsc[seg].v().re("p (c o) -> p c o", o=1).bc([128, 8, 64]), ALU.mult, eng='pool')
            p.tt(FA, FA, FB, ALU.add)
            for h in range(2):
                ps_ = slice(64 * h, 64 * h + 64)
                p.copy(owide[ps_, :, 64 * h:64 * h + 64], c3(zr[ps_, :]), eng='pool' if h else 'dve')
            bk = bank()
            for c in range(8):
                p.mm(bk[:, c * 64:(c + 1) * 64], owide[:, c, :], sel_b.v())
            bg = bank()
            p.mm(bg.v(), gup0[:, cols], sgd0[:, sl], start=True, stop=False)
            p.mm(bg.v(), gup1[:, cols], sgd1[:, sl], start=False, stop=True)
            p.copy(gT.v(), bg.v(), eng='act')
            ms_ = mst2[fcnt[0] % 2]
            fcnt[0] += 1
            p.tt(ms_.v(), bk.v(), gT.v(), ALU.mult)
            p.dma(mix_d[:, 8 + hp, sl], ms_.v(), eng='pool')

    NHP = L.get('NHP', 8)
    gb = 0

    def drain(gen):
        for _ in gen:
            pass
    for hp in range(NHP):
        for d in range(2):
            p.memset(Sm[d][scnt[d] % 2].v(), 0.0)
            p.memset(Sb[d][scnt[d] % 2].v(), 0.0)
        U = []
        for s_ in range(4):
            U.append((0, s_, s_ < 2))
            U.append((1, 3 - s_, s_ < 2))
        drain(prepA(hp, U[0][0], U[0][1], U[0][2]))
        prepB(U[0][0], U[0][1])
        Gs = {}
        for k, (d, seg, first) in enumerate(U):
            nxt = U[k + 1] if k + 1 < len(U) else None
            extra = [prepA(hp, nxt[0], nxt[1], nxt[2])] if nxt else []
            Gs[d] = unit_parallel(d, gb, extra)
            if k % 2 == 1:
                gb += 1
                st0 = seq_steps(0, U[k - 1][1], Gs[0], first)
                st1 = seq_steps(1, seg, Gs[1], first)
                for a_, b_ in zip(st0, st1):
                    a_()
                    b_()
            if nxt:
                prepB(nxt[0], nxt[1])
        finalize(hp)
    p.pop()
def tail_stage(p, L):
    IN = L['IN']
    C = L['C']
    PB = L['PB']
    bank = L['bank']
    hbank = L['hbank']
    ident_b = L['ident_b']
    ident_f = L['ident_f']
    mix_d = L['mix_d']
    mixT = p.sb("mixT", [128, 16, T], BF16)
    x = L['x']
    out = L['out']
    scr = L['scr']
    scr2 = L['scr2']
    projT = L['projT']
    NE = L.get('NE', 16)
    w_out = IN("w_out", [D, D])
    nfw = IN("nfw", [1, D])
    n2w = IN("n2w", [1, D])
    wr_d = IN("w_router", [D, 16])
    eg_d = IN("e_gate", [16, D, D])
    eu_d = IN("e_up", [16, D, D])
    ed_d = IN("e_down", [16, D, D])
    xmid_d = p.dram("xmid_d", [T, D], F32)
    h2_d = p.dram("h2_d", [T, D], BF16)
    ye_d = p.dram("ye_d", [16, 2, 128, D], BF16)
    aff = p.sb("aff", [128, 16, 16], F32)
    valT = p.sb("valT", [128, 16, 16], F32)
    iota1 = p.sb("iota1", [128, 256], F32)
    p.dma(iota1.v(), C['iota1'].v())
    idxs = p.sb("idxs", [128, 32], F32)
    iop_f = p.sb("iop_f", [128, 1], F32)
    p.dma(iop_f.v(), C['iota_p'].v())

    p.push()
    for kc in range(16):
        p.dma(mixT[:, kc, :], mix_d[:, kc, :], eng='sp' if kc % 2 else 'pool')
    wo = p.sb("wo", [128, 16, D], BF16)
    wst = [p.sb("wst0", [128, D], F32)] * 2
    for kc in range(16):
        p.dma(wst[kc % 2].v(), w_out[kc * 128:(kc + 1) * 128, :], eng='sp')
        p.copy(wo[:, kc, :], wst[kc % 2].v(), eng='pool' if kc % 2 == 0 else 'dve')
    n2w_s = p.sb("n2w_s", [128, D], F32)
    p.dma(n2w_s.v(), n2w[0:1, :].bc([128, D]))
    wr = p.sb("wr", [128, 16, 16], F32)
    p.dma(wr.v(), wr_d.v().re("(k p) e -> p k e", p=128))
    xt2 = [p.sb("xt2_%d" % i, [128, D], F32) for i in range(2)]
    xm = [p.sb("xm%d" % i, [128, D], F32) for i in range(2)]
    h2f = p.sb("h2f", [128, D], F32)
    h2b = [p.sb("h2b0", [128, D], BF16)] * 2
    h2T = p.sb("h2T", [128, 16, 128], F32)
    ss2 = p.sb("ss2", [128, NT], F32)
    mx = p.sb("mx", [128, NT], F32)
    sm = p.sb("sm", [128, NT], F32)
    def stA(i):
        xa = xt2[i % 2]
        xo = xm[i % 2]
        p.dma(xa.v(), x[i * 128:(i + 1) * 128, :], eng='sp')
        for db in range(4):
            bk = bank()
            for kc in range(16):
                p.mm(bk.v(), mixT[:, kc, i * 128:(i + 1) * 128], wo[:, kc, db * 512:(db + 1) * 512],
                     start=(kc == 0), stop=(kc == 15))
            p.tt(xo[:, db * 512:(db + 1) * 512], bk.v(), xa[:, db * 512:(db + 1) * 512], ALU.add)
        p.dma(xmid_d[i * 128:(i + 1) * 128, :], xo.v(), eng='pool')

    def stB(i):
        xo = xm[i % 2]
        hb_ = h2b[i % 2]
        p.act(h2f.v(), xo.v(), AF.Square, accum=ss2[:, i:i + 1])
        p.rsqrt(ss2[:, i:i + 1], ss2[:, i:i + 1], 1.0 / D, EPS)
        p.ts(h2f.v(), xo.v(), ss2[:, i:i + 1], ALU.mult)
        p.tt(h2f.v(), h2f.v(), n2w_s.v(), ALU.mult)
        p.copy(hb_.v(), h2f.v(), eng='act')
        p.dma(h2_d[i * 128:(i + 1) * 128, :], hb_.v(), eng='pool')

    def stC(i):
        for q in range(4):
            bk = bank()
            for j in range(4):
                kc = q * 4 + j
                p.tr(bk[:, j * 128:(j + 1) * 128], h2f[:, kc * 128:(kc + 1) * 128], ident_f.v())
            p.copy(h2T[:, q * 4:(q + 1) * 4, :], bk.v().re("p (j t) -> p j t", j=4), eng='act' if q % 2 else 'dve')
        bk = bank()
        for kc in range(16):
            p.mm(bk[:, 0:16], h2T[:, kc, :], wr[:, kc, :], start=(kc == 0), stop=(kc == 15))
        p.red(mx[:, i:i + 1], bk[:, 0:16], ALU.max)
        p.ts(mx[:, i:i + 1], mx[:, i:i + 1], -1.0, ALU.mult)
        p.act(aff[:, i, :], bk[:, 0:16], AF.Exp, bias=mx[:, i:i + 1], accum=sm[:, i:i + 1])
        p.recip(sm[:, i:i + 1], sm[:, i:i + 1])
        p.ts(aff[:, i, :], aff[:, i, :], sm[:, i:i + 1], ALU.mult)

    stA(0)
    for i in range(NT):
        if i + 1 < NT:
            stA(i + 1)
        stB(i)
        stC(i)
    p.pop()

    p.push()
    for i in range(NT):
        p.dma(mixT[:, i, :], h2_d[i * 128:(i + 1) * 128, :], eng='sp' if i % 2 else 'pool')
    affT = p.sb("affT", [16, T], F32)
    work = p.sb("work", [16, T], F32)
    maskT = p.sb("maskT", [16, T], F32)
    onesT = p.sb("onesT", [16, T], F32)
    m8 = p.sb("m8", [16, 8], F32)
    p.memset(onesT.v(), 1.0, eng='pool')
    for q in range(4):
        bk = bank()
        for j in range(4):
            i = q * 4 + j
            p.tr(bk[0:16, j * 128:(j + 1) * 128], aff[:, i, :], ident_f.v())
        p.copy(affT[:, q * 512:(q + 1) * 512], bk[0:16, :], eng='dve')
    p.copy(work.v(), affT.v(), eng='dve')
    for r_ in range(32):
        p.op('dve', lambda e: e.max(out=m8.h[:], in_=work.h[:]), [work.v()], [m8.v()])
        if r_ < 31:
            p.op('dve', lambda e: e.match_replace(out=work.h[:], in_to_replace=m8.h[:], in_values=work.h[:],
                                                  imm_value=-1.0), [work.v(), m8.v()], [work.v()])
    p.ts(maskT.v(), affT.v(), m8[:, 7:8], ALU.is_ge)
    p.op('dve', lambda e: e.tensor_tensor_scan(work.h[:], onesT.h[:], maskT.h[:], 0.0, ALU.mult, ALU.add),
         [onesT.v(), maskT.v()], [work.v()])
    p.tt(work.v(), work.v(), maskT.v(), ALU.mult)
    bk = bank()
    for i in range(16):
        p.tr(bk[:, i * 16:(i + 1) * 16], work[:, i * 128:(i + 1) * 128], ident_f[0:16, 0:16])
    p.copy(valT.v(), bk[:, 0:256].re("p (i e) -> p i e", i=16), eng='dve')
    p.pop()

    p.push()
    h2s = mixT
    oh = p.sb("oh", [128, 16, 256], BF16)
    gm = p.sb("gm", [128, 16, 5], BF16)
    for i in range(16):
        p.memset(gm[:, i, 3:4], float(i))
        p.copy(gm[:, i, 4:5], iop_f.v())
    g1 = p.sb("g1", [128, 16], F32)
    g2 = p.sb("g2", [128, 16], F32)
    gb = p.sb("gb", [128, 16], BF16)
    gate = p.sb("gate", [128, 2], F32)
    g3 = p.sb("g3", [128, 2, 5], F32)
    xeT = p.sb("xeT", [128, 16, 256], BF16)
    hidT = oh
    ye = p.sb("ye", [128, 2, D], BF16)
    wb = [p.sb("ewb%d" % i, [128, 16, 1024], BF16) for i in range(3)]
    sg = [p.sb("sg%d" % i, [128, 256], F32) for i in range(2)]
    wc = [0]

    def wload(src, e, cb):
        k = wc[0]
        wc[0] += 1
        b = wb[k % 3]
        p.dma(b.v(), src[e][:, cb * 1024:(cb + 1) * 1024].re("(k p) c -> p k c", p=128), eng='pool')
        return b

    iota_bc = iota1.v().re("p (o s) -> p o s", o=1).bc([128, 16, 256])
    for e in range(NE):
        p.tt(oh.v(), iota_bc, valT[:, :, e:e + 1].bc([128, 16, 256]), ALU.is_equal)
        p.copy(gb.v(), aff[:, :, e])
        p.copy(gm[:, :, 0], gb.v())
        p.tt(g1.v(), aff[:, :, e], gb.v(), ALU.subtract)
        p.copy(gb.v(), g1.v())
        p.copy(gm[:, :, 1], gb.v())
        p.tt(g2.v(), g1.v(), gb.v(), ALU.subtract)
        p.copy(gm[:, :, 2], g2.v())
        bk = bank()
        for sh in range(2):
            for i in range(16):
                p.mm(bk[:, sh * 8:sh * 8 + 5], oh[:, i, sh * 128:(sh + 1) * 128], gm[:, i, :],
                     start=(i == 0), stop=(i == 15))
        p.copy(g3.v(), bk[:, 0:16].re("p (a c) -> p a c", a=2)[:, :, 0:5])
        p.red(gate.v(), g3[:, :, 0:3], ALU.add)
        p.stt(idxs[:, 2 * e:2 * e + 2], g3[:, :, 3], 128.0, g3[:, :, 4], ALU.mult, ALU.add)
        for dq in range(8):
            bk = bank()
            for j in range(2):
                dc = dq * 2 + j
                for i in range(16):
                    p.mm(bk[:, j * 256:(j + 1) * 256], h2s[:, i, dc * 128:(dc + 1) * 128], oh[:, i, :],
                         start=(i == 0), stop=(i == 15))
            p.copy(xeT[:, dq * 2:dq * 2 + 2, :], bk.v().re("p (j s) -> p j s", j=2), eng='act' if dq % 2 else 'dve')
        for fb in range(2):
            wg = wload(eg_d, e, fb)
            wu = wload(eu_d, e, fb)
            for fc2 in range(8):
                fc = fb * 8 + fc2
                bg = bank()
                for dc in range(16):
                    p.mm(bg[:, 0:256], wg[:, dc, fc2 * 128:(fc2 + 1) * 128], xeT[:, dc, :],
                         start=(dc == 0), stop=(dc == 15))
                bu = bank()
                for dc in range(16):
                    p.mm(bu[:, 0:256], wu[:, dc, fc2 * 128:(fc2 + 1) * 128], xeT[:, dc, :],
                         start=(dc == 0), stop=(dc == 15))
                s_ = sg[fc % 2]
                p.act(s_.v(), bg[:, 0:256], AF.Silu)
                p.tt(hidT[:, fc, :], s_.v(), bu[:, 0:256], ALU.mult)
        for db in range(2):
            wd = wload(ed_d, e, db)
            for sh in range(2):
                for j in range(2):
                    bk = bank()
                    for fc in range(16):
                        p.mm(bk.v(), hidT[:, fc, sh * 128:(sh + 1) * 128], wd[:, fc, j * 512:(j + 1) * 512],
                             start=(fc == 0), stop=(fc == 15))
                    c0 = db * 1024 + j * 512
                    p.ts(ye[:, sh, c0:c0 + 512], bk.v(), gate[:, sh:sh + 1], ALU.mult)
        p.dma(ye_d[e].re("s p d -> p s d"), ye.v(), eng='sp')
    p.pop()

    p.push()
    yeh = p.sb("yeh", [128, 2 * NE, 1024], BF16)
    idm = p.sb("idm", [128, 32], F32)
    GT = p.sb("GT", [128, 32, 128], BF16)
    xr = [p.sb("xr%d" % i, [128, 1024], F32) for i in range(2)]
    iota_bc2 = iota1.v().re("p (o s) -> p o s", o=1).bc([128, 16, 256])
    for half in range(2):
        hs = slice(half * 1024, (half + 1) * 1024)
        for e in range(NE):
            p.dma(yeh[:, 2 * e:2 * e + 2, :], ye_d[e][:, :, hs].re("s p d -> p s d"), eng='sp' if e % 2 else 'pool')
        for i in range(NT):
            p.ts(idm.v(), idxs.v(), float(1 - 128 * i), ALU.add)
            p.tt(GT.v(), iota1[:, 0:128].re("p (o t) -> p o t", o=1).bc([128, 32, 128]),
                 idm.v().re("p (k o) -> p k o", o=1).bc([128, 32, 128]), ALU.is_equal)
            xa = xr[i % 2]
            p.dma(xa.v(), xmid_d[i * 128:(i + 1) * 128, hs], eng='sp')
            for db in range(2):
                bk = bank()
                for k in range(2 * NE):
                    p.mm(bk.v(), GT[:, k, :], yeh[:, k, db * 512:(db + 1) * 512], start=(k == 0), stop=(k == 2 * NE - 1))
                p.tt(xa[:, db * 512:(db + 1) * 512], xa[:, db * 512:(db + 1) * 512], bk.v(), ALU.add)
            p.dma(xmid_d[i * 128:(i + 1) * 128, hs], xa.v(), eng='pool')
    p.pop()

    p.push()
    nfw_s = p.sb("nfw_s", [128, D], F32)
    p.dma(nfw_s.v(), nfw[0:1, :].bc([128, D]))
    xf = [p.sb("xf%d" % i, [128, D], F32) for i in range(2)]
    junk2 = p.sb("junk2", [128, D], F32)
    ss3 = p.sb("ss3", [128, NT], F32)
    for i in range(NT):
        xo = xf[i % 2]
        p.dma(xo.v(), xmid_d[i * 128:(i + 1) * 128, :], eng='sp')
        p.act(junk2.v(), xo.v(), AF.Square, accum=ss3[:, i:i + 1])
        p.rsqrt(ss3[:, i:i + 1], ss3[:, i:i + 1], 1.0 / D, EPS)
        p.ts(xo.v(), xo.v(), ss3[:, i:i + 1], ALU.mult)
        p.tt(xo.v(), xo.v(), nfw_s.v(), ALU.mult)
        p.dma(out[i * 128:(i + 1) * 128, :], xo.v(), eng='pool')
    p.dma(scr2.v(), projT[0:1, 0:16])
    p.finish([out.v()], scr.v(), scr2.v())
    p.pop()
```

```python
import numpy as np
import ml_dtypes
from contextlib import ExitStack
import concourse.bass as bass
import concourse.mybir as mybir
from concourse.bass_utils import run_bass_kernel_spmd

F32 = mybir.dt.float32
BF16 = mybir.dt.bfloat16
I32 = mybir.dt.int32
ALU = mybir.AluOpType
AF = mybir.ActivationFunctionType
AX = mybir.AxisListType
ENG = ['pe', 'act', 'dve', 'pool', 'sp']
NDS = 24


class V:
    def __init__(s, tile, ap):
        s.tile = tile
        s.ap = ap

    def __getitem__(s, k):
        return V(s.tile, s.ap[k])

    def re(s, pat, **kw):
        return V(s.tile, s.ap.rearrange(pat, **kw))

    def bc(s, shape):
        return V(s.tile, s.ap.to_broadcast(shape))

    def bitcast(s, dt):
        return V(s.tile, s.ap.bitcast(dt))


class Tile:
    def __init__(s, h, name):
        s.h = h
        s.name = name
        s.w = None
        s.r = {}

    def __getitem__(s, k):
        return V(s, s.h[k])

    def v(s):
        return V(s, s.h[:])


class Prog:
    def __init__(s, nc, es):
        s.nc = nc
        s.es = es
        s.q = {e: [] for e in ENG}
        s.cnt = {e: 0 for e in ENG}
        s.sems = {e: es.enter_context(nc.semaphore("s_" + e)) for e in ENG}
        s.dsems = [es.enter_context(nc.semaphore("d%d" % i)) for i in range(NDS)]
        s.dcnt = [0] * NDS
        s.dnext = 0
        s.seen = {e: {} for e in ENG}
        s.n = 0

    def semof(s, k):
        return s.dsems[k[1]] if isinstance(k, tuple) else s.sems[k]

    def sb(s, name, shape, dt):
        return Tile(s.es.enter_context(s.nc.sbuf_tensor(name, list(shape), dt)), name)

    def ps(s, name, shape, dt):
        return Tile(s.es.enter_context(s.nc.psum_tensor(name, list(shape), dt)), name)

    def dram(s, name, shape, dt, kind="Internal"):
        return Tile(s.nc.dram_tensor(name, list(shape), dt, kind=kind), name)

    def op(s, eng, fn, reads, writes, dma=False, acc=False):
        waits = {}

        def need(k):
            if k is None:
                return
            waits[k[0]] = max(waits.get(k[0], 0), k[1])

        for v in reads:
            need(v.tile.w)
        for v in writes:
            t = v.tile
            if not (acc and t.w is not None and t.w[0] == 'pe'):
                need(t.w)
            for k, val in t.r.items():
                need((k, val))
        if dma:
            i = s.dnext
            s.dnext = (i + 1) % NDS
            key = ('d', i)
            need((key, s.dcnt[i]))
            s.dcnt[i] += 16
            val = s.dcnt[i]
            inc = 16
        else:
            key = eng
            s.cnt[eng] += 1
            val = s.cnt[eng]
            inc = 1
        wl = []
        for k, v in waits.items():
            if v <= 0 or s.seen[eng].get(k, 0) >= v:
                continue
            s.seen[eng][k] = v
            wl.append((k, v))
        s.q[eng].append((wl, fn, key, inc))
        s.n += 1
        for v in reads:
            v.tile.r[key] = max(v.tile.r.get(key, 0), val)
        for v in writes:
            v.tile.w = (key, val)
            v.tile.r = {}

    def dma(s, out, in_, eng='sp', **kw):
        s.op(eng, lambda e: e.dma_start(out=out.ap, in_=in_.ap, **kw), [in_], [out], dma=True)

    def mm(s, out, lhsT, rhs, start=True, stop=True):
        s.op('pe', lambda e: e.matmul(out.ap, lhsT.ap, rhs.ap, start=start, stop=stop),
             [lhsT, rhs], [out], acc=not start)

    def tr(s, out, in_, ident):
        s.op('pe', lambda e: e.transpose(out.ap, in_.ap, ident.ap), [in_, ident], [out])

    def act(s, out, in_, func, bias=None, scale=None, accum=None):
        kw = {}
        rd = [in_]
        wr = [out]
        if bias is not None:
            if isinstance(bias, V):
                kw['bias'] = bias.ap
                rd.append(bias)
            else:
                kw['bias'] = bias
        if scale is not None:
            if isinstance(scale, V):
                kw['scale'] = scale.ap
                rd.append(scale)
            else:
                kw['scale'] = scale
        if accum is not None:
            kw['accum_out'] = accum.ap
            wr.append(accum)
        s.op('act', lambda e: e.activation(out.ap, in_.ap, func, **kw), rd, wr)

    def tt(s, out, a, b, op, eng='dve'):
        s.op(eng, lambda e: e.tensor_tensor(out.ap, a.ap, b.ap, op), [a, b], [out])

    def ts(s, out, a, s1, op0, s2=None, op1=None, eng='dve', accum=None):
        rd = [a]
        wr = [out]
        a1 = s1
        a2 = s2
        if isinstance(s1, V):
            rd.append(s1)
            a1 = s1.ap
        if isinstance(s2, V):
            rd.append(s2)
            a2 = s2.ap
        kw = {}
        if op1 is not None:
            kw['op1'] = op1
        if accum is not None:
            kw['accum_out'] = accum.ap
            wr.append(accum)
        s.op(eng, lambda e: e.tensor_scalar(out.ap, a.ap, a1, a2, op0, **kw), rd, wr)

    def stt(s, out, a, sc, b, op0, op1, eng='dve'):
        rd = [a, b]
        a1 = sc
        if isinstance(sc, V):
            rd.append(sc)
            a1 = sc.ap
        s.op(eng, lambda e: e.scalar_tensor_tensor(out.ap, a.ap, a1, b.ap, op0, op1), rd, [out])

    def copy(s, out, in_, eng='dve'):
        if eng == 'act':
            s.op('act', lambda e: e.copy(out.ap, in_.ap), [in_], [out])
        else:
            s.op(eng, lambda e: e.tensor_copy(out.ap, in_.ap), [in_], [out])

    def memset(s, out, val, eng='dve'):
        s.op(eng, lambda e: e.memset(out.ap, val), [], [out])

    def red(s, out, in_, op, axis=None, eng='dve'):
        ax = AX.X if axis is None else axis
        s.op(eng, lambda e: e.tensor_reduce(out.ap, in_.ap, ax, op), [in_], [out])

    def recip(s, out, in_):
        s.op('dve', lambda e: e.reciprocal(out.ap, in_.ap), [in_], [out])

    def rsqrt(s, out, in_, mul, add):
        s.ts(out, in_, mul, ALU.mult, add, ALU.add)
        s.act(out, out, AF.Sqrt)
        s.recip(out, out)

    def push(s):
        s._outer = s.es
        s._inner = ExitStack()
        s.es = s._inner

    def pop(s):
        targets = [(e, s.cnt[e]) for e in ENG] + [(('d', i), s.dcnt[i]) for i in range(NDS)]
        for e in ENG:
            wl = []
            for k, v in targets:
                if v <= 0 or s.seen[e].get(k, 0) >= v:
                    continue
                s.seen[e][k] = v
                wl.append((k, v))
            s.q[e].append((wl, None, None, 0))
        s.emit()
        s._inner.close()
        s.es = s._outer

    def coll(s, kind, op, ins, outs, groups):
        s.op('pool', lambda e: e.collective_compute(kind, op, replica_groups=groups,
                                                    ins=[i.ap for i in ins], outs=[o.ap for o in outs]),
             ins, outs, dma=True)

    def finish(s, outs, scratch_dst, scratch_src):
        s.op('sp', lambda e: e.dma_start(out=scratch_dst.ap, in_=scratch_src.ap), list(outs) + [scratch_src],
             [scratch_dst], dma=True)
        k, val = scratch_dst.tile.w
        s.q['sp'].append(([(k, val)], None, None, 0))

    def emit(s):
        nc = s.nc
        qs = s.q
        s.q = {e: [] for e in ENG}
        with nc.Block() as block:
            s._emit_block(block, qs)

    def _emit_block(s, block, qs):
        waited = set()
        for e in ENG:
            for wl, fn, key, inc in qs[e]:
                for k, v in wl:
                    waited.add((k, v))
        if not hasattr(s, 'sig'):
            s.sig = {e: 0 for e in ENG}
            s.pos = {e: 0 for e in ENG}
            s.remap = {e: {0: 0} for e in ENG}
        plan = {}
        for e in ENG:
            pos = s.pos[e]
            sig = s.sig[e]
            flags = []
            for wl, fn, key, inc in qs[e]:
                if fn is None or isinstance(key, tuple):
                    flags.append(False)
                    continue
                pos += 1
                if (e, pos) in waited:
                    sig += 1
                    s.remap[e][pos] = sig
                    flags.append(True)
                else:
                    flags.append(False)
            s.pos[e] = pos
            s.sig[e] = sig
            plan[e] = flags

        def mk(e):
            def body(eng):
                for (wl, fn, key, inc), flag in zip(qs[e], plan[e]):
                    for k, v in wl:
                        if isinstance(k, tuple):
                            eng.wait_ge(s.semof(k), v)
                        else:
                            eng.wait_ge(s.semof(k), s.remap[k][v])
                    if fn is None:
                        continue
                    ins = fn(eng)
                    if isinstance(key, tuple):
                        ins.then_inc(s.semof(key), inc)
                    elif flag:
                        ins.then_inc(s.semof(key), 1)
            return body

        block.tensor(mk('pe'))
        block.scalar(mk('act'))
        block.vector(mk('dve'))
        block.gpsimd(mk('pool'))
        block.sync(mk('sp'))
D = 2048
T = 2048
NT = 16
NCOL = 6432
NCH = 51
EPS = 1e-6


def consts_np():
    c = {}
    c['ident_f'] = np.eye(128, dtype=np.float32)
    rho = np.arange(128)
    hh = rho // 64
    ii = rho % 64
    same = (hh[:, None] == hh[None, :])
    SL = (same & (ii[None, :] < ii[:, None])).astype(np.float32)
    IL = (same & (ii[None, :] <= ii[:, None])).astype(np.float32)
    masks = np.stack([SL, SL.T, IL, IL.T], 0)
    c['masks'] = np.ascontiguousarray(np.tile(masks[:, :, None, :], (1, 1, 4, 1)).reshape(4, 128, 512))
    rm = np.ones((128, T), np.float32)
    rm[:, ::64] = 0.0
    c['resetmask'] = rm
    c['iota1'] = np.tile(np.arange(1, 257, dtype=np.float32)[None, :], (128, 1))
    invf = np.zeros((128, 1), np.float32)
    PT = np.zeros((128, 128), np.float32)
    for cc in range(2):
        for j in range(8):
            f = 500000.0 ** (-(2.0 * j) / 16.0)
            p1 = cc * 64 + j
            p2 = cc * 64 + 8 + j
            invf[p1, 0] = f
            invf[p2, 0] = f
            PT[p2, p1] = -1.0
            PT[p1, p2] = 1.0
    c['invf'] = invf
    c['PT'] = PT
    c['blockones'] = same.astype(np.float32)
    sel = np.zeros((128, 64), np.float32)
    sel[rho, ii] = 1.0
    c['sel'] = sel
    c['iota_p'] = np.arange(128, dtype=np.float32).reshape(128, 1)
    return c


def build(stage=99):
    nc = bass.Bass("TRN2", target_bir_lowering=False)
    es = ExitStack()
    dbg = {}
    with es:
        p = Prog(nc, es)
        IN = lambda n, s, dt=F32: p.dram(n, s, dt, kind="ExternalInput")
        x = IN("x", [T, D])
        pos = IN("pos", [1, T], I32)
        n1w = IN("n1w", [128, 16])
        w_in = IN("w_in", [D, NCOL])
        out = p.dram("out", [T, D], F32, kind="ExternalOutput")
        projT = p.dram("projT", [NCH * 128, T], F32, kind=("ExternalOutput" if stage == 1 else "Internal"))
        scr = p.dram("scr", [1, 16], F32)
        scr2 = p.dram("scr2", [1, 16], F32)
        cn = consts_np()
        C = {k: IN("c_" + k, list(v.shape)) for k, v in cn.items()}

        ident_f = p.sb("ident_f", [128, 128], F32)
        ident_b = p.sb("ident_b", [128, 128], BF16)
        p.dma(ident_f.v(), C['ident_f'].v())
        p.copy(ident_b.v(), ident_f.v())
        n1w_s = p.sb("n1w_s", [128, 16], F32)
        p.dma(n1w_s.v(), n1w.v())

        PB = [p.ps("pb%d" % i, [128, 512], F32) for i in range(6)]
        PH = [p.ps("ph%d" % i, [128, 1024], BF16) for i in range(2)]
        st = {'pb': 0, 'ph': 0}

        def bank():
            st['pb'] = (st['pb'] + 1) % 6
            return PB[st['pb']]

        def hbank():
            st['ph'] = (st['ph'] + 1) % 2
            return PH[st['ph']]

        p.push()
        hT = p.sb("hT", [128, 16, T], BF16)
        xt = [p.sb("xt%d" % i, [128, D], F32) for i in range(2)]
        xn = [p.sb("xn%d" % i, [128, D], BF16) for i in range(2)]
        junk = p.sb("junk", [128, D], F32)
        ssq = p.sb("ssq", [128, NT], F32)
        rstd = p.sb("rstd", [128, NT], F32)
        for i in range(NT):
            a = xt[i % 2]
            b = xn[i % 2]
            p.dma(a.v(), x[i * 128:(i + 1) * 128, :], eng='sp' if i % 2 == 0 else 'pool')
            p.act(junk.v(), a.v(), AF.Square, accum=ssq[:, i:i + 1])
            p.rsqrt(rstd[:, i:i + 1], ssq[:, i:i + 1], 1.0 / D, EPS)
            p.ts(b.v(), a.v(), rstd[:, i:i + 1], ALU.mult)
            for g in range(2):
                hb = hbank()
                for j in range(8):
                    kc = g * 8 + j
                    p.tr(hb[:, j * 128:(j + 1) * 128], b[:, kc * 128:(kc + 1) * 128], ident_b.v())
                src = hb.v().re("p (j t) -> p j t", j=8)
                dst = hT[:, g * 8:(g + 1) * 8, i * 128:(i + 1) * 128]
                if g == 0:
                    p.copy(dst, src, eng='act')
                else:
                    p.copy(dst, src, eng='dve')

        wf = [p.sb("wf%d" % i, [128, 16, 128], F32) for i in range(2)]
        wb = [p.sb("wb%d" % i, [128, 16, 128], BF16) for i in range(2)]
        prow = [p.sb("prow%d" % i, [128, T], F32) for i in range(2)]
        n1w_bc = n1w_s.v().re("p (k o) -> p k o", o=1)
        for ch in range(NCH):
            c0 = ch * 128
            m = min(128, NCOL - c0)
            f = wf[ch % 2]
            b = wb[ch % 2]
            pr = prow[ch % 2]
            p.dma(f[:, :, 0:m], w_in[:, c0:c0 + m].re("(k p) c -> p k c", p=128), eng='sp')
            p.tt(b[:, :, 0:m], f[:, :, 0:m], n1w_bc.bc([128, 16, m]), ALU.mult, eng='pool')
            for tb in range(4):
                bk = bank()
                for kc in range(16):
                    p.mm(bk[0:m, :], b[:, kc, 0:m], hT[:, kc, tb * 512:(tb + 1) * 512],
                         start=(kc == 0), stop=(kc == 15))
                p.copy(pr[0:m, tb * 512:(tb + 1) * 512], bk[0:m, :], eng='act' if tb % 2 == 0 else 'dve')
            p.dma(projT[c0:c0 + m, :], pr[0:m, :], eng='pool')
        if stage == 1:
            p.dma(scr2.v(), projT[0:1, 0:16])
            p.finish([projT.v()], scr.v(), scr2.v())
            p.pop()
            return nc, cn
        p.pop()

        mix_d = p.dram("mix_d", [128, 16, T], BF16)
        lam4 = IN("lam4", [1, 256])
        sublnw = IN("sublnw", [1, 128])
        p.push()
        PTf = p.sb("PTf", [128, 128], F32)
        PTb = p.sb("PTb", [128, 128], BF16)
        p.dma(PTf.v(), C['PT'].v())
        p.copy(PTb.v(), PTf.v())
        invf = p.sb("invf", [128, 1], F32)
        p.dma(invf.v(), C['invf'].v())
        posi = p.sb("posi", [128, T], I32)
        p.dma(posi.v(), pos[0:1, :].bc([128, T]))
        ang = p.sb("ang", [128, T], F32)
        cosF = p.sb("cosF", [128, T], F32)
        sinF = p.sb("sinF", [128, T], F32)
        p.copy(ang.v(), posi.v())
        p.ts(ang.v(), ang.v(), invf[:, 0:1], ALU.mult)
        angi = posi
        def rangered(dst, src):
            p.ts(t1x.v(), src, 1.0 / (2 * np.pi), ALU.mult)
            p.copy(angi.v(), t1x.v())
            p.copy(t1x.v(), angi.v())
            p.stt(dst, t1x.v(), -2 * np.pi, src, ALU.mult, ALU.add)
            p.ts(t1x.v(), dst, np.pi, ALU.is_gt, 2 * np.pi, ALU.mult)
            p.tt(dst, dst, t1x.v(), ALU.subtract)
            p.ts(t1x.v(), dst, -np.pi, ALU.is_lt, 2 * np.pi, ALU.mult)
            p.tt(dst, dst, t1x.v(), ALU.add)
        t1x = p.sb("t1x", [128, T], F32)
        rangered(sinF.v(), ang.v())
        p.act(sinF.v(), sinF.v(), AF.Sin)
        p.ts(ang.v(), ang.v(), np.pi / 2, ALU.add)
        rangered(cosF.v(), ang.v())
        p.act(cosF.v(), cosF.v(), AF.Sin)
        l4 = p.sb("l4", [128, 256], F32)
        p.dma(l4.v(), lam4[0:1, :].bc([128, 256]))
        lprod = p.sb("lprod", [128, 2, 64], F32)
        p.tt(lprod.v(), l4.v().re("p (a b d) -> p a b d", a=2, b=2)[:, :, 0, :],
             l4.v().re("p (a b d) -> p a b d", a=2, b=2)[:, :, 1, :], ALU.mult)
        lsum = p.sb("lsum", [128, 2], F32)
        p.red(lsum.v(), lprod.v(), ALU.add)
        p.act(lsum.v(), lsum.v(), AF.Exp)
        lam = p.sb("lam", [128, 1], F32)
        p.tt(lam.v(), lsum[:, 0:1], lsum[:, 1:2], ALU.subtract)
        p.ts(lam.v(), lam.v(), 0.2, ALU.add)
        slw = p.sb("slw", [128, 128], F32)
        p.dma(slw.v(), sublnw[0:1, :].bc([128, 128]))
        p.ts(slw.v(), slw.v(), 0.8, ALU.mult)

        qf = p.sb("qf", [128, T], F32)
        kf = p.sb("kf", [128, T], F32)
        vf = p.sb("vf", [128, T], F32)
        xq16 = p.sb("xq16", [128, T], BF16)
        xk16 = p.sb("xk16", [128, T], BF16)
        xv16 = p.sb("xv16", [128, T], BF16)
        t1 = p.sb("t1", [128, T], F32)
        QR = [p.sb("qr%d" % i, [128, T], BF16) for i in range(2)]
        KR = [p.sb("kr%d" % i, [128, T], BF16) for i in range(2)]
        VA = [p.sb("vaug%d" % i, [128, 16, 129], BF16) for i in range(2)]
        for i in range(2):
            p.memset(VA[i].v(), 1.0)
        pT = [p.sb("pT%d" % i, [128, 512], BF16) for i in range(3)]
        oacc = [p.sb("oacc%d" % i, [128, 4, 129], F32) for i in range(2)]
        att = p.sb("att", [128, 4, 128], F32)
        att1 = p.sb("att1", [128, 4, 128], F32)
        attb = p.sb("attb", [128, 4, 128], BF16)
        rr = p.sb("rr", [128, 2, 4], F32)
        ssa = p.sb("ssa", [128, 4], F32)
        mst = [p.sb("mst%d" % i, [128, 512], BF16) for i in range(2)]
        cnt = 0

        def prep_load(h):
            p.dma(qf.v(), projT[h * 128:(h + 1) * 128, :], eng='sp')
            p.dma(kf.v(), projT[1024 + h * 128:1024 + (h + 1) * 128, :], eng='sp')
            p.dma(vf.v(), projT[2048 + h * 128:2048 + (h + 1) * 128, :], eng='sp')
            p.copy(xq16.v(), qf.v(), eng='pool')
            p.copy(xk16.v(), kf.v(), eng='pool')
            p.copy(xv16.v(), vf.v(), eng='pool')
            p.tt(qf.v(), qf.v(), cosF.v(), ALU.mult, eng='pool')
            p.tt(kf.v(), kf.v(), cosF.v(), ALU.mult, eng='pool')

        def prep_pe(h):
            qr, kr, vaug = QR[h % 2], KR[h % 2], VA[h % 2]
            for (x16, src, dst) in ((xq16, qf, qr), (xk16, kf, kr)):
                for tb in range(4):
                    bk = PB[4 + tb % 2]
                    sl = slice(tb * 512, (tb + 1) * 512)
                    p.mm(bk.v(), PTb.v(), x16[:, sl])
                    p.tt(t1[:, sl], bk.v(), sinF[:, sl], ALU.mult)
                p.tt(dst.v(), src.v(), t1.v(), ALU.add)
            for g in range(2):
                hb = hbank()
                for j in range(8):
                    kt = g * 8 + j
                    p.tr(hb[:, j * 128:(j + 1) * 128], xv16[:, kt * 128:(kt + 1) * 128], ident_b.v())
                p.copy(vaug[:, g * 8:(g + 1) * 8, 0:128], hb.v().re("p (j e) -> p j e", j=8), eng='dve')

        prep_load(0)
        prep_pe(0)
        for h in range(8):
            qr, kr, vaug = QR[h % 2], KR[h % 2], VA[h % 2]
            for qb in range(4):
                if h + 1 < 8 and qb == 0:
                    prep_load(h + 1)
                if h + 1 < 8 and qb == 2:
                    prep_pe(h + 1)
                qsl = slice(qb * 512, (qb + 1) * 512)
                its = [(c, kt) for c in range(2) for kt in range(16)]

                def qk(i):
                    c, kt = its[i]
                    ps_ = slice(64 * c, 64 * c + 64)
                    p.mm(PB[4 + i % 2].v(), kr[ps_, kt * 128:(kt + 1) * 128], qr[ps_, qsl])
                qk(0)
                for i, (c, kt) in enumerate(its):
                    if i + 1 < len(its):
                        qk(i + 1)
                    sbk = PB[4 + i % 2]
                    pt_ = pT[i % 3]
                    p.act(pt_.v(), sbk.v(), AF.Exp, scale=0.125)
                    for qs in range(4):
                        p.mm(PB[qs][:, 0:129], pt_[:, qs * 128:(qs + 1) * 128], vaug[:, kt, :],
                             start=(kt == 0), stop=(kt == 15))
                    if kt == 15:
                        for qs in range(4):
                            p.copy(oacc[c][:, qs, :], PB[qs][:, 0:129], eng='dve')
                p.recip(rr[:, 0, :], oacc[0][:, :, 128])
                p.recip(rr[:, 1, :], oacc[1][:, :, 128])
                p.ts(rr[:, 1, :], rr[:, 1, :], lam[:, 0:1], ALU.mult)
                p.tt(att.v(), oacc[0][:, :, 0:128], rr[:, 0, :].re("p (q o) -> p q o", o=1).bc([128, 4, 128]), ALU.mult)
                p.tt(att1.v(), oacc[1][:, :, 0:128], rr[:, 1, :].re("p (q o) -> p q o", o=1).bc([128, 4, 128]), ALU.mult, eng='pool')
                p.tt(att.v(), att.v(), att1.v(), ALU.subtract)
                p.tt(att1.v(), att.v(), att.v(), ALU.mult, eng='pool')
                p.red(ssa.v(), att1.v(), ALU.add)
                p.rsqrt(ssa.v(), ssa.v(), 1.0 / 128, 1e-5)
                p.tt(att.v(), att.v(), ssa.v().re("p (q o) -> p q o", o=1).bc([128, 4, 128]), ALU.mult)
                p.tt(attb.v(), att.v(), slw.v().re("p (o e) -> p o e", o=1).bc([128, 4, 128]), ALU.mult)
                hb = hbank()
                for qs in range(4):
                    p.tr(hb[:, qs * 128:(qs + 1) * 128], attb[:, qs, :], ident_b.v())
                ms_ = mst[(h * 4 + qb) % 2]
                p.copy(ms_.v(), hb[:, 0:512], eng='act')
                p.dma(mix_d[:, h, qsl], ms_.v(), eng='pool')
        p.pop()

        NHP = 8 if stage != 3 else 1
        rwkv_stage(p, locals())
        tail_stage(p, locals())
    return nc, cn


def moe_hook(p, L, i):
    pass


def rwkv_inputs(inp):
    m = {}

    def cols28(mu):
        o = np.zeros((128, 28), np.float32)
        o[:, 0:24] = mu[0:3072].reshape(24, 128).T
        o[0:64, 24] = mu[3072:3136]
        o[64:128, 25] = mu[3136:3200]
        o[:, 26] = mu[3200:3328]
        o[0:32, 27] = mu[3328:3360]
        return o
    m["muP"] = cols28(inp['mu_prev'][0])
    m["muN"] = cols28(inp['mu_next'][0])
    m["w0r"] = np.ascontiguousarray(inp['w0'][0].reshape(2, 8, 128).transpose(2, 0, 1).reshape(128, 16))
    m["a0r"] = np.ascontiguousarray(inp['a0'][0].reshape(2, 8, 128).transpose(2, 0, 1).reshape(128, 16))
    m["kkr"] = np.ascontiguousarray(inp['k_k'][0].reshape(8, 128).T)
    m["kar"] = np.ascontiguousarray(inp['k_a'][0].reshape(8, 128).T)
    m["rkr"] = np.ascontiguousarray(inp['r_k'][0].reshape(8, 128).T)
    m["lnw_tok"] = np.ascontiguousarray(np.repeat(inp['ln_x_w'][0].reshape(8, 2, 1, 64), 64, axis=2).reshape(8, 128, 64))
    m["lnb_tok"] = np.ascontiguousarray(np.repeat(inp['ln_x_b'][0].reshape(8, 2, 1, 64), 64, axis=2).reshape(8, 128, 64))
    m["decay_up"] = np.ascontiguousarray(inp['decay_up'][0])
    m["iclr_up"] = np.ascontiguousarray(inp['iclr_up'][0])
    m["gate_up"] = np.ascontiguousarray(inp['gate_up'][0])
    return {k: v.astype(np.float32) for k, v in m.items()}


def kernel(**inputs):
    inp = {k: np.asarray(v) for k, v in inputs.items()}
    nc, cn = build(99)

    def core_inputs(b):
        m = {"x": np.ascontiguousarray(inp['x'][b]),
             "pos": np.ascontiguousarray(inp['positions'][b][None, :].astype(np.int32)),
             "n1w": np.ascontiguousarray(inp['norm1_w'][0].reshape(16, 128).T),
             "w_in": np.ascontiguousarray(inp['w_in'][0]),
             "lam4": np.concatenate([inp['lambda_q1'][0], inp['lambda_k1'][0], inp['lambda_q2'][0],
                                     inp['lambda_k2'][0]])[None, :].astype(np.float32),
             "sublnw": inp['subln_w'].astype(np.float32).reshape(1, 128),
             "w_out": np.ascontiguousarray(inp['w_out'][0]),
             "nfw": inp['norm_f_w'].astype(np.float32).reshape(1, D),
             "n2w": inp['norm2_w'].astype(np.float32).reshape(1, D),
             "w_router": w_router, "e_gate": e_gate, "e_up": e_up, "e_down": e_down}
        m.update(rw)
        for k, v in cn.items():
            m["c_" + k] = v
        return m

    rw = rwkv_inputs(inp)
    w_router = np.ascontiguousarray(inp['w_router'][0])
    e_gate = np.ascontiguousarray(inp['e_gate'][0])
    e_up = np.ascontiguousarray(inp['e_up'][0])
    e_down = np.ascontiguousarray(inp['e_down'][0])
    maps = [core_inputs(c // 2) for c in range(8)]
    res = run_bass_kernel_spmd(nc, maps, core_ids=list(range(8)))
    return np.stack([res.results[2 * b]["out"] for b in range(4)], 0).astype(np.float32)


def rwkv_stage(p, L):
    IN = L['IN']
    C = L['C']
    PB = L['PB']
    bank = L['bank']
    hbank = L['hbank']
    ident_b = L['ident_b']
    ident_f = L['ident_f']
    projT = L['projT']
    mix_d = L['mix_d']
    muP_d = IN("muP", [128, 28])
    muN_d = IN("muN", [128, 28])
    w0r_d = IN("w0r", [128, 16])
    a0r_d = IN("a0r", [128, 16])
    kkr_d = IN("kkr", [128, 8])
    kar_d = IN("kar", [128, 8])
    rkr_d = IN("rkr", [128, 8])
    lnw_d = IN("lnw_tok", [8, 128, 64])
    lnb_d = IN("lnb_tok", [8, 128, 64])
    dup_d = IN("decay_up", [2, 64, 1024])
    iup_d = IN("iclr_up", [2, 64, 1024])
    gup_d = IN("gate_up", [160, 1024])
    p.push()
    SEG = 512
    muP = p.sb("muP_s", [128, 28], F32)
    muN = p.sb("muN_s", [128, 28], F32)
    mu0 = p.sb("mu0_s", [128, 28], F32)
    p.dma(muP.v(), muP_d.v())
    p.dma(muN.v(), muN_d.v())
    p.tt(mu0.v(), muP.v(), muN.v(), ALU.add)
    p.ts(mu0.v(), mu0.v(), -1.0, ALU.mult, 1.0, ALU.add)
    w0r = p.sb("w0r_s", [128, 16], F32)
    a0r = p.sb("a0r_s", [128, 16], F32)
    kkr = p.sb("kkr_s", [128, 8], F32)
    kar = p.sb("kar_s", [128, 8], F32)
    rkr = p.sb("rkr_s", [128, 8], F32)
    for t_, d_ in ((w0r, w0r_d), (a0r, a0r_d), (kkr, kkr_d), (kar, kar_d), (rkr, rkr_d)):
        p.dma(t_.v(), d_.v())
    masks = p.sb("masks_s", [128, 4, 128], F32)
    p.dma(masks.v(), C['masks'].v().re("m p (n r) -> p m n r", n=4)[:, :, 0, :])
    rmask = p.sb("rmask", [128, SEG], F32)
    p.dma(rmask.v(), C['resetmask'][:, 0:SEG])
    bones_f = p.sb("bones_f", [128, 128], F32)
    bones = p.sb("bones", [128, 128], BF16)
    p.dma(bones_f.v(), C['blockones'].v())
    p.copy(bones.v(), bones_f.v())
    sel_f = p.sb("sel_f", [128, 64], F32)
    sel_b = p.sb("sel_b", [128, 64], BF16)
    p.dma(sel_f.v(), C['sel'].v())
    p.copy(sel_b.v(), sel_f.v())
    ones_b = p.sb("ones_b", [128, 1], BF16)
    p.memset(ones_b.v(), 1.0)
    stg = p.sb("lstg", [128, 512], F32)
    lwb = p.sb("lwb", [128, 2, 1024], BF16)
    gup0 = p.sb("gup0", [128, 1024], BF16)
    gup1 = p.sb("gup1", [32, 1024], BF16)
    for d_ in range(2):
        for hf in range(2):
            cs_ = slice(hf * 512, (hf + 1) * 512)
            p.dma(stg[0:64, :], dup_d[d_][:, cs_])
            p.dma(stg[64:128, :], iup_d[d_][:, cs_])
            p.copy(lwb[:, d_, cs_], stg.v())
    for hf in range(2):
        cs_ = slice(hf * 512, (hf + 1) * 512)
        p.dma(stg.v(), gup_d[0:128, cs_])
        p.copy(gup0[:, cs_], stg.v())
        p.dma(stg[0:32, :], gup_d[128:160, cs_])
        p.copy(gup1[:, cs_], stg[0:32, :])

    zs = [p.sb("zs%d" % i, [128, SEG + 2], F32) for i in range(2)]
    zcnt = [0]

    def shift_load(r0, m, mucol, seg, dst, p0=0):
        z = zs[zcnt[0] % 2]
        zcnt[0] += 1
        t0 = seg * SEG - 1
        lo = max(t0, 0)
        hi = min(seg * SEG + SEG + 1, T)
        ps_ = slice(p0, p0 + m)
        if seg == 0:
            p.memset(z[ps_, 0:1], 0.0, eng='pool')
        if hi - t0 < SEG + 2:
            p.memset(z[ps_, SEG + 1:SEG + 2], 0.0, eng='pool')
        p.dma(z[ps_, lo - t0:hi - t0], projT[r0:r0 + m, lo:hi], eng='sp')
        p.ts(dst, z[ps_, 1:SEG + 1], mu0[ps_, mucol:mucol + 1], ALU.mult)
        p.stt(dst, z[ps_, 0:SEG], muP[ps_, mucol:mucol + 1], dst, ALU.mult, ALU.add)
        p.stt(dst, z[ps_, 2:SEG + 2], muN[ps_, mucol:mucol + 1], dst, ALU.mult, ALU.add)

    lact = p.sb("lact", [128, T], BF16)
    sgd0 = p.sb("sgd0", [128, T], BF16)
    sgd1 = p.sb("sgd1", [32, T], BF16)
    TA_ = [p.sb("rw_ta%d" % d_, [128, SEG], F32) for d_ in range(2)]
    ltmp = TA_[0]
    for seg in range(4):
        sl = slice(seg * SEG, (seg + 1) * SEG)
        shift_load(6144, 64, 24, seg, ltmp[0:64, :])
        p.act(lact[0:64, sl], ltmp[0:64, :], AF.Tanh)
        shift_load(6208, 64, 25, seg, ltmp[64:128, :], p0=64)
        p.copy(lact[64:128, sl], ltmp[64:128, :], eng='dve')
        shift_load(6272, 128, 26, seg, ltmp[0:128, :])
        p.act(sgd0[:, sl], ltmp[0:128, :], AF.Sigmoid)
        shift_load(6400, 32, 27, seg, ltmp[0:32, :])
        p.act(sgd1[:, sl], ltmp[0:32, :], AF.Sigmoid)

    def f32t(name):
        return p.sb(name, [128, SEG], F32)
    F32S = [{n: f32t("rw_%s%d" % (n, d_)) for n in "zr zk zv kk logw aic kdir bq cf ci tb".split()} for d_ in range(2)]
    SQB = [p.sb("rw_sqb%d" % d_, [128, SEG], BF16) for d_ in range(2)]
    gam = [p.sb("rw_gam%d" % d_, [128, 8], F32) for d_ in range(2)]

    def etile(name):
        t_ = p.sb(name, [128, 8, 128], BF16)
        p.memset(t_.v(), 0.0, eng='pool')
        return t_
    ES = [{n: etile("rw_%s%d" % (n, d_)) for n in "kE bE khE bhE vE zE".split()} for d_ in range(2)]
    for d_ in range(2):
        t_ = p.sb("rw_arE%d" % d_, [128, 8, 256], BF16)
        p.memset(t_.v(), 0.0, eng='pool')
        ES[d_]['arE'] = t_
    CM = p.sb("rw_cmask", [128, 2, 256], F32)
    p.copy(CM[:, 0, 0:128], masks[:, 1, :], eng='pool')
    p.copy(CM[:, 0, 128:256], masks[:, 3, :], eng='pool')
    p.copy(CM[:, 1, 0:128], masks[:, 0, :], eng='pool')
    p.copy(CM[:, 1, 128:256], masks[:, 2, :], eng='pool')
    AW = [p.sb("rw_aW%d" % d_, [128, 8, 128], BF16) for d_ in range(2)]
    VW = [p.sb("rw_vW%d" % d_, [128, 8, 128], BF16) for d_ in range(2)]
    bW = [p.sb("rw_bW%d" % d_, [128, 8, 128], BF16) for d_ in range(2)]
    kW = [p.sb("rw_kW%d" % d_, [128, 8, 128], BF16) for d_ in range(2)]
    vst = [p.sb("rw_vst%d" % d_, [128, 8, 64], BF16) for d_ in range(2)]
    yacc = [p.sb("rw_yacc%d" % i, [128, 8, 64], F32) for i in range(4)]
    vst_all = [p.sb("rw_vstall%d" % i, [128, 8, 64], BF16) for i in range(4)]
    bsc = [p.sb("rw_bsc%d" % i, [128, 8], F32) for i in range(4)]
    Sm = [[p.sb("rw_Sm%d%d" % (d_, i), [128, 64], F32) for i in range(2)] for d_ in range(2)]
    Sb = [[p.sb("rw_Sb%d%d" % (d_, i), [128, 64], BF16) for i in range(2)] for d_ in range(2)]
    scnt = [0, 0]

    GS, NG = 2, 4

    def gt(name, n2=NG, w=128):
        return [[p.sb("rw_%s%d%d" % (name, d_, i), [128, GS, w], BF16) for i in range(n2)] for d_ in range(2)]
    SLb, YLb = [gt(n) for n in "SL YL".split()]
    LMb = gt("LM", w=256)
    KMb = gt("KM", w=256)
    TNb = gt("TN", w=64)
    Lb = [p.sb("rw_L%d" % i, [128, GS, 128], BF16) for i in range(NG)]
    TAb = [p.sb("rw_TA%d" % i, [128, GS, 128], BF16) for i in range(NG)]
    Pb = [[p.sb("rw_P%d%d" % (g_, i), [128, GS, 128], BF16) for i in range(2)] for g_ in range(NG)]
    PTb = [[p.sb("rw_PT%d%d" % (g_, i), [128, GS, 128], BF16) for i in range(2)] for g_ in range(NG)]
    TTb = [[p.sb("rw_TT%d%d" % (g_, i), [128, GS, 128], BF16) for i in range(2)] for g_ in range(NG)]
    Nb = [p.sb("rw_N%d" % i, [128, GS, 64], BF16) for i in range(NG)]
    owide = ES[0]['zE']
    zr, zk, zv = F32S[0]['zr'], F32S[0]['zk'], F32S[0]['zv']
    mst2 = [p.sb("rw_mst%d" % i, [128, SEG], BF16) for i in range(2)]
    fcnt = [0]
    fin_s = p.sb("rw_fs", [128, 8], F32)
    fin_r = p.sb("rw_fr", [128, 8], F32)
    lnw = p.sb("rw_lnw", [128, 64], F32)
    lnb = p.sb("rw_lnb", [128, 64], F32)
    gT = zv
    ev = [0]

    def evac_eng():
        ev[0] += 1
        return 'act' if ev[0] % 2 == 0 else 'dve'

    def c3(v):
        return v.re("p (c i) -> p c i", i=64)

    def prepA(hp, d, seg, first):
        zr, zk, zv, kk, logw, aic, kdir, bq, cf, ci, tb_ = [F32S[d][n] for n in "zr zk zv kk logw aic kdir bq cf ci tb".split()]
        ta = TA_[d]
        sqb = SQB[d]
        kE, bE, khE, bhE, vE, zE = [ES[d][n] for n in "kE bE khE bhE vE zE".split()]
        aE = ES[d]['arE'][:, :, 0:128]
        rE = ES[d]['arE'][:, :, 128:256]
        aW, vW = AW[d], VW[d]
        sl = slice(seg * SEG, (seg + 1) * SEG)
        cols = slice(hp * 128, (hp + 1) * 128)
        shift_load(3072 + hp * 128, 128, hp, seg, zr.v())
        yield
        shift_load(4096 + hp * 128, 128, 8 + hp, seg, zk.v())
        yield
        shift_load(5120 + hp * 128, 128, 16 + hp, seg, zv.v())
        yield
        bk = bank()
        p.mm(bk.v(), lwb[0:64, d, cols], lact[0:64, sl])
        p.act(logw.v(), bk.v(), AF.Sigmoid, bias=w0r[:, d * 8 + hp:d * 8 + hp + 1])
        p.ts(logw.v(), logw.v(), -0.6065306597126334, ALU.mult)
        yield
        bk = bank()
        p.mm(bk.v(), lwb[64:128, d, cols], lact[64:128, sl])
        p.act(aic.v(), bk.v(), AF.Sigmoid, bias=a0r[:, d * 8 + hp:d * 8 + hp + 1])
        yield
        p.ts(kk.v(), zk.v(), kkr[:, hp:hp + 1], ALU.mult)
        p.tt(sqb.v(), kk.v(), kk.v(), ALU.mult, eng='pool')
        yield
        bk = bank()
        p.mm(bk.v(), bones.v(), sqb.v())
        p.ts(ta.v(), bk.v(), 1e-24, ALU.max)
        p.act(ta.v(), ta.v(), AF.Sqrt)
        yield
        p.recip(ta.v(), ta.v())
        p.tt(kk.v(), kk.v(), ta.v(), ALU.mult, eng='pool')
        yield
        p.ts(ta.v(), aic.v(), -1.0, ALU.add, kar[:, hp:hp + 1], ALU.mult)
        p.stt(kdir.v(), ta.v(), 1.0, zk.v(), ALU.add, ALU.mult)
        yield
        p.tt(bq.v(), kk.v(), aic.v(), ALU.mult, eng='pool')
        p.tt(ta.v(), zr.v(), kdir.v(), ALU.mult, eng='pool')
        yield
        for h in range(2):
            ps_ = slice(64 * h, 64 * h + 64)
            p.ts(zE[ps_, :, 64 * h:64 * h + 64], c3(ta[ps_, :]), rkr[ps_, hp:hp + 1], ALU.mult)
        bk = bank()
        for c in range(8):
            p.mm(bk[:, c:c + 1], zE[:, c, :], ones_b.v())
        if first:
            p.copy(bsc[seg].v(), bk[:, 0:8], eng='act')
        else:
            p.tt(bsc[seg].v(), bsc[seg].v(), bk[:, 0:8], ALU.add)
        yield
        p.op('dve', lambda e: e.tensor_tensor_scan(cf.h[:], rmask.h[:], logw.h[:], 0.0, ALU.mult, ALU.add),
             [rmask.v(), logw.v()], [cf.v()])
        tot = c3(cf.v())[:, :, 63:64]
        if d == 0:
            cisrc = cf
        else:
            p.tt(ci.v(), logw.v(), cf.v(), ALU.subtract, eng='pool')
            p.tt(c3(ci.v()), c3(ci.v()), tot.bc([128, 8, 64]), ALU.add, eng='pool')
            cisrc = ci

        def wE(dst, a_, b_, neg=False):
            for h in range(2):
                ps_ = slice(64 * h, 64 * h + 64)
                o = dst[ps_, :, 64 * h:64 * h + 64]
                if neg:
                    p.stt(o, c3(a_[ps_, :]), -1.0, c3(b_[ps_, :]), ALU.mult, ALU.mult)
                else:
                    p.tt(o, c3(a_[ps_, :]), c3(b_[ps_, :]), ALU.mult, eng='pool' if h else 'dve')
        p.act(ta.v(), cisrc.v(), AF.Exp)
        wE(rE, zr, ta)
        yield
        p.act(ta.v(), cisrc.v(), AF.Exp, scale=-1.0)
        wE(kE, kdir, ta)
        yield
        wE(bE, bq, ta)
        yield
        p.tt(tb_.v(), cisrc.v(), logw.v(), ALU.subtract, eng='pool')
        p.act(tb_.v(), tb_.v(), AF.Exp)
        wE(aE, kk, tb_, neg=True)
        yield
        p.tt(c3(tb_.v()), tot.bc([128, 8, 64]), c3(cisrc.v()), ALU.subtract, eng='pool')
        p.act(tb_.v(), tb_.v(), AF.Exp)
        wE(khE, kdir, tb_)
        yield
        wE(bhE, bq, tb_)
        yield
        for h in range(2):
            ps_ = slice(64 * h, 64 * h + 64)
            p.copy(vE[ps_, :, 64 * h:64 * h + 64], c3(zv[ps_, :]), eng='pool')
        yield

    def prepB(d, seg):
        cf = F32S[d]['cf']
        khE, bhE, vE = [ES[d][n] for n in "khE bhE vE".split()]
        aE = ES[d]['arE'][:, :, 0:128]
        aW, vW = AW[d], VW[d]
        tot = c3(cf.v())[:, :, 63:64]
        p.act(gam[d].v().re("p (c o) -> p c o", o=1), tot, AF.Exp)
        for srcE, dstW in ((aE, aW), (bhE, bW[d]), (khE, kW[d]), (vE, vW)):
            hb = hbank()
            for c in range(8):
                p.tr(hb[:, c * 128:(c + 1) * 128], srcE[:, c, :], ident_b.v())
            p.copy(dstW.v(), hb.v().re("p (c r) -> p c r", c=8), eng=evac_eng())
        p.tt(vst[d].v(), vW[:, :, 0:64], vW[:, :, 64:128], ALU.add, eng='pool')
        if d == 0:
            p.copy(vst_all[seg].v(), vst[d].v(), eng='pool')

    def par_group(d, g, gi):
        kE, bE = ES[d]['kE'], ES[d]['bE']
        arE = ES[d]['arE']
        aE = arE[:, :, 0:128]
        rE = arE[:, :, 128:256]
        aW = AW[d]
        LM, KM = LMb[d][gi], KMb[d][gi]
        LT, MrbT = LM[:, :, 0:128], LM[:, :, 128:256]
        LakT, MrkT = KM[:, :, 0:128], KM[:, :, 128:256]

        def mm2(dst, lhsE):
            bk_ = bank()
            for n in range(GS):
                p.mm(bk_[:, n * 256:(n + 1) * 256], lhsE[:, g * GS + n, :], arE[:, g * GS + n, :])
            p.tt(dst.v(), bk_[:, 0:GS * 256].re("p (n r) -> p n r", n=GS),
                 CM[:, d:d + 1, :].bc([128, GS, 256]), ALU.mult)
        MS, MST, MIT = (0, 1, 3) if d == 0 else (1, 0, 2)

        def mmg(lf, rf, n_out=128):
            bk_ = bank()
            for n in range(GS):
                p.mm(bk_[:, n * n_out:(n + 1) * n_out], lf(n), rf(n))
            return bk_

        def Ec(tl):
            return lambda n, tl=tl: tl[:, g * GS + n, :]

        def Gc(tl):
            return lambda n, tl=tl: tl[:, n, :]

        def b4(bk_):
            return bk_[:, 0:GS * 128].re("p (n r) -> p n r", n=GS)

        def masked(dst, bk_, mi):
            p.tt(dst.v(), b4(bk_), masks[:, mi:mi + 1, :].bc([128, GS, 128]), ALU.mult)

        bk_ = mmg(Ec(aE), Ec(bE))
        masked(Lb[g], bk_, MS)
        mm2(LM, bE)
        tt_ = TTb[g][0]
        p.tt(tt_.v(), LT, ident_f.v().re("p (o r) -> p o r", o=1).bc([128, GS, 128]), ALU.add, eng='pool')
        yield
        P_, PT_ = Lb[g], LT
        for it in range(5):
            bk_ = mmg(Gc(PT_), Gc(P_))
            P2 = Pb[g][it % 2]
            p.copy(P2.v(), b4(bk_), eng='act')
            if it < 4:
                bk_ = mmg(Gc(P_), Gc(PT_))
                PT2 = PTb[g][it % 2]
                p.copy(PT2.v(), b4(bk_), eng='act')
            yield
            bk_ = mmg(Gc(P2), Gc(tt_))
            ttn = TTb[g][(it + 1) % 2]
            p.tt(ttn.v(), b4(bk_), tt_.v(), ALU.add)
            tt_ = ttn
            P_ = P2
            if it < 4:
                PT_ = PT2
            yield
        mm2(KM, kE)
        bk_ = mmg(Gc(tt_), Ec(aW))
        p.copy(TAb[g].v(), b4(bk_), eng='act')
        yield
        bk_ = mmg(Gc(LakT), lambda n: vst[d][:, g * GS + n, :], n_out=64)
        p.copy(Nb[g].v(), bk_[:, 0:GS * 64].re("p (n r) -> p n r", n=GS), eng='act')
        bk_ = mmg(Gc(TAb[g]), Ec(bW[d]))
        p.copy(SLb[d][gi].v(), b4(bk_), eng='act')
        bk_ = mmg(Gc(TAb[g]), Gc(MrbT))
        p.tt(YLb[d][gi].v(), b4(bk_), rE[:, g * GS:(g + 1) * GS, :], ALU.add)
        yield
        bk_ = mmg(Gc(tt_), Gc(Nb[g]), n_out=64)
        p.copy(TNb[d][gi].v(), bk_[:, 0:GS * 64].re("p (n r) -> p n r", n=GS), eng='act')
        yield

    def unit_parallel(d, gbase, extra=()):
        gens = [par_group(d, g, g) for g in range(NG)] + list(extra)
        alive = list(gens)
        while alive:
            for gen in list(alive):
                try:
                    next(gen)
                except StopIteration:
                    alive.remove(gen)
        return {g: g for g in range(NG)}

    def seq_steps(d, seg, G, first):
        order = range(8) if d == 0 else range(7, -1, -1)
        steps = []
        for c in order:
            def step(c=c):
                g = c // GS
                n = c % GS
                gi = G[g]
                k_ = scnt[d]
                scnt[d] += 1
                s_old, s_new = Sm[d][k_ % 2], Sm[d][(k_ + 1) % 2]
                b_old, b_new = Sb[d][k_ % 2], Sb[d][(k_ + 1) % 2]
                bs = bank()
                p.mm(bs[:, 0:64], bW[d][:, c, :], TNb[d][gi][:, n, :], start=True, stop=False)
                p.mm(bs[:, 0:64], kW[d][:, c, :], vst[d][:, c, :], start=False, stop=False)
                p.mm(bs[:, 0:64], SLb[d][gi][:, n, :], b_old.v(), start=False, stop=True)
                by = bank()
                p.mm(by[:, 0:64], LMb[d][gi][:, n, 128:256], TNb[d][gi][:, n, :], start=True, stop=False)
                p.mm(by[:, 0:64], KMb[d][gi][:, n, 128:256], vst[d][:, c, :], start=False, stop=False)
                p.mm(by[:, 0:64], YLb[d][gi][:, n, :], b_old.v(), start=False, stop=True)
                p.stt(b_new.v(), s_old.v(), gam[d][:, c:c + 1], bs[:, 0:64], ALU.mult, ALU.add)
                p.stt(s_new.v(), s_old.v(), gam[d][:, c:c + 1], bs[:, 0:64], ALU.mult, ALU.add)
                if first:
                    p.copy(yacc[seg][:, c, :], by[:, 0:64], eng='act')
                else:
                    p.tt(yacc[seg][:, c, :], yacc[seg][:, c, :], by[:, 0:64], ALU.add, eng='dve')
            steps.append(step)
        return steps

    def finalize(hp):
        FA = c3(zr.v())
        FB = c3(zk.v())
        p.dma(lnw.v(), lnw_d[hp])
        p.dma(lnb.v(), lnb_d[hp])
        cols = slice(hp * 128, (hp + 1) * 128)
        for seg in range(4):
            sl = slice(seg * SEG, (seg + 1) * SEG)
            cs = slice(seg * 8, (seg + 1) * 8)
            y = yacc[seg].v()
            p.red(fin_s.v(), y, ALU.add)
            p.ts(fin_s.v(), fin_s.v(), 1.0 / 64, ALU.mult)
            p.tt(FA, y, fin_s.v().re("p (c o) -> p c o", o=1).bc([128, 8, 64]), ALU.subtract)
            p.tt(FB, FA, FA, ALU.mult, eng='pool')
            p.red(fin_r.v(), FB, ALU.add)
            p.rsqrt(fin_r.v(), fin_r.v(), 1.0 / 64, 64e-5)
            p.tt(FA, FA, fin_r.v().re("p (c o) -> p c o", o=1).bc([128, 8, 64]), ALU.mult)
            p.tt(FA, FA, lnw.v().re("p (o v) -> p o v", o=1).bc([128, 8, 64]), ALU.mult)
            p.tt(FA, FA, lnb.v().re("p (o v) -> p o v", o=1).bc([128, 8, 64]), ALU.add)
            p.tt(FB, vst_all[seg].v(), bsc[seg].v().re("p (c o) -> p c o", o=1).bc([128, 8, 64]), ALU.mult, eng='pool')
            p.tt(FA, FA, FB, ALU.add)
            for h in range(2):
                ps_ = slice(64 * h, 64 * h + 64)
                p.copy(owide[ps_, :, 64 * h:64 * h + 64], c3(zr[ps_, :]), eng='pool' if h else 'dve')
            bk = bank()
            for c in range(8):
                p.mm(bk[:, c * 64:(c + 1) * 64], owide[:, c, :], sel_b.v())
            bg = bank()
            p.mm(bg.v(), gup0[:, cols], sgd0[:, sl], start=True, stop=False)
            p.mm(bg.v(), gup1[:, cols], sgd1[:, sl], start=False, stop=True)
            p.copy(gT.v(), bg.v(), eng='act')
            ms_ = mst2[fcnt[0] % 2]
            fcnt[0] += 1
            p.tt(ms_.v(), bk.v(), gT.v(), ALU.mult)
            p.dma(mix_d[:, 8 + hp, sl], ms_.v(), eng='pool')

    NHP = L.get('NHP', 8)
    gb = 0

    def drain(gen):
        for _ in gen:
            pass
    for hp in range(NHP):
        for d in range(2):
            p.memset(Sm[d][scnt[d] % 2].v(), 0.0)
            p.memset(Sb[d][scnt[d] % 2].v(), 0.0)
        U = []
        for s_ in range(4):
            U.append((0, s_, s_ < 2))
            U.append((1, 3 - s_, s_ < 2))
        drain(prepA(hp, U[0][0], U[0][1], U[0][2]))
        prepB(U[0][0], U[0][1])
        Gs = {}
        for k, (d, seg, first) in enumerate(U):
            nxt = U[k + 1] if k + 1 < len(U) else None
            extra = [prepA(hp, nxt[0], nxt[1], nxt[2])] if nxt else []
            Gs[d] = unit_parallel(d, gb, extra)
            if k % 2 == 1:
                gb += 1
                st0 = seq_steps(0, U[k - 1][1], Gs[0], first)
                st1 = seq_steps(1, seg, Gs[1], first)
                for a_, b_ in zip(st0, st1):
                    a_()
                    b_()
            if nxt:
                prepB(nxt[0], nxt[1])
        finalize(hp)
    p.pop()
def tail_stage(p, L):
    IN = L['IN']
    C = L['C']
    PB = L['PB']
    bank = L['bank']
    hbank = L['hbank']
    ident_b = L['ident_b']
    ident_f = L['ident_f']
    mix_d = L['mix_d']
    mixT = p.sb("mixT", [128, 16, T], BF16)
    x = L['x']
    out = L['out']
    scr = L['scr']
    scr2 = L['scr2']
    projT = L['projT']
    NE = L.get('NE', 16)
    w_out = IN("w_out", [D, D])
    nfw = IN("nfw", [1, D])
    n2w = IN("n2w", [1, D])
    wr_d = IN("w_router", [D, 16])
    eg_d = IN("e_gate", [16, D, D])
    eu_d = IN("e_up", [16, D, D])
    ed_d = IN("e_down", [16, D, D])
    xmid_d = p.dram("xmid_d", [T, D], F32)
    h2_d = p.dram("h2_d", [T, D], BF16)
    ye_d = p.dram("ye_d", [16, 2, 128, D], BF16)
    aff = p.sb("aff", [128, 16, 16], F32)
    valT = p.sb("valT", [128, 16, 16], F32)
    iota1 = p.sb("iota1", [128, 256], F32)
    p.dma(iota1.v(), C['iota1'].v())
    idxs = p.sb("idxs", [128, 32], F32)
    iop_f = p.sb("iop_f", [128, 1], F32)
    p.dma(iop_f.v(), C['iota_p'].v())

    p.push()
    for kc in range(16):
        p.dma(mixT[:, kc, :], mix_d[:, kc, :], eng='sp')
    wo = p.sb("wo", [128, 16, D], BF16)
    for kc in range(16):
        p.dma(wo[:, kc, :], w_out[kc * 128:(kc + 1) * 128, :], eng='pool')
    n2w_s = p.sb("n2w_s", [128, D], F32)
    p.dma(n2w_s.v(), n2w[0:1, :].bc([128, D]))
    wr = p.sb("wr", [128, 16, 16], F32)
    p.dma(wr.v(), wr_d.v().re("(k p) e -> p k e", p=128))
    xt2 = [p.sb("xt2_%d" % i, [128, D], F32) for i in range(2)]
    xm = [p.sb("xm%d" % i, [128, D], F32) for i in range(2)]
    h2f = p.sb("h2f", [128, D], F32)
    h2b = [p.sb("h2b0", [128, D], BF16)] * 2
    h2T = p.sb("h2T", [128, 16, 128], F32)
    ss2 = p.sb("ss2", [128, NT], F32)
    mx = p.sb("mx", [128, NT], F32)
    sm = p.sb("sm", [128, NT], F32)
    def stA(i):
        xa = xt2[i % 2]
        xo = xm[i % 2]
        p.dma(xa.v(), x[i * 128:(i + 1) * 128, :], eng='sp')
        for db in range(4):
            bk = bank()
            for kc in range(16):
                p.mm(bk.v(), mixT[:, kc, i * 128:(i + 1) * 128], wo[:, kc, db * 512:(db + 1) * 512],
                     start=(kc == 0), stop=(kc == 15))
            p.tt(xo[:, db * 512:(db + 1) * 512], bk.v(), xa[:, db * 512:(db + 1) * 512], ALU.add)
        p.dma(xmid_d[i * 128:(i + 1) * 128, :], xo.v(), eng='pool')

    def stB(i):
        xo = xm[i % 2]
        hb_ = h2b[i % 2]
        p.act(h2f.v(), xo.v(), AF.Square, accum=ss2[:, i:i + 1])
        p.rsqrt(ss2[:, i:i + 1], ss2[:, i:i + 1], 1.0 / D, EPS)
        p.ts(h2f.v(), xo.v(), ss2[:, i:i + 1], ALU.mult)
        p.tt(h2f.v(), h2f.v(), n2w_s.v(), ALU.mult)
        p.copy(hb_.v(), h2f.v(), eng='act')
        p.dma(h2_d[i * 128:(i + 1) * 128, :], hb_.v(), eng='pool')

    def stC(i):
        for q in range(4):
            bk = bank()
            for j in range(4):
                kc = q * 4 + j
                p.tr(bk[:, j * 128:(j + 1) * 128], h2f[:, kc * 128:(kc + 1) * 128], ident_f.v())
            p.copy(h2T[:, q * 4:(q + 1) * 4, :], bk.v().re("p (j t) -> p j t", j=4), eng='act' if q % 2 else 'dve')
        bk = bank()
        for kc in range(16):
            p.mm(bk[:, 0:16], h2T[:, kc, :], wr[:, kc, :], start=(kc == 0), stop=(kc == 15))
        p.red(mx[:, i:i + 1], bk[:, 0:16], ALU.max)
        p.ts(mx[:, i:i + 1], mx[:, i:i + 1], -1.0, ALU.mult)
        p.act(aff[:, i, :], bk[:, 0:16], AF.Exp, bias=mx[:, i:i + 1], accum=sm[:, i:i + 1])
        p.recip(sm[:, i:i + 1], sm[:, i:i + 1])
        p.ts(aff[:, i, :], aff[:, i, :], sm[:, i:i + 1], ALU.mult)

    stA(0)
    for i in range(NT):
        if i + 1 < NT:
            stA(i + 1)
        stB(i)
        stC(i)
    p.pop()

    p.push()
    for i in range(NT):
        p.dma(mixT[:, i, :], h2_d[i * 128:(i + 1) * 128, :], eng='sp' if i % 2 else 'pool')
    affT = p.sb("affT", [16, T], F32)
    work = p.sb("work", [16, T], F32)
    maskT = p.sb("maskT", [16, T], F32)
    onesT = p.sb("onesT", [16, T], F32)
    m8 = p.sb("m8", [16, 8], F32)
    p.memset(onesT.v(), 1.0, eng='pool')
    for q in range(4):
        bk = bank()
        for j in range(4):
            i = q * 4 + j
            p.tr(bk[0:16, j * 128:(j + 1) * 128], aff[:, i, :], ident_f.v())
        p.copy(affT[:, q * 512:(q + 1) * 512], bk[0:16, :], eng='dve')
    p.copy(work.v(), affT.v(), eng='dve')
    for r_ in range(32):
        p.op('dve', lambda e: e.max(out=m8.h[:], in_=work.h[:]), [work.v()], [m8.v()])
        if r_ < 31:
            p.op('dve', lambda e: e.match_replace(out=work.h[:], in_to_replace=m8.h[:], in_values=work.h[:],
                                                  imm_value=-1.0), [work.v(), m8.v()], [work.v()])
    p.ts(maskT.v(), affT.v(), m8[:, 7:8], ALU.is_ge)
    p.op('dve', lambda e: e.tensor_tensor_scan(work.h[:], onesT.h[:], maskT.h[:], 0.0, ALU.mult, ALU.add),
         [onesT.v(), maskT.v()], [work.v()])
    p.tt(work.v(), work.v(), maskT.v(), ALU.mult)
    bk = bank()
    for i in range(16):
        p.tr(bk[:, i * 16:(i + 1) * 16], work[:, i * 128:(i + 1) * 128], ident_f[0:16, 0:16])
    p.copy(valT.v(), bk[:, 0:256].re("p (i e) -> p i e", i=16), eng='dve')
    p.pop()

    p.push()
    h2s = mixT
    oh = p.sb("oh", [128, 16, 256], BF16)
    gm = p.sb("gm", [128, 16, 5], BF16)
    for i in range(16):
        p.memset(gm[:, i, 3:4], float(i))
        p.copy(gm[:, i, 4:5], iop_f.v())
    g1 = p.sb("g1", [128, 16], F32)
    g2 = p.sb("g2", [128, 16], F32)
    gb = p.sb("gb", [128, 16], BF16)
    gate = p.sb("gate", [128, 2], F32)
    g3 = p.sb("g3", [128, 2, 5], F32)
    xeT = p.sb("xeT", [128, 16, 256], BF16)
    hidT = oh
    ye = p.sb("ye", [128, 2, D], BF16)
    wb = [p.sb("ewb%d" % i, [128, 16, 512], BF16) for i in range(5)]
    sg = [p.sb("sg%d" % i, [128, 256], F32) for i in range(2)]
    wc = [0]

    def wload(src, e, cb):
        k = wc[0]
        wc[0] += 1
        b = wb[k % 5]
        p.dma(b.v(), src[e][:, cb * 512:(cb + 1) * 512].re("(k p) c -> p k c", p=128), eng='pool')
        return b

    iota_bc = iota1.v().re("p (o s) -> p o s", o=1).bc([128, 16, 256])
    for e in range(NE):
        p.tt(oh.v(), iota_bc, valT[:, :, e:e + 1].bc([128, 16, 256]), ALU.is_equal)
        p.copy(gb.v(), aff[:, :, e])
        p.copy(gm[:, :, 0], gb.v())
        p.tt(g1.v(), aff[:, :, e], gb.v(), ALU.subtract)
        p.copy(gb.v(), g1.v())
        p.copy(gm[:, :, 1], gb.v())
        p.tt(g2.v(), g1.v(), gb.v(), ALU.subtract)
        p.copy(gm[:, :, 2], g2.v())
        bk = bank()
        for sh in range(2):
            for i in range(16):
                p.mm(bk[:, sh * 8:sh * 8 + 5], oh[:, i, sh * 128:(sh + 1) * 128], gm[:, i, :],
                     start=(i == 0), stop=(i == 15))
        p.copy(g3.v(), bk[:, 0:16].re("p (a c) -> p a c", a=2)[:, :, 0:5])
        p.red(gate.v(), g3[:, :, 0:3], ALU.add)
        p.stt(idxs[:, 2 * e:2 * e + 2], g3[:, :, 3], 128.0, g3[:, :, 4], ALU.mult, ALU.add)
        for dq in range(8):
            bk = bank()
            for j in range(2):
                dc = dq * 2 + j
                for i in range(16):
                    p.mm(bk[:, j * 256:(j + 1) * 256], h2s[:, i, dc * 128:(dc + 1) * 128], oh[:, i, :],
                         start=(i == 0), stop=(i == 15))
            p.copy(xeT[:, dq * 2:dq * 2 + 2, :], bk.v().re("p (j s) -> p j s", j=2), eng='act' if dq % 2 else 'dve')
        for fb in range(4):
            wg = wload(eg_d, e, fb)
            wu = wload(eu_d, e, fb)
            for fc2 in range(4):
                fc = fb * 4 + fc2
                bg = bank()
                for dc in range(16):
                    p.mm(bg[:, 0:256], wg[:, dc, fc2 * 128:(fc2 + 1) * 128], xeT[:, dc, :],
                         start=(dc == 0), stop=(dc == 15))
                bu = bank()
                for dc in range(16):
                    p.mm(bu[:, 0:256], wu[:, dc, fc2 * 128:(fc2 + 1) * 128], xeT[:, dc, :],
                         start=(dc == 0), stop=(dc == 15))
                s_ = sg[fc % 2]
                p.act(s_.v(), bg[:, 0:256], AF.Silu)
                p.tt(hidT[:, fc, :], s_.v(), bu[:, 0:256], ALU.mult)
        for db in range(4):
            wd = wload(ed_d, e, db)
            for sh in range(2):
                bk = bank()
                for fc in range(16):
                    p.mm(bk.v(), hidT[:, fc, sh * 128:(sh + 1) * 128], wd[:, fc, :],
                         start=(fc == 0), stop=(fc == 15))
                p.ts(ye[:, sh, db * 512:(db + 1) * 512], bk.v(), gate[:, sh:sh + 1], ALU.mult)
        p.dma(ye_d[e].re("s p d -> p s d"), ye.v(), eng='sp')
    p.pop()

    p.push()
    yeh = p.sb("yeh", [128, 2 * NE, 1024], BF16)
    idm = p.sb("idm", [128, 32], F32)
    GT = p.sb("GT", [128, 32, 128], BF16)
    xr = [p.sb("xr%d" % i, [128, 1024], F32) for i in range(2)]
    iota_bc2 = iota1.v().re("p (o s) -> p o s", o=1).bc([128, 16, 256])
    for half in range(2):
        hs = slice(half * 1024, (half + 1) * 1024)
        for e in range(NE):
            p.dma(yeh[:, 2 * e:2 * e + 2, :], ye_d[e][:, :, hs].re("s p d -> p s d"), eng='sp' if e % 2 else 'pool')
        for i in range(NT):
            p.ts(idm.v(), idxs.v(), float(1 - 128 * i), ALU.add)
            p.tt(GT.v(), iota1[:, 0:128].re("p (o t) -> p o t", o=1).bc([128, 32, 128]),
                 idm.v().re("p (k o) -> p k o", o=1).bc([128, 32, 128]), ALU.is_equal)
            xa = xr[i % 2]
            p.dma(xa.v(), xmid_d[i * 128:(i + 1) * 128, hs], eng='sp')
            for db in range(2):
                bk = bank()
                for k in range(2 * NE):
                    p.mm(bk.v(), GT[:, k, :], yeh[:, k, db * 512:(db + 1) * 512], start=(k == 0), stop=(k == 2 * NE - 1))
                p.tt(xa[:, db * 512:(db + 1) * 512], xa[:, db * 512:(db + 1) * 512], bk.v(), ALU.add)
            p.dma(xmid_d[i * 128:(i + 1) * 128, hs], xa.v(), eng='pool')
    p.pop()

    p.push()
    nfw_s = p.sb("nfw_s", [128, D], F32)
    p.dma(nfw_s.v(), nfw[0:1, :].bc([128, D]))
    xf = [p.sb("xf%d" % i, [128, D], F32) for i in range(2)]
    junk2 = p.sb("junk2", [128, D], F32)
    ss3 = p.sb("ss3", [128, NT], F32)
    for i in range(NT):
        xo = xf[i % 2]
        p.dma(xo.v(), xmid_d[i * 128:(i + 1) * 128, :], eng='sp')
        p.act(junk2.v(), xo.v(), AF.Square, accum=ss3[:, i:i + 1])
        p.rsqrt(ss3[:, i:i + 1], ss3[:, i:i + 1], 1.0 / D, EPS)
        p.ts(xo.v(), xo.v(), ss3[:, i:i + 1], ALU.mult)
        p.tt(xo.v(), xo.v(), nfw_s.v(), ALU.mult)
        p.dma(out[i * 128:(i + 1) * 128, :], xo.v(), eng='pool')
    p.dma(scr2.v(), projT[0:1, 0:16])
    p.finish([out.v()], scr.v(), scr2.v())
    p.pop()
```

```python
import numpy as np
import ml_dtypes
from contextlib import ExitStack
import concourse.bass as bass
import concourse.mybir as mybir
from concourse.bass_utils import run_bass_kernel_spmd

F32 = mybir.dt.float32
BF16 = mybir.dt.bfloat16
I32 = mybir.dt.int32
ALU = mybir.AluOpType
AF = mybir.ActivationFunctionType
AX = mybir.AxisListType
ENG = ['pe', 'act', 'dve', 'pool', 'sp']
NDS = 24


class V:
    def __init__(s, tile, ap):
        s.tile = tile
        s.ap = ap

    def __getitem__(s, k):
        return V(s.tile, s.ap[k])

    def re(s, pat, **kw):
        return V(s.tile, s.ap.rearrange(pat, **kw))

    def bc(s, shape):
        return V(s.tile, s.ap.to_broadcast(shape))

    def bitcast(s, dt):
        return V(s.tile, s.ap.bitcast(dt))


class Tile:
    def __init__(s, h, name):
        s.h = h
        s.name = name
        s.w = None
        s.r = {}

    def __getitem__(s, k):
        return V(s, s.h[k])

    def v(s):
        return V(s, s.h[:])


class Prog:
    def __init__(s, nc, es):
        s.nc = nc
        s.es = es
        s.q = {e: [] for e in ENG}
        s.cnt = {e: 0 for e in ENG}
        s.sems = {e: es.enter_context(nc.semaphore("s_" + e)) for e in ENG}
        s.dsems = [es.enter_context(nc.semaphore("d%d" % i)) for i in range(NDS)]
        s.dcnt = [0] * NDS
        s.dnext = 0
        s.seen = {e: {} for e in ENG}
        s.n = 0

    def semof(s, k):
        return s.dsems[k[1]] if isinstance(k, tuple) else s.sems[k]

    def sb(s, name, shape, dt):
        return Tile(s.es.enter_context(s.nc.sbuf_tensor(name, list(shape), dt)), name)

    def ps(s, name, shape, dt):
        return Tile(s.es.enter_context(s.nc.psum_tensor(name, list(shape), dt)), name)

    def dram(s, name, shape, dt, kind="Internal"):
        return Tile(s.nc.dram_tensor(name, list(shape), dt, kind=kind), name)

    def op(s, eng, fn, reads, writes, dma=False, acc=False):
        waits = {}

        def need(k):
            if k is None:
                return
            waits[k[0]] = max(waits.get(k[0], 0), k[1])

        for v in reads:
            need(v.tile.w)
        for v in writes:
            t = v.tile
            if not (acc and t.w is not None and t.w[0] == 'pe'):
                need(t.w)
            for k, val in t.r.items():
                need((k, val))
        if dma:
            i = s.dnext
            s.dnext = (i + 1) % NDS
            key = ('d', i)
            need((key, s.dcnt[i]))
            s.dcnt[i] += 16
            val = s.dcnt[i]
            inc = 16
        else:
            key = eng
            s.cnt[eng] += 1
            val = s.cnt[eng]
            inc = 1
        wl = []
        for k, v in waits.items():
            if v <= 0 or s.seen[eng].get(k, 0) >= v:
                continue
            s.seen[eng][k] = v
            wl.append((k, v))
        s.q[eng].append((wl, fn, key, inc))
        s.n += 1
        for v in reads:
            v.tile.r[key] = max(v.tile.r.get(key, 0), val)
        for v in writes:
            v.tile.w = (key, val)
            v.tile.r = {}

    def dma(s, out, in_, eng='sp', **kw):
        s.op(eng, lambda e: e.dma_start(out=out.ap, in_=in_.ap, **kw), [in_], [out], dma=True)

    def mm(s, out, lhsT, rhs, start=True, stop=True):
        s.op('pe', lambda e: e.matmul(out.ap, lhsT.ap, rhs.ap, start=start, stop=stop),
             [lhsT, rhs], [out], acc=not start)

    def tr(s, out, in_, ident):
        s.op('pe', lambda e: e.transpose(out.ap, in_.ap, ident.ap), [in_, ident], [out])

    def act(s, out, in_, func, bias=None, scale=None, accum=None):
        kw = {}
        rd = [in_]
        wr = [out]
        if bias is not None:
            if isinstance(bias, V):
                kw['bias'] = bias.ap
                rd.append(bias)
            else:
                kw['bias'] = bias
        if scale is not None:
            if isinstance(scale, V):
                kw['scale'] = scale.ap
                rd.append(scale)
            else:
                kw['scale'] = scale
        if accum is not None:
            kw['accum_out'] = accum.ap
            wr.append(accum)
        s.op('act', lambda e: e.activation(out.ap, in_.ap, func, **kw), rd, wr)

    def tt(s, out, a, b, op, eng='dve'):
        s.op(eng, lambda e: e.tensor_tensor(out.ap, a.ap, b.ap, op), [a, b], [out])

    def ts(s, out, a, s1, op0, s2=None, op1=None, eng='dve', accum=None):
        rd = [a]
        wr = [out]
        a1 = s1
        a2 = s2
        if isinstance(s1, V):
            rd.append(s1)
            a1 = s1.ap
        if isinstance(s2, V):
            rd.append(s2)
            a2 = s2.ap
        kw = {}
        if op1 is not None:
            kw['op1'] = op1
        if accum is not None:
            kw['accum_out'] = accum.ap
            wr.append(accum)
        s.op(eng, lambda e: e.tensor_scalar(out.ap, a.ap, a1, a2, op0, **kw), rd, wr)

    def stt(s, out, a, sc, b, op0, op1, eng='dve'):
        rd = [a, b]
        a1 = sc
        if isinstance(sc, V):
            rd.append(sc)
            a1 = sc.ap
        s.op(eng, lambda e: e.scalar_tensor_tensor(out.ap, a.ap, a1, b.ap, op0, op1), rd, [out])

    def copy(s, out, in_, eng='dve'):
        if eng == 'act':
            s.op('act', lambda e: e.copy(out.ap, in_.ap), [in_], [out])
        else:
            s.op(eng, lambda e: e.tensor_copy(out.ap, in_.ap), [in_], [out])

    def memset(s, out, val, eng='dve'):
        s.op(eng, lambda e: e.memset(out.ap, val), [], [out])

    def red(s, out, in_, op, axis=None, eng='dve'):
        ax = AX.X if axis is None else axis
        s.op(eng, lambda e: e.tensor_reduce(out.ap, in_.ap, ax, op), [in_], [out])

    def recip(s, out, in_):
        s.op('dve', lambda e: e.reciprocal(out.ap, in_.ap), [in_], [out])

    def rsqrt(s, out, in_, mul, add):
        s.ts(out, in_, mul, ALU.mult, add, ALU.add)
        s.act(out, out, AF.Sqrt)
        s.recip(out, out)

    def push(s):
        s._outer = s.es
        s._inner = ExitStack()
        s.es = s._inner

    def pop(s):
        targets = [(e, s.cnt[e]) for e in ENG] + [(('d', i), s.dcnt[i]) for i in range(NDS)]
        for e in ENG:
            wl = []
            for k, v in targets:
                if v <= 0 or s.seen[e].get(k, 0) >= v:
                    continue
                s.seen[e][k] = v
                wl.append((k, v))
            s.q[e].append((wl, None, None, 0))
        s.emit()
        s._inner.close()
        s.es = s._outer

    def coll(s, kind, op, ins, outs, groups):
        s.op('pool', lambda e: e.collective_compute(kind, op, replica_groups=groups,
                                                    ins=[i.ap for i in ins], outs=[o.ap for o in outs]),
             ins, outs, dma=True)

    def finish(s, outs, scratch_dst, scratch_src):
        s.op('sp', lambda e: e.dma_start(out=scratch_dst.ap, in_=scratch_src.ap), list(outs) + [scratch_src],
             [scratch_dst], dma=True)
        k, val = scratch_dst.tile.w
        s.q['sp'].append(([(k, val)], None, None, 0))

    def emit(s):
        nc = s.nc
        qs = s.q
        s.q = {e: [] for e in ENG}
        with nc.Block() as block:
            s._emit_block(block, qs)

    def _emit_block(s, block, qs):
        waited = set()
        for e in ENG:
            for wl, fn, key, inc in qs[e]:
                for k, v in wl:
                    waited.add((k, v))
        if not hasattr(s, 'sig'):
            s.sig = {e: 0 for e in ENG}
            s.pos = {e: 0 for e in ENG}
            s.remap = {e: {0: 0} for e in ENG}
        plan = {}
        for e in ENG:
            pos = s.pos[e]
            sig = s.sig[e]
            flags = []
            for wl, fn, key, inc in qs[e]:
                if fn is None or isinstance(key, tuple):
                    flags.append(False)
                    continue
                pos += 1
                if (e, pos) in waited:
                    sig += 1
                    s.remap[e][pos] = sig
                    flags.append(True)
                else:
                    flags.append(False)
            s.pos[e] = pos
            s.sig[e] = sig
            plan[e] = flags

        def mk(e):
            def body(eng):
                for (wl, fn, key, inc), flag in zip(qs[e], plan[e]):
                    for k, v in wl:
                        if isinstance(k, tuple):
                            eng.wait_ge(s.semof(k), v)
                        else:
                            eng.wait_ge(s.semof(k), s.remap[k][v])
                    if fn is None:
                        continue
                    ins = fn(eng)
                    if isinstance(key, tuple):
                        ins.then_inc(s.semof(key), inc)
                    elif flag:
                        ins.then_inc(s.semof(key), 1)
            return body

        block.tensor(mk('pe'))
        block.scalar(mk('act'))
        block.vector(mk('dve'))
        block.gpsimd(mk('pool'))
        block.sync(mk('sp'))
D = 2048
T = 2048
NT = 16
NCOL = 6432
NCH = 51
EPS = 1e-6


def consts_np():
    c = {}
    c['ident_f'] = np.eye(128, dtype=np.float32)
    rho = np.arange(128)
    hh = rho // 64
    ii = rho % 64
    same = (hh[:, None] == hh[None, :])
    SL = (same & (ii[None, :] < ii[:, None])).astype(np.float32)
    IL = (same & (ii[None, :] <= ii[:, None])).astype(np.float32)
    masks = np.stack([SL, SL.T, IL, IL.T], 0)
    c['masks'] = np.ascontiguousarray(np.tile(masks[:, :, None, :], (1, 1, 4, 1)).reshape(4, 128, 512))
    rm = np.ones((128, T), np.float32)
    rm[:, ::64] = 0.0
    c['resetmask'] = rm
    c['iota1'] = np.tile(np.arange(1, 257, dtype=np.float32)[None, :], (128, 1))
    invf = np.zeros((128, 1), np.float32)
    PT = np.zeros((128, 128), np.float32)
    for cc in range(2):
        for j in range(8):
            f = 500000.0 ** (-(2.0 * j) / 16.0)
            p1 = cc * 64 + j
            p2 = cc * 64 + 8 + j
            invf[p1, 0] = f
            invf[p2, 0] = f
            PT[p2, p1] = -1.0
            PT[p1, p2] = 1.0
    c['invf'] = invf
    c['PT'] = PT
    c['blockones'] = same.astype(np.float32)
    sel = np.zeros((128, 64), np.float32)
    sel[rho, ii] = 1.0
    c['sel'] = sel
    c['iota_p'] = np.arange(128, dtype=np.float32).reshape(128, 1)
    return c


def build(stage=99):
    nc = bass.Bass("TRN2", target_bir_lowering=False)
    es = ExitStack()
    dbg = {}
    with es:
        p = Prog(nc, es)
        IN = lambda n, s, dt=F32: p.dram(n, s, dt, kind="ExternalInput")
        x = IN("x", [T, D])
        pos = IN("pos", [1, T], I32)
        n1w = IN("n1w", [128, 16])
        w_in = IN("w_in", [D, NCOL])
        out = p.dram("out", [T, D], F32, kind="ExternalOutput")
        projT = p.dram("projT", [NCH * 128, T], F32, kind=("ExternalOutput" if stage == 1 else "Internal"))
        scr = p.dram("scr", [1, 16], F32)
        scr2 = p.dram("scr2", [1, 16], F32)
        cn = consts_np()
        C = {k: IN("c_" + k, list(v.shape)) for k, v in cn.items()}

        ident_f = p.sb("ident_f", [128, 128], F32)
        ident_b = p.sb("ident_b", [128, 128], BF16)
        p.dma(ident_f.v(), C['ident_f'].v())
        p.copy(ident_b.v(), ident_f.v())
        n1w_s = p.sb("n1w_s", [128, 16], F32)
        p.dma(n1w_s.v(), n1w.v())

        PB = [p.ps("pb%d" % i, [128, 512], F32) for i in range(6)]
        PH = [p.ps("ph%d" % i, [128, 1024], BF16) for i in range(2)]
        st = {'pb': 0, 'ph': 0}

        def bank():
            st['pb'] = (st['pb'] + 1) % 6
            return PB[st['pb']]

        def hbank():
            st['ph'] = (st['ph'] + 1) % 2
            return PH[st['ph']]

        p.push()
        hT = p.sb("hT", [128, 16, T], BF16)
        xt = [p.sb("xt%d" % i, [128, D], F32) for i in range(2)]
        xn = [p.sb("xn%d" % i, [128, D], BF16) for i in range(2)]
        junk = p.sb("junk", [128, D], F32)
        ssq = p.sb("ssq", [128, NT], F32)
        rstd = p.sb("rstd", [128, NT], F32)
        for i in range(NT):
            a = xt[i % 2]
            b = xn[i % 2]
            p.dma(a.v(), x[i * 128:(i + 1) * 128, :], eng='sp' if i % 2 == 0 else 'pool')
            p.act(junk.v(), a.v(), AF.Square, accum=ssq[:, i:i + 1])
            p.rsqrt(rstd[:, i:i + 1], ssq[:, i:i + 1], 1.0 / D, EPS)
            p.ts(b.v(), a.v(), rstd[:, i:i + 1], ALU.mult)
            for g in range(2):
                hb = hbank()
                for j in range(8):
                    kc = g * 8 + j
                    p.tr(hb[:, j * 128:(j + 1) * 128], b[:, kc * 128:(kc + 1) * 128], ident_b.v())
                src = hb.v().re("p (j t) -> p j t", j=8)
                dst = hT[:, g * 8:(g + 1) * 8, i * 128:(i + 1) * 128]
                if g == 0:
                    p.copy(dst, src, eng='act')
                else:
                    p.copy(dst, src, eng='dve')

        wf = [p.sb("wf%d" % i, [128, 16, 128], F32) for i in range(2)]
        wb = [p.sb("wb%d" % i, [128, 16, 128], BF16) for i in range(2)]
        prow = [p.sb("prow%d" % i, [128, T], F32) for i in range(2)]
        n1w_bc = n1w_s.v().re("p (k o) -> p k o", o=1)
        for ch in range(NCH):
            c0 = ch * 128
            m = min(128, NCOL - c0)
            f = wf[ch % 2]
            b = wb[ch % 2]
            pr = prow[ch % 2]
            p.dma(f[:, :, 0:m], w_in[:, c0:c0 + m].re("(k p) c -> p k c", p=128), eng='sp')
            p.tt(b[:, :, 0:m], f[:, :, 0:m], n1w_bc.bc([128, 16, m]), ALU.mult, eng='pool')
            for tb in range(4):
                bk = bank()
                for kc in range(16):
                    p.mm(bk[0:m, :], b[:, kc, 0:m], hT[:, kc, tb * 512:(tb + 1) * 512],
                         start=(kc == 0), stop=(kc == 15))
                p.copy(pr[0:m, tb * 512:(tb + 1) * 512], bk[0:m, :], eng='act' if tb % 2 == 0 else 'dve')
            p.dma(projT[c0:c0 + m, :], pr[0:m, :], eng='pool')
        if stage == 1:
            p.dma(scr2.v(), projT[0:1, 0:16])
            p.finish([projT.v()], scr.v(), scr2.v())
            p.pop()
            return nc, cn
        p.pop()

        mix_d = p.dram("mix_d", [128, 16, T], BF16)
        lam4 = IN("lam4", [1, 256])
        sublnw = IN("sublnw", [1, 128])
        p.push()
        PTf = p.sb("PTf", [128, 128], F32)
        PTb = p.sb("PTb", [128, 128], BF16)
        p.dma(PTf.v(), C['PT'].v())
        p.copy(PTb.v(), PTf.v())
        invf = p.sb("invf", [128, 1], F32)
        p.dma(invf.v(), C['invf'].v())
        posi = p.sb("posi", [128, T], I32)
        p.dma(posi.v(), pos[0:1, :].bc([128, T]))
        ang = p.sb("ang", [128, T], F32)
        cosF = p.sb("cosF", [128, T], F32)
        sinF = p.sb("sinF", [128, T], F32)
        p.copy(ang.v(), posi.v())
        p.ts(ang.v(), ang.v(), invf[:, 0:1], ALU.mult)
        angi = posi
        def rangered(dst, src):
            p.ts(t1x.v(), src, 1.0 / (2 * np.pi), ALU.mult)
            p.copy(angi.v(), t1x.v())
            p.copy(t1x.v(), angi.v())
            p.stt(dst, t1x.v(), -2 * np.pi, src, ALU.mult, ALU.add)
            p.ts(t1x.v(), dst, np.pi, ALU.is_gt, 2 * np.pi, ALU.mult)
            p.tt(dst, dst, t1x.v(), ALU.subtract)
            p.ts(t1x.v(), dst, -np.pi, ALU.is_lt, 2 * np.pi, ALU.mult)
            p.tt(dst, dst, t1x.v(), ALU.add)
        t1x = p.sb("t1x", [128, T], F32)
        rangered(sinF.v(), ang.v())
        p.act(sinF.v(), sinF.v(), AF.Sin)
        p.ts(ang.v(), ang.v(), np.pi / 2, ALU.add)
        rangered(cosF.v(), ang.v())
        p.act(cosF.v(), cosF.v(), AF.Sin)
        l4 = p.sb("l4", [128, 256], F32)
        p.dma(l4.v(), lam4[0:1, :].bc([128, 256]))
        lprod = p.sb("lprod", [128, 2, 64], F32)
        p.tt(lprod.v(), l4.v().re("p (a b d) -> p a b d", a=2, b=2)[:, :, 0, :],
             l4.v().re("p (a b d) -> p a b d", a=2, b=2)[:, :, 1, :], ALU.mult)
        lsum = p.sb("lsum", [128, 2], F32)
        p.red(lsum.v(), lprod.v(), ALU.add)
        p.act(lsum.v(), lsum.v(), AF.Exp)
        lam = p.sb("lam", [128, 1], F32)
        p.tt(lam.v(), lsum[:, 0:1], lsum[:, 1:2], ALU.subtract)
        p.ts(lam.v(), lam.v(), 0.2, ALU.add)
        slw = p.sb("slw", [128, 128], F32)
        p.dma(slw.v(), sublnw[0:1, :].bc([128, 128]))
        p.ts(slw.v(), slw.v(), 0.8, ALU.mult)

        qf = p.sb("qf", [128, T], F32)
        kf = p.sb("kf", [128, T], F32)
        vf = p.sb("vf", [128, T], F32)
        xq16 = p.sb("xq16", [128, T], BF16)
        xk16 = p.sb("xk16", [128, T], BF16)
        xv16 = p.sb("xv16", [128, T], BF16)
        t1 = p.sb("t1", [128, T], F32)
        QR = [p.sb("qr%d" % i, [128, T], BF16) for i in range(2)]
        KR = [p.sb("kr%d" % i, [128, T], BF16) for i in range(2)]
        VA = [p.sb("vaug%d" % i, [128, 16, 129], BF16) for i in range(2)]
        for i in range(2):
            p.memset(VA[i].v(), 1.0)
        pT = [p.sb("pT%d" % i, [128, 512], BF16) for i in range(3)]
        oacc = [p.sb("oacc%d" % i, [128, 4, 129], F32) for i in range(2)]
        att = p.sb("att", [128, 4, 128], F32)
        att1 = p.sb("att1", [128, 4, 128], F32)
        attb = p.sb("attb", [128, 4, 128], BF16)
        rr = p.sb("rr", [128, 2, 4], F32)
        ssa = p.sb("ssa", [128, 4], F32)
        mst = [p.sb("mst%d" % i, [128, 512], BF16) for i in range(2)]
        cnt = 0

        def prep_load(h):
            p.dma(qf.v(), projT[h * 128:(h + 1) * 128, :], eng='sp')
            p.dma(kf.v(), projT[1024 + h * 128:1024 + (h + 1) * 128, :], eng='sp')
            p.dma(vf.v(), projT[2048 + h * 128:2048 + (h + 1) * 128, :], eng='sp')
            p.copy(xq16.v(), qf.v(), eng='pool')
            p.copy(xk16.v(), kf.v(), eng='pool')
            p.copy(xv16.v(), vf.v(), eng='pool')
            p.tt(qf.v(), qf.v(), cosF.v(), ALU.mult, eng='pool')
            p.tt(kf.v(), kf.v(), cosF.v(), ALU.mult, eng='pool')

        def prep_pe(h):
            qr, kr, vaug = QR[h % 2], KR[h % 2], VA[h % 2]
            for (x16, src, dst) in ((xq16, qf, qr), (xk16, kf, kr)):
                for tb in range(4):
                    bk = PB[4 + tb % 2]
                    sl = slice(tb * 512, (tb + 1) * 512)
                    p.mm(bk.v(), PTb.v(), x16[:, sl])
                    p.tt(t1[:, sl], bk.v(), sinF[:, sl], ALU.mult)
                p.tt(dst.v(), src.v(), t1.v(), ALU.add)
            for g in range(2):
                hb = hbank()
                for j in range(8):
                    kt = g * 8 + j
                    p.tr(hb[:, j * 128:(j + 1) * 128], xv16[:, kt * 128:(kt + 1) * 128], ident_b.v())
                p.copy(vaug[:, g * 8:(g + 1) * 8, 0:128], hb.v().re("p (j e) -> p j e", j=8), eng='dve')

        prep_load(0)
        prep_pe(0)
        for h in range(8):
            qr, kr, vaug = QR[h % 2], KR[h % 2], VA[h % 2]
            for qb in range(4):
                if h + 1 < 8 and qb == 0:
                    prep_load(h + 1)
                if h + 1 < 8 and qb == 2:
                    prep_pe(h + 1)
                qsl = slice(qb * 512, (qb + 1) * 512)
                its = [(c, kt) for c in range(2) for kt in range(16)]

                def qk(i):
                    c, kt = its[i]
                    ps_ = slice(64 * c, 64 * c + 64)
                    p.mm(PB[4 + i % 2].v(), kr[ps_, kt * 128:(kt + 1) * 128], qr[ps_, qsl])
                qk(0)
                for i, (c, kt) in enumerate(its):
                    if i + 1 < len(its):
                        qk(i + 1)
                    sbk = PB[4 + i % 2]
                    pt_ = pT[i % 3]
                    p.act(pt_.v(), sbk.v(), AF.Exp, scale=0.125)
                    for qs in range(4):
                        p.mm(PB[qs][:, 0:129], pt_[:, qs * 128:(qs + 1) * 128], vaug[:, kt, :],
                             start=(kt == 0), stop=(kt == 15))
                    if kt == 15:
                        for qs in range(4):
                            p.copy(oacc[c][:, qs, :], PB[qs][:, 0:129], eng='dve')
                p.recip(rr[:, 0, :], oacc[0][:, :, 128])
                p.recip(rr[:, 1, :], oacc[1][:, :, 128])
                p.ts(rr[:, 1, :], rr[:, 1, :], lam[:, 0:1], ALU.mult)
                p.tt(att.v(), oacc[0][:, :, 0:128], rr[:, 0, :].re("p (q o) -> p q o", o=1).bc([128, 4, 128]), ALU.mult)
                p.tt(att1.v(), oacc[1][:, :, 0:128], rr[:, 1, :].re("p (q o) -> p q o", o=1).bc([128, 4, 128]), ALU.mult, eng='pool')
                p.tt(att.v(), att.v(), att1.v(), ALU.subtract)
                p.tt(att1.v(), att.v(), att.v(), ALU.mult, eng='pool')
                p.red(ssa.v(), att1.v(), ALU.add)
                p.rsqrt(ssa.v(), ssa.v(), 1.0 / 128, 1e-5)
                p.tt(att.v(), att.v(), ssa.v().re("p (q o) -> p q o", o=1).bc([128, 4, 128]), ALU.mult)
                p.tt(attb.v(), att.v(), slw.v().re("p (o e) -> p o e", o=1).bc([128, 4, 128]), ALU.mult)
                hb = hbank()
                for qs in range(4):
                    p.tr(hb[:, qs * 128:(qs + 1) * 128], attb[:, qs, :], ident_b.v())
                ms_ = mst[(h * 4 + qb) % 2]
                p.copy(ms_.v(), hb[:, 0:512], eng='act')
                p.dma(mix_d[:, h, qsl], ms_.v(), eng='pool')
        p.pop()

        NHP = 8 if stage != 3 else 1
        rwkv_stage(p, locals())
        tail_stage(p, locals())
    return nc, cn


def moe_hook(p, L, i):
    pass


def rwkv_inputs(inp):
    m = {}

    def cols28(mu):
        o = np.zeros((128, 28), np.float32)
        o[:, 0:24] = mu[0:3072].reshape(24, 128).T
        o[0:64, 24] = mu[3072:3136]
        o[64:128, 25] = mu[3136:3200]
        o[:, 26] = mu[3200:3328]
        o[0:32, 27] = mu[3328:3360]
        return o
    m["muP"] = cols28(inp['mu_prev'][0])
    m["muN"] = cols28(inp['mu_next'][0])
    m["w0r"] = np.ascontiguousarray(inp['w0'][0].reshape(2, 8, 128).transpose(2, 0, 1).reshape(128, 16))
    m["a0r"] = np.ascontiguousarray(inp['a0'][0].reshape(2, 8, 128).transpose(2, 0, 1).reshape(128, 16))
    m["kkr"] = np.ascontiguousarray(inp['k_k'][0].reshape(8, 128).T)
    m["kar"] = np.ascontiguousarray(inp['k_a'][0].reshape(8, 128).T)
    m["rkr"] = np.ascontiguousarray(inp['r_k'][0].reshape(8, 128).T)
    m["lnw_tok"] = np.ascontiguousarray(np.repeat(inp['ln_x_w'][0].reshape(8, 2, 1, 64), 64, axis=2).reshape(8, 128, 64))
    m["lnb_tok"] = np.ascontiguousarray(np.repeat(inp['ln_x_b'][0].reshape(8, 2, 1, 64), 64, axis=2).reshape(8, 128, 64))
    m["decay_up"] = np.ascontiguousarray(inp['decay_up'][0])
    m["iclr_up"] = np.ascontiguousarray(inp['iclr_up'][0])
    m["gate_up"] = np.ascontiguousarray(inp['gate_up'][0])
    return {k: v.astype(np.float32) for k, v in m.items()}


def kernel(**inputs):
    inp = {k: np.asarray(v) for k, v in inputs.items()}
    nc, cn = build(99)

    def core_inputs(b):
        m = {"x": np.ascontiguousarray(inp['x'][b]),
             "pos": np.ascontiguousarray(inp['positions'][b][None, :].astype(np.int32)),
             "n1w": np.ascontiguousarray(inp['norm1_w'][0].reshape(16, 128).T),
             "w_in": np.ascontiguousarray(inp['w_in'][0]),
             "lam4": np.concatenate([inp['lambda_q1'][0], inp['lambda_k1'][0], inp['lambda_q2'][0],
                                     inp['lambda_k2'][0]])[None, :].astype(np.float32),
             "sublnw": inp['subln_w'].astype(np.float32).reshape(1, 128),
             "w_out": np.ascontiguousarray(inp['w_out'][0]),
             "nfw": inp['norm_f_w'].astype(np.float32).reshape(1, D),
             "n2w": inp['norm2_w'].astype(np.float32).reshape(1, D),
             "w_router": w_router, "e_gate": e_gate, "e_up": e_up, "e_down": e_down}
        m.update(rw)
        for k, v in cn.items():
            m["c_" + k] = v
        return m

    rw = rwkv_inputs(inp)
    w_router = np.ascontiguousarray(inp['w_router'][0])
    e_gate = np.ascontiguousarray(inp['e_gate'][0])
    e_up = np.ascontiguousarray(inp['e_up'][0])
    e_down = np.ascontiguousarray(inp['e_down'][0])
    maps = [core_inputs(c // 2) for c in range(8)]
    res = run_bass_kernel_spmd(nc, maps, core_ids=list(range(8)))
    return np.stack([res.results[2 * b]["out"] for b in range(4)], 0).astype(np.float32)


def rwkv_stage(p, L):
    IN = L['IN']
    C = L['C']
    PB = L['PB']
    bank = L['bank']
    hbank = L['hbank']
    ident_b = L['ident_b']
    ident_f = L['ident_f']
    projT = L['projT']
    mix_d = L['mix_d']
    muP_d = IN("muP", [128, 28])
    muN_d = IN("muN", [128, 28])
    w0r_d = IN("w0r", [128, 16])
    a0r_d = IN("a0r", [128, 16])
    kkr_d = IN("kkr", [128, 8])
    kar_d = IN("kar", [128, 8])
    rkr_d = IN("rkr", [128, 8])
    lnw_d = IN("lnw_tok", [8, 128, 64])
    lnb_d = IN("lnb_tok", [8, 128, 64])
    dup_d = IN("decay_up", [2, 64, 1024])
    iup_d = IN("iclr_up", [2, 64, 1024])
    gup_d = IN("gate_up", [160, 1024])
    p.push()
    SEG = 512
    muP = p.sb("muP_s", [128, 28], F32)
    muN = p.sb("muN_s", [128, 28], F32)
    mu0 = p.sb("mu0_s", [128, 28], F32)
    p.dma(muP.v(), muP_d.v())
    p.dma(muN.v(), muN_d.v())
    p.tt(mu0.v(), muP.v(), muN.v(), ALU.add)
    p.ts(mu0.v(), mu0.v(), -1.0, ALU.mult, 1.0, ALU.add)
    w0r = p.sb("w0r_s", [128, 16], F32)
    a0r = p.sb("a0r_s", [128, 16], F32)
    kkr = p.sb("kkr_s", [128, 8], F32)
    kar = p.sb("kar_s", [128, 8], F32)
    rkr = p.sb("rkr_s", [128, 8], F32)
    for t_, d_ in ((w0r, w0r_d), (a0r, a0r_d), (kkr, kkr_d), (kar, kar_d), (rkr, rkr_d)):
        p.dma(t_.v(), d_.v())
    masks = p.sb("masks_s", [128, 4, 128], F32)
    p.dma(masks.v(), C['masks'].v().re("m p (n r) -> p m n r", n=4)[:, :, 0, :])
    rmask = p.sb("rmask", [128, SEG], F32)
    p.dma(rmask.v(), C['resetmask'][:, 0:SEG])
    bones_f = p.sb("bones_f", [128, 128], F32)
    bones = p.sb("bones", [128, 128], BF16)
    p.dma(bones_f.v(), C['blockones'].v())
    p.copy(bones.v(), bones_f.v())
    sel_f = p.sb("sel_f", [128, 64], F32)
    sel_b = p.sb("sel_b", [128, 64], BF16)
    p.dma(sel_f.v(), C['sel'].v())
    p.copy(sel_b.v(), sel_f.v())
    ones_b = p.sb("ones_b", [128, 1], BF16)
    p.memset(ones_b.v(), 1.0)
    stg = p.sb("lstg", [128, 512], F32)
    lwb = p.sb("lwb", [128, 2, 1024], BF16)
    gup0 = p.sb("gup0", [128, 1024], BF16)
    gup1 = p.sb("gup1", [32, 1024], BF16)
    for d_ in range(2):
        for hf in range(2):
            cs_ = slice(hf * 512, (hf + 1) * 512)
            p.dma(stg[0:64, :], dup_d[d_][:, cs_])
            p.dma(stg[64:128, :], iup_d[d_][:, cs_])
            p.copy(lwb[:, d_, cs_], stg.v())
    for hf in range(2):
        cs_ = slice(hf * 512, (hf + 1) * 512)
        p.dma(stg.v(), gup_d[0:128, cs_])
        p.copy(gup0[:, cs_], stg.v())
        p.dma(stg[0:32, :], gup_d[128:160, cs_])
        p.copy(gup1[:, cs_], stg[0:32, :])

    zs = [p.sb("zs%d" % i, [128, SEG + 2], F32) for i in range(2)]
    zcnt = [0]

    def shift_load(r0, m, mucol, seg, dst, p0=0):
        z = zs[zcnt[0] % 2]
        zcnt[0] += 1
        t0 = seg * SEG - 1
        lo = max(t0, 0)
        hi = min(seg * SEG + SEG + 1, T)
        ps_ = slice(p0, p0 + m)
        if seg == 0:
            p.memset(z[ps_, 0:1], 0.0, eng='pool')
        if hi - t0 < SEG + 2:
            p.memset(z[ps_, SEG + 1:SEG + 2], 0.0, eng='pool')
        p.dma(z[ps_, lo - t0:hi - t0], projT[r0:r0 + m, lo:hi], eng='sp')
        p.ts(dst, z[ps_, 1:SEG + 1], mu0[ps_, mucol:mucol + 1], ALU.mult)
        p.stt(dst, z[ps_, 0:SEG], muP[ps_, mucol:mucol + 1], dst, ALU.mult, ALU.add)
        p.stt(dst, z[ps_, 2:SEG + 2], muN[ps_, mucol:mucol + 1], dst, ALU.mult, ALU.add)

    lact = p.sb("lact", [128, T], BF16)
    sgd0 = p.sb("sgd0", [128, T], BF16)
    sgd1 = p.sb("sgd1", [32, T], BF16)
    TA_ = [p.sb("rw_ta%d" % d_, [128, SEG], F32) for d_ in range(2)]
    ltmp = TA_[0]
    for seg in range(4):
        sl = slice(seg * SEG, (seg + 1) * SEG)
        shift_load(6144, 64, 24, seg, ltmp[0:64, :])
        p.act(lact[0:64, sl], ltmp[0:64, :], AF.Tanh)
        shift_load(6208, 64, 25, seg, ltmp[64:128, :], p0=64)
        p.copy(lact[64:128, sl], ltmp[64:128, :], eng='dve')
        shift_load(6272, 128, 26, seg, ltmp[0:128, :])
        p.act(sgd0[:, sl], ltmp[0:128, :], AF.Sigmoid)
        shift_load(6400, 32, 27, seg, ltmp[0:32, :])
        p.act(sgd1[:, sl], ltmp[0:32, :], AF.Sigmoid)

    def f32t(name):
        return p.sb(name, [128, SEG], F32)
    F32S = [{n: f32t("rw_%s%d" % (n, d_)) for n in "zr zk zv kk logw aic kdir bq cf ci tb".split()} for d_ in range(2)]
    SQB = [p.sb("rw_sqb%d" % d_, [128, SEG], BF16) for d_ in range(2)]
    gam = [p.sb("rw_gam%d" % d_, [128, 8], F32) for d_ in range(2)]

    def etile(name):
        t_ = p.sb(name, [128, 8, 128], BF16)
        p.memset(t_.v(), 0.0, eng='pool')
        return t_
    ES = [{n: etile("rw_%s%d" % (n, d_)) for n in "kE bE khE bhE vE zE".split()} for d_ in range(2)]
    for d_ in range(2):
        t_ = p.sb("rw_arE%d" % d_, [128, 8, 256], BF16)
        p.memset(t_.v(), 0.0, eng='pool')
        ES[d_]['arE'] = t_
    CM = p.sb("rw_cmask", [128, 2, 256], F32)
    p.copy(CM[:, 0, 0:128], masks[:, 1, :], eng='pool')
    p.copy(CM[:, 0, 128:256], masks[:, 3, :], eng='pool')
    p.copy(CM[:, 1, 0:128], masks[:, 0, :], eng='pool')
    p.copy(CM[:, 1, 128:256], masks[:, 2, :], eng='pool')
    AW = [p.sb("rw_aW%d" % d_, [128, 8, 128], BF16) for d_ in range(2)]
    VW = [p.sb("rw_vW%d" % d_, [128, 8, 128], BF16) for d_ in range(2)]
    bW = [p.sb("rw_bW%d" % d_, [128, 8, 128], BF16) for d_ in range(2)]
    kW = [p.sb("rw_kW%d" % d_, [128, 8, 128], BF16) for d_ in range(2)]
    vst = [p.sb("rw_vst%d" % d_, [128, 8, 64], BF16) for d_ in range(2)]
    yacc = [p.sb("rw_yacc%d" % i, [128, 8, 64], F32) for i in range(4)]
    vst_all = [p.sb("rw_vstall%d" % i, [128, 8, 64], BF16) for i in range(4)]
    bsc = [p.sb("rw_bsc%d" % i, [128, 8], F32) for i in range(4)]
    Sm = [[p.sb("rw_Sm%d%d" % (d_, i), [128, 64], F32) for i in range(2)] for d_ in range(2)]
    Sb = [[p.sb("rw_Sb%d%d" % (d_, i), [128, 64], BF16) for i in range(2)] for d_ in range(2)]
    scnt = [0, 0]

    GS, NG = 2, 4

    def gt(name, n2=NG, w=128):
        return [[p.sb("rw_%s%d%d" % (name, d_, i), [128, GS, w], BF16) for i in range(n2)] for d_ in range(2)]
    SLb, YLb = [gt(n) for n in "SL YL".split()]
    LMb = gt("LM", w=256)
    KMb = gt("KM", w=256)
    TNb = gt("TN", w=64)
    Lb = [p.sb("rw_L%d" % i, [128, GS, 128], BF16) for i in range(NG)]
    TAb = [p.sb("rw_TA%d" % i, [128, GS, 128], BF16) for i in range(NG)]
    Pb = [[p.sb("rw_P%d%d" % (g_, i), [128, GS, 128], BF16) for i in range(2)] for g_ in range(NG)]
    PTb = [[p.sb("rw_PT%d%d" % (g_, i), [128, GS, 128], BF16) for i in range(2)] for g_ in range(NG)]
    TTb = [[p.sb("rw_TT%d%d" % (g_, i), [128, GS, 128], BF16) for i in range(2)] for g_ in range(NG)]
    Nb = [p.sb("rw_N%d" % i, [128, GS, 64], BF16) for i in range(NG)]
    owide = ES[0]['zE']
    zr, zk, zv = F32S[0]['zr'], F32S[0]['zk'], F32S[0]['zv']
    mst2 = [p.sb("rw_mst%d" % i, [128, SEG], BF16) for i in range(2)]
    fcnt = [0]
    fin_s = p.sb("rw_fs", [128, 8], F32)
    fin_r = p.sb("rw_fr", [128, 8], F32)
    lnw = p.sb("rw_lnw", [128, 64], F32)
    lnb = p.sb("rw_lnb", [128, 64], F32)
    gT = zv
    ev = [0]

    def evac_eng():
        ev[0] += 1
        return 'act' if ev[0] % 2 == 0 else 'dve'

    def c3(v):
        return v.re("p (c i) -> p c i", i=64)

    def prepA(hp, d, seg, first):
        zr, zk, zv, kk, logw, aic, kdir, bq, cf, ci, tb_ = [F32S[d][n] for n in "zr zk zv kk logw aic kdir bq cf ci tb".split()]
        ta = TA_[d]
        sqb = SQB[d]
        kE, bE, khE, bhE, vE, zE = [ES[d][n] for n in "kE bE khE bhE vE zE".split()]
        aE = ES[d]['arE'][:, :, 0:128]
        rE = ES[d]['arE'][:, :, 128:256]
        aW, vW = AW[d], VW[d]
        sl = slice(seg * SEG, (seg + 1) * SEG)
        cols = slice(hp * 128, (hp + 1) * 128)
        shift_load(3072 + hp * 128, 128, hp, seg, zr.v())
        yield
        shift_load(4096 + hp * 128, 128, 8 + hp, seg, zk.v())
        yield
        shift_load(5120 + hp * 128, 128, 16 + hp, seg, zv.v())
        yield
        bk = bank()
        p.mm(bk.v(), lwb[0:64, d, cols], lact[0:64, sl])
        p.act(logw.v(), bk.v(), AF.Sigmoid, bias=w0r[:, d * 8 + hp:d * 8 + hp + 1])
        p.ts(logw.v(), logw.v(), -0.6065306597126334, ALU.mult)
        yield
        bk = bank()
        p.mm(bk.v(), lwb[64:128, d, cols], lact[64:128, sl])
        p.act(aic.v(), bk.v(), AF.Sigmoid, bias=a0r[:, d * 8 + hp:d * 8 + hp + 1])
        yield
        p.ts(kk.v(), zk.v(), kkr[:, hp:hp + 1], ALU.mult)
        p.tt(sqb.v(), kk.v(), kk.v(), ALU.mult, eng='pool')
        yield
        bk = bank()
        p.mm(bk.v(), bones.v(), sqb.v())
        p.ts(ta.v(), bk.v(), 1e-24, ALU.max)
        p.act(ta.v(), ta.v(), AF.Sqrt)
        yield
        p.recip(ta.v(), ta.v())
        p.tt(kk.v(), kk.v(), ta.v(), ALU.mult, eng='pool')
        yield
        p.ts(ta.v(), aic.v(), -1.0, ALU.add, kar[:, hp:hp + 1], ALU.mult)
        p.stt(kdir.v(), ta.v(), 1.0, zk.v(), ALU.add, ALU.mult)
        yield
        p.tt(bq.v(), kk.v(), aic.v(), ALU.mult, eng='pool')
        p.tt(ta.v(), zr.v(), kdir.v(), ALU.mult, eng='pool')
        yield
        for h in range(2):
            ps_ = slice(64 * h, 64 * h + 64)
            p.ts(zE[ps_, :, 64 * h:64 * h + 64], c3(ta[ps_, :]), rkr[ps_, hp:hp + 1], ALU.mult)
        bk = bank()
        for c in range(8):
            p.mm(bk[:, c:c + 1], zE[:, c, :], ones_b.v())
        if first:
            p.copy(bsc[seg].v(), bk[:, 0:8], eng='act')
        else:
            p.tt(bsc[seg].v(), bsc[seg].v(), bk[:, 0:8], ALU.add)
        yield
        p.op('dve', lambda e: e.tensor_tensor_scan(cf.h[:], rmask.h[:], logw.h[:], 0.0, ALU.mult, ALU.add),
             [rmask.v(), logw.v()], [cf.v()])
        tot = c3(cf.v())[:, :, 63:64]
        if d == 0:
            cisrc = cf
        else:
            p.tt(ci.v(), logw.v(), cf.v(), ALU.subtract, eng='pool')
            p.tt(c3(ci.v()), c3(ci.v()), tot.bc([128, 8, 64]), ALU.add, eng='pool')
            cisrc = ci

        def wE(dst, a_, b_, neg=False):
            for h in range(2):
                ps_ = slice(64 * h, 64 * h + 64)
                o = dst[ps_, :, 64 * h:64 * h + 64]
                if neg:
                    p.stt(o, c3(a_[ps_, :]), -1.0, c3(b_[ps_, :]), ALU.mult, ALU.mult)
                else:
                    p.tt(o, c3(a_[ps_, :]), c3(b_[ps_, :]), ALU.mult, eng='pool' if h else 'dve')
        p.act(ta.v(), cisrc.v(), AF.Exp)
        wE(rE, zr, ta)
        yield
        p.act(ta.v(), cisrc.v(), AF.Exp, scale=-1.0)
        wE(kE, kdir, ta)
        yield
        wE(bE, bq, ta)
        yield
        p.tt(tb_.v(), cisrc.v(), logw.v(), ALU.subtract, eng='pool')
        p.act(tb_.v(), tb_.v(), AF.Exp)
        wE(aE, kk, tb_, neg=True)
        yield
        p.tt(c3(tb_.v()), tot.bc([128, 8, 64]), c3(cisrc.v()), ALU.subtract, eng='pool')
        p.act(tb_.v(), tb_.v(), AF.Exp)
        wE(khE, kdir, tb_)
        yield
        wE(bhE, bq, tb_)
        yield
        for h in range(2):
            ps_ = slice(64 * h, 64 * h + 64)
            p.copy(vE[ps_, :, 64 * h:64 * h + 64], c3(zv[ps_, :]), eng='pool')
        yield

    def prepB(d, seg):
        cf = F32S[d]['cf']
        khE, bhE, vE = [ES[d][n] for n in "khE bhE vE".split()]
        aE = ES[d]['arE'][:, :, 0:128]
        aW, vW = AW[d], VW[d]
        tot = c3(cf.v())[:, :, 63:64]
        p.act(gam[d].v().re("p (c o) -> p c o", o=1), tot, AF.Exp)
        for srcE, dstW in ((aE, aW), (bhE, bW[d]), (khE, kW[d]), (vE, vW)):
            hb = hbank()
            for c in range(8):
                p.tr(hb[:, c * 128:(c + 1) * 128], srcE[:, c, :], ident_b.v())
            p.copy(dstW.v(), hb.v().re("p (c r) -> p c r", c=8), eng=evac_eng())
        p.tt(vst[d].v(), vW[:, :, 0:64], vW[:, :, 64:128], ALU.add, eng='pool')
        if d == 0:
            p.copy(vst_all[seg].v(), vst[d].v(), eng='pool')

    def par_group(d, g, gi):
        kE, bE = ES[d]['kE'], ES[d]['bE']
        arE = ES[d]['arE']
        aE = arE[:, :, 0:128]
        rE = arE[:, :, 128:256]
        aW = AW[d]
        LM, KM = LMb[d][gi], KMb[d][gi]
        LT, MrbT = LM[:, :, 0:128], LM[:, :, 128:256]
        LakT, MrkT = KM[:, :, 0:128], KM[:, :, 128:256]

        def mm2(dst, lhsE):
            bk_ = bank()
            for n in range(GS):
                p.mm(bk_[:, n * 256:(n + 1) * 256], lhsE[:, g * GS + n, :], arE[:, g * GS + n, :])
            p.tt(dst.v(), bk_[:, 0:GS * 256].re("p (n r) -> p n r", n=GS),
                 CM[:, d:d + 1, :].bc([128, GS, 256]), ALU.mult)
        MS, MST, MIT = (0, 1, 3) if d == 0 else (1, 0, 2)

        def mmg(lf, rf, n_out=128):
            bk_ = bank()
            for n in range(GS):
                p.mm(bk_[:, n * n_out:(n + 1) * n_out], lf(n), rf(n))
            return bk_

        def Ec(tl):
            return lambda n, tl=tl: tl[:, g * GS + n, :]

        def Gc(tl):
            return lambda n, tl=tl: tl[:, n, :]

        def b4(bk_):
            return bk_[:, 0:GS * 128].re("p (n r) -> p n r", n=GS)

        def masked(dst, bk_, mi):
            p.tt(dst.v(), b4(bk_), masks[:, mi:mi + 1, :].bc([128, GS, 128]), ALU.mult)

        bk_ = mmg(Ec(aE), Ec(bE))
        masked(Lb[g], bk_, MS)
        mm2(LM, bE)
        tt_ = TTb[g][0]
        p.tt(tt_.v(), LT, ident_f.v().re("p (o r) -> p o r", o=1).bc([128, GS, 128]), ALU.add, eng='pool')
        yield
        P_, PT_ = Lb[g], LT
        for it in range(5):
            bk_ = mmg(Gc(PT_), Gc(P_))
            P2 = Pb[g][it % 2]
            p.copy(P2.v(), b4(bk_), eng='act')
            if it < 4:
                bk_ = mmg(Gc(P_), Gc(PT_))
                PT2 = PTb[g][it % 2]
                p.copy(PT2.v(), b4(bk_), eng='act')
            yield
            bk_ = mmg(Gc(P2), Gc(tt_))
            ttn = TTb[g][(it + 1) % 2]
            p.tt(ttn.v(), b4(bk_), tt_.v(), ALU.add)
            tt_ = ttn
            P_ = P2
            if it < 4:
                PT_ = PT2
            yield
        mm2(KM, kE)
        bk_ = mmg(Gc(tt_), Ec(aW))
        p.copy(TAb[g].v(), b4(bk_), eng='act')
        yield
        bk_ = mmg(Gc(LakT), lambda n: vst[d][:, g * GS + n, :], n_out=64)
        p.copy(Nb[g].v(), bk_[:, 0:GS * 64].re("p (n r) -> p n r", n=GS), eng='act')
        bk_ = mmg(Gc(TAb[g]), Ec(bW[d]))
        p.copy(SLb[d][gi].v(), b4(bk_), eng='act')
        bk_ = mmg(Gc(TAb[g]), Gc(MrbT))
        p.tt(YLb[d][gi].v(), b4(bk_), rE[:, g * GS:(g + 1) * GS, :], ALU.add)
        yield
        bk_ = mmg(Gc(tt_), Gc(Nb[g]), n_out=64)
        p.copy(TNb[d][gi].v(), bk_[:, 0:GS * 64].re("p (n r) -> p n r", n=GS), eng='act')
        yield

    def unit_parallel(d, gbase, extra=()):
        gens = [par_group(d, g, g) for g in range(NG)] + list(extra)
        alive = list(gens)
        while alive:
            for gen in list(alive):
                try:
                    next(gen)
                except StopIteration:
                    alive.remove(gen)
        return {g: g for g in range(NG)}

    def seq_steps(d, seg, G, first):
        order = range(8) if d == 0 else range(7, -1, -1)
        steps = []
        for c in order:
            def step(c=c):
                g = c // GS
                n = c % GS
                gi = G[g]
                k_ = scnt[d]
                scnt[d] += 1
                s_old, s_new = Sm[d][k_ % 2], Sm[d][(k_ + 1) % 2]
                b_old, b_new = Sb[d][k_ % 2], Sb[d][(k_ + 1) % 2]
                bs = bank()
                p.mm(bs[:, 0:64], bW[d][:, c, :], TNb[d][gi][:, n, :], start=True, stop=False)
                p.mm(bs[:, 0:64], kW[d][:, c, :], vst[d][:, c, :], start=False, stop=False)
                p.mm(bs[:, 0:64], SLb[d][gi][:, n, :], b_old.v(), start=False, stop=True)
                by = bank()
                p.mm(by[:, 0:64], LMb[d][gi][:, n, 128:256], TNb[d][gi][:, n, :], start=True, stop=False)
                p.mm(by[:, 0:64], KMb[d][gi][:, n, 128:256], vst[d][:, c, :], start=False, stop=False)
                p.mm(by[:, 0:64], YLb[d][gi][:, n, :], b_old.v(), start=False, stop=True)
                p.stt(b_new.v(), s_old.v(), gam[d][:, c:c + 1], bs[:, 0:64], ALU.mult, ALU.add)
                p.stt(s_new.v(), s_old.v(), gam[d][:, c:c + 1], bs[:, 0:64], ALU.mult, ALU.add)
                if first:
                    p.copy(yacc[seg][:, c, :], by[:, 0:64], eng='act')
                else:
                    p.tt(yacc[seg][:, c, :], yacc[seg][:, c, :], by[:, 0:64], ALU.add, eng='dve')
            steps.append(step)
        return steps

    def finalize(hp):
        FA = c3(zr.v())
        FB = c3(zk.v())
        p.dma(lnw.v(), lnw_d[hp])
        p.dma(lnb.v(), lnb_d[hp])
        cols = slice(hp * 128, (hp + 1) * 128)
        for seg in range(4):
            sl = slice(seg * SEG, (seg + 1) * SEG)
            cs = slice(seg * 8, (seg + 1) * 8)
            y = yacc[seg].v()
            p.red(fin_s.v(), y, ALU.add)
            p.ts(fin_s.v(), fin_s.v(), 1.0 / 64, ALU.mult)
            p.tt(FA, y, fin_s.v().re("p (c o) -> p c o", o=1).bc([128, 8, 64]), ALU.subtract)
            p.tt(FB, FA, FA, ALU.mult, eng='pool')
            p.red(fin_r.v(), FB, ALU.add)
            p.rsqrt(fin_r.v(), fin_r.v(), 1.0 / 64, 64e-5)
            p.tt(FA, FA, fin_r.v().re("p (c o) -> p c o", o=1).bc([128, 8, 64]), ALU.mult)
            p.tt(FA, FA, lnw.v().re("p (o v) -> p o v", o=1).bc([128, 8, 64]), ALU.mult)
            p.tt(FA, FA, lnb.v().re("p (o v) -> p o v", o=1).bc([128, 8, 64]), ALU.add)
            p.tt(FB, vst_all[seg].v(), bsc[seg].v().re("p (c o) -> p c o", o=1).bc([128, 8, 64]), ALU.mult, eng='pool')
            p.tt(FA, FA, FB, ALU.add)
            for h in range(2):
                ps_ = slice(64 * h, 64 * h + 64)
                p.copy(owide[ps_, :, 64 * h:64 * h + 64], c3(zr[ps_, :]), eng='pool' if h else 'dve')
            bk = bank()
            for c in range(8):
                p.mm(bk[:, c * 64:(c + 1) * 64], owide[:, c, :], sel_b.v())
            bg = bank()
            p.mm(bg.v(), gup0[:, cols], sgd0[:, sl], start=True, stop=False)
            p.mm(bg.v(), gup1[:, cols], sgd1[:, sl], start=False, stop=True)
            p.copy(gT.v(), bg.v(), eng='act')
            ms_ = mst2[fcnt[0] % 2]
            fcnt[0] += 1
            p.tt(ms_.v(), bk.v(), gT.v(), ALU.mult)
            p.dma(mix_d[:, 8 + hp, sl], ms_.v(), eng='pool')

    NHP = L.get('NHP', 8)
    gb = 0

    def drain(gen):
        for _ in gen:
            pass
    for hp in range(NHP):
        for d in range(2):
            p.memset(Sm[d][scnt[d] % 2].v(), 0.0)
            p.memset(Sb[d][scnt[d] % 2].v(), 0.0)
        U = []
        for s_ in range(4):
            U.append((0, s_, s_ < 2))
            U.append((1, 3 - s_, s_ < 2))
        drain(prepA(hp, U[0][0], U[0][1], U[0][2]))
        prepB(U[0][0], U[0][1])
        Gs = {}
        for k, (d, seg, first) in enumerate(U):
            nxt = U[k + 1] if k + 1 < len(U) else None
            extra = [prepA(hp, nxt[0], nxt[1], nxt[2])] if nxt else []
            Gs[d] = unit_parallel(d, gb, extra)
            if k % 2 == 1:
                gb += 1
                st0 = seq_steps(0, U[k - 1][1], Gs[0], first)
                st1 = seq_steps(1, seg, Gs[1], first)
                for a_, b_ in zip(st0, st1):
                    a_()
                    b_()
            if nxt:
                prepB(nxt[0], nxt[1])
        finalize(hp)
    p.pop()
def tail_stage(p, L):
    IN = L['IN']
    C = L['C']
    PB = L['PB']
    bank = L['bank']
    hbank = L['hbank']
    ident_b = L['ident_b']
    ident_f = L['ident_f']
    mix_d = L['mix_d']
    mixT = p.sb("mixT", [128, 16, T], BF16)
    x = L['x']
    out = L['out']
    scr = L['scr']
    scr2 = L['scr2']
    projT = L['projT']
    NE = L.get('NE', 16)
    w_out = IN("w_out", [D, D])
    nfw = IN("nfw", [1, D])
    n2w = IN("n2w", [1, D])
    wr_d = IN("w_router", [D, 16])
    eg_d = IN("e_gate", [16, D, D])
    eu_d = IN("e_up", [16, D, D])
    ed_d = IN("e_down", [16, D, D])
    xmid_d = p.dram("xmid_d", [T, D], F32)
    h2_d = p.dram("h2_d", [T, D], BF16)
    ye_d = p.dram("ye_d", [16, 2, 128, D], BF16)
    aff = p.sb("aff", [128, 16, 16], F32)
    valT = p.sb("valT", [128, 16, 16], F32)
    iota1 = p.sb("iota1", [128, 256], F32)
    p.dma(iota1.v(), C['iota1'].v())
    idxs = p.sb("idxs", [128, 32], F32)
    iop_f = p.sb("iop_f", [128, 1], F32)
    p.dma(iop_f.v(), C['iota_p'].v())

    p.push()
    for kc in range(16):
        p.dma(mixT[:, kc, :], mix_d[:, kc, :], eng='sp')
    wo = p.sb("wo", [128, 16, D], BF16)
    for kc in range(16):
        p.dma(wo[:, kc, :], w_out[kc * 128:(kc + 1) * 128, :], eng='pool')
    n2w_s = p.sb("n2w_s", [128, D], F32)
    p.dma(n2w_s.v(), n2w[0:1, :].bc([128, D]))
    wr = p.sb("wr", [128, 16, 16], F32)
    p.dma(wr.v(), wr_d.v().re("(k p) e -> p k e", p=128))
    xt2 = [p.sb("xt2_%d" % i, [128, D], F32) for i in range(2)]
    xm = [p.sb("xm%d" % i, [128, D], F32) for i in range(2)]
    h2f = p.sb("h2f", [128, D], F32)
    h2b = [p.sb("h2b0", [128, D], BF16)] * 2
    h2T = p.sb("h2T", [128, 16, 128], F32)
    ss2 = p.sb("ss2", [128, NT], F32)
    mx = p.sb("mx", [128, NT], F32)
    sm = p.sb("sm", [128, NT], F32)
    def stA(i):
        xa = xt2[i % 2]
        xo = xm[i % 2]
        p.dma(xa.v(), x[i * 128:(i + 1) * 128, :], eng='sp')
        for db in range(4):
            bk = bank()
            for kc in range(16):
                p.mm(bk.v(), mixT[:, kc, i * 128:(i + 1) * 128], wo[:, kc, db * 512:(db + 1) * 512],
                     start=(kc == 0), stop=(kc == 15))
            p.tt(xo[:, db * 512:(db + 1) * 512], bk.v(), xa[:, db * 512:(db + 1) * 512], ALU.add)
        p.dma(xmid_d[i * 128:(i + 1) * 128, :], xo.v(), eng='pool')

    def stB(i):
        xo = xm[i % 2]
        hb_ = h2b[i % 2]
        p.act(h2f.v(), xo.v(), AF.Square, accum=ss2[:, i:i + 1])
        p.rsqrt(ss2[:, i:i + 1], ss2[:, i:i + 1], 1.0 / D, EPS)
        p.ts(h2f.v(), xo.v(), ss2[:, i:i + 1], ALU.mult)
        p.tt(h2f.v(), h2f.v(), n2w_s.v(), ALU.mult)
        p.copy(hb_.v(), h2f.v(), eng='act')
        p.dma(h2_d[i * 128:(i + 1) * 128, :], hb_.v(), eng='pool')

    def stC(i):
        for q in range(4):
            bk = bank()
            for j in range(4):
                kc = q * 4 + j
                p.tr(bk[:, j * 128:(j + 1) * 128], h2f[:, kc * 128:(kc + 1) * 128], ident_f.v())
            p.copy(h2T[:, q * 4:(q + 1) * 4, :], bk.v().re("p (j t) -> p j t", j=4), eng='act' if q % 2 else 'dve')
        bk = bank()
        for kc in range(16):
            p.mm(bk[:, 0:16], h2T[:, kc, :], wr[:, kc, :], start=(kc == 0), stop=(kc == 15))
        p.red(mx[:, i:i + 1], bk[:, 0:16], ALU.max)
        p.ts(mx[:, i:i + 1], mx[:, i:i + 1], -1.0, ALU.mult)
        p.act(aff[:, i, :], bk[:, 0:16], AF.Exp, bias=mx[:, i:i + 1], accum=sm[:, i:i + 1])
        p.recip(sm[:, i:i + 1], sm[:, i:i + 1])
        p.ts(aff[:, i, :], aff[:, i, :], sm[:, i:i + 1], ALU.mult)

    stA(0)
    for i in range(NT):
        if i + 1 < NT:
            stA(i + 1)
        stB(i)
        stC(i)
    p.pop()

    p.push()
    for i in range(NT):
        p.dma(mixT[:, i, :], h2_d[i * 128:(i + 1) * 128, :], eng='sp' if i % 2 else 'pool')
    affT = p.sb("affT", [16, T], F32)
    work = p.sb("work", [16, T], F32)
    maskT = p.sb("maskT", [16, T], F32)
    onesT = p.sb("onesT", [16, T], F32)
    m8 = p.sb("m8", [16, 8], F32)
    p.memset(onesT.v(), 1.0, eng='pool')
    for q in range(4):
        bk = bank()
        for j in range(4):
            i = q * 4 + j
            p.tr(bk[0:16, j * 128:(j + 1) * 128], aff[:, i, :], ident_f.v())
        p.copy(affT[:, q * 512:(q + 1) * 512], bk[0:16, :], eng='dve')
    p.copy(work.v(), affT.v(), eng='dve')
    for r_ in range(32):
        p.op('dve', lambda e: e.max(out=m8.h[:], in_=work.h[:]), [work.v()], [m8.v()])
        if r_ < 31:
            p.op('dve', lambda e: e.match_replace(out=work.h[:], in_to_replace=m8.h[:], in_values=work.h[:],
                                                  imm_value=-1.0), [work.v(), m8.v()], [work.v()])
    p.ts(maskT.v(), affT.v(), m8[:, 7:8], ALU.is_ge)
    p.op('dve', lambda e: e.tensor_tensor_scan(work.h[:], onesT.h[:], maskT.h[:], 0.0, ALU.mult, ALU.add),
         [onesT.v(), maskT.v()], [work.v()])
    p.tt(work.v(), work.v(), maskT.v(), ALU.mult)
    bk = bank()
    for i in range(16):
        p.tr(bk[:, i * 16:(i + 1) * 16], work[:, i * 128:(i + 1) * 128], ident_f[0:16, 0:16])
    p.copy(valT.v(), bk[:, 0:256].re("p (i e) -> p i e", i=16), eng='dve')
    p.pop()

    p.push()
    h2s = mixT
    oh = p.sb("oh", [128, 16, 256], BF16)
    gm = p.sb("gm", [128, 16, 5], BF16)
    for i in range(16):
        p.memset(gm[:, i, 3:4], float(i))
        p.copy(gm[:, i, 4:5], iop_f.v())
    g1 = p.sb("g1", [128, 16], F32)
    g2 = p.sb("g2", [128, 16], F32)
    gb = p.sb("gb", [128, 16], BF16)
    gate = p.sb("gate", [128, 2], F32)
    g3 = p.sb("g3", [128, 2, 5], F32)
    xeT = p.sb("xeT", [128, 16, 256], BF16)
    hidT = p.sb("hidT", [128, 16, 256], BF16)
    ye = p.sb("ye", [128, 2, D], BF16)
    wb = [p.sb("ewb%d" % i, [128, 16, 512], BF16) for i in range(5)]
    sg = [p.sb("sg%d" % i, [128, 256], F32) for i in range(2)]
    wc = [0]

    def wload(src, e, cb):
        k = wc[0]
        wc[0] += 1
        b = wb[k % 5]
        p.dma(b.v(), src[e][:, cb * 512:(cb + 1) * 512].re("(k p) c -> p k c", p=128), eng='pool')
        return b

    iota_bc = iota1.v().re("p (o s) -> p o s", o=1).bc([128, 16, 256])
    for e in range(NE):
        p.tt(oh.v(), iota_bc, valT[:, :, e:e + 1].bc([128, 16, 256]), ALU.is_equal)
        p.copy(gb.v(), aff[:, :, e])
        p.copy(gm[:, :, 0], gb.v())
        p.tt(g1.v(), aff[:, :, e], gb.v(), ALU.subtract)
        p.copy(gb.v(), g1.v())
        p.copy(gm[:, :, 1], gb.v())
        p.tt(g2.v(), g1.v(), gb.v(), ALU.subtract)
        p.copy(gm[:, :, 2], g2.v())
        bk = bank()
        for sh in range(2):
            for i in range(16):
                p.mm(bk[:, sh * 8:sh * 8 + 5], oh[:, i, sh * 128:(sh + 1) * 128], gm[:, i, :],
                     start=(i == 0), stop=(i == 15))
        p.copy(g3.v(), bk[:, 0:16].re("p (a c) -> p a c", a=2)[:, :, 0:5])
        p.red(gate.v(), g3[:, :, 0:3], ALU.add)
        p.stt(idxs[:, 2 * e:2 * e + 2], g3[:, :, 3], 128.0, g3[:, :, 4], ALU.mult, ALU.add)
        for dq in range(8):
            bk = bank()
            for j in range(2):
                dc = dq * 2 + j
                for i in range(16):
                    p.mm(bk[:, j * 256:(j + 1) * 256], h2s[:, i, dc * 128:(dc + 1) * 128], oh[:, i, :],
                         start=(i == 0), stop=(i == 15))
            p.copy(xeT[:, dq * 2:dq * 2 + 2, :], bk.v().re("p (j s) -> p j s", j=2), eng='act' if dq % 2 else 'dve')
        for fb in range(4):
            wg = wload(eg_d, e, fb)
            wu = wload(eu_d, e, fb)
            for fc2 in range(4):
                fc = fb * 4 + fc2
                bg = bank()
                for dc in range(16):
                    p.mm(bg[:, 0:256], wg[:, dc, fc2 * 128:(fc2 + 1) * 128], xeT[:, dc, :],
                         start=(dc == 0), stop=(dc == 15))
                bu = bank()
                for dc in range(16):
                    p.mm(bu[:, 0:256], wu[:, dc, fc2 * 128:(fc2 + 1) * 128], xeT[:, dc, :],
                         start=(dc == 0), stop=(dc == 15))
                s_ = sg[fc % 2]
                p.act(s_.v(), bg[:, 0:256], AF.Silu)
                p.tt(hidT[:, fc, :], s_.v(), bu[:, 0:256], ALU.mult)
        for db in range(4):
            wd = wload(ed_d, e, db)
            for sh in range(2):
                bk = bank()
                for fc in range(16):
                    p.mm(bk.v(), hidT[:, fc, sh * 128:(sh + 1) * 128], wd[:, fc, :],
                         start=(fc == 0), stop=(fc == 15))
                p.ts(ye[:, sh, db * 512:(db + 1) * 512], bk.v(), gate[:, sh:sh + 1], ALU.mult)
        p.dma(ye_d[e].re("s p d -> p s d"), ye.v(), eng='sp')
    p.pop()

    p.push()
    yeh = p.sb("yeh", [128, 2 * NE, 1024], BF16)
    idm = p.sb("idm", [128, 32], F32)
    GT = p.sb("GT", [128, 32, 128], BF16)
    xr = [p.sb("xr%d" % i, [128, 1024], F32) for i in range(2)]
    iota_bc2 = iota1.v().re("p (o s) -> p o s", o=1).bc([128, 16, 256])
    for half in range(2):
        hs = slice(half * 1024, (half + 1) * 1024)
        for e in range(NE):
            p.dma(yeh[:, 2 * e:2 * e + 2, :], ye_d[e][:, :, hs].re("s p d -> p s d"), eng='sp' if e % 2 else 'pool')
        for i in range(NT):
            p.ts(idm.v(), idxs.v(), float(1 - 128 * i), ALU.add)
            p.tt(GT.v(), iota1[:, 0:128].re("p (o t) -> p o t", o=1).bc([128, 32, 128]),
                 idm.v().re("p (k o) -> p k o", o=1).bc([128, 32, 128]), ALU.is_equal)
            xa = xr[i % 2]
            p.dma(xa.v(), xmid_d[i * 128:(i + 1) * 128, hs], eng='sp')
            for db in range(2):
                bk = bank()
                for k in range(2 * NE):
                    p.mm(bk.v(), GT[:, k, :], yeh[:, k, db * 512:(db + 1) * 512], start=(k == 0), stop=(k == 2 * NE - 1))
                p.tt(xa[:, db * 512:(db + 1) * 512], xa[:, db * 512:(db + 1) * 512], bk.v(), ALU.add)
            p.dma(xmid_d[i * 128:(i + 1) * 128, hs], xa.v(), eng='pool')
    p.pop()

    p.push()
    nfw_s = p.sb("nfw_s", [128, D], F32)
    p.dma(nfw_s.v(), nfw[0:1, :].bc([128, D]))
    xf = [p.sb("xf%d" % i, [128, D], F32) for i in range(2)]
    junk2 = p.sb("junk2", [128, D], F32)
    ss3 = p.sb("ss3", [128, NT], F32)
    for i in range(NT):
        xo = xf[i % 2]
        p.dma(xo.v(), xmid_d[i * 128:(i + 1) * 128, :], eng='sp')
        p.act(junk2.v(), xo.v(), AF.Square, accum=ss3[:, i:i + 1])
        p.rsqrt(ss3[:, i:i + 1], ss3[:, i:i + 1], 1.0 / D, EPS)
        p.ts(xo.v(), xo.v(), ss3[:, i:i + 1], ALU.mult)
        p.tt(xo.v(), xo.v(), nfw_s.v(), ALU.mult)
        p.dma(out[i * 128:(i + 1) * 128, :], xo.v(), eng='pool')
    p.dma(scr2.v(), projT[0:1, 0:16])
    p.finish([out.v()], scr.v(), scr2.v())
    p.pop()
```
